# Optimizing a Trainium2 kernel written in Bass

```python
import jax
import jax.numpy as jnp
from jax import lax
import numpy as np

D_MODEL = 2048
BATCH = 4
SEQ = 2048
DEPTH = 2
DEC_BATCH = 128
DEC_SEQ = 8
PAST_LEN = 16384
PAGE_SIZE = 128

N_MIXERS = 2
N_A_LAYERS = (DEPTH + 1) // 2
N_B_LAYERS = DEPTH // 2
EPS = 1e-6

GDN_HEAD_K = 128
GDN_HEAD_V = 128
GDN_QK_HEADS = D_MODEL // GDN_HEAD_K
GDN_V_HEADS = 2 * GDN_QK_HEADS
GDN_KEY_DIM = GDN_QK_HEADS * GDN_HEAD_K
GDN_VAL_DIM = GDN_V_HEADS * GDN_HEAD_V
GDN_CONV_DIM = 2 * GDN_KEY_DIM + GDN_VAL_DIM
GDN_PROJ_DIM = GDN_CONV_DIM + GDN_VAL_DIM + 2 * GDN_V_HEADS
GDN_CONV = 4
GDN_CHUNK = 64

CONF_CH = D_MODEL
CONF_K = 31

PEER_HEADS = 8
PEER_NKEYS = 128
PEER_EXPERTS = PEER_NKEYS * PEER_NKEYS
PEER_QDIM = 256
PEER_HALF = PEER_QDIM // 2
PEER_TOPK = 16
PEER_TOKEN_BLOCK = 128

PLE_DIM = 256

kernel_name = 'hybrid_gdn_conformer_peer_step'


def rms_norm(x, g):
    xf = x.astype(jnp.float32)
    y = xf * lax.rsqrt(jnp.mean(xf * xf, axis=-1, keepdims=True) + EPS)
    return (y * g.astype(jnp.float32)).astype(x.dtype)


def layer_norm(x, g, b):
    xf = x.astype(jnp.float32)
    mu = jnp.mean(xf, axis=-1, keepdims=True)
    xc = xf - mu
    y = xc * lax.rsqrt(jnp.mean(xc * xc, axis=-1, keepdims=True) + EPS)
    return (y * g.astype(jnp.float32) + b.astype(jnp.float32)).astype(x.dtype)


def l2_normalize(x):
    return x * lax.rsqrt(jnp.sum(x * x, axis=-1, keepdims=True) + EPS)


def causal_depthwise_conv(x, buf, w):
    K, C = w.shape
    xx = jnp.concatenate([buf.astype(x.dtype), x], axis=1)
    y = lax.conv_general_dilated(xx, w[:, None, :].astype(x.dtype), window_strides=(1,),
                                 padding='VALID', dimension_numbers=('NWC', 'WIO', 'NWC'),
                                 feature_group_count=C)
    return y, xx[:, xx.shape[1] - (K - 1):]


def gated_delta_rule(q, k, v, g, beta, S0):
    B, T, H, DK = q.shape
    DV = v.shape[-1]
    C = min(GDN_CHUNK, T)
    pad = (-T) % C
    if pad:
        pw = ((0, 0), (0, pad), (0, 0), (0, 0))
        q, k, v = jnp.pad(q, pw), jnp.pad(k, pw), jnp.pad(v, pw)
        g, beta = jnp.pad(g, pw[:3]), jnp.pad(beta, pw[:3])
    N = (T + pad) // C

    def to_blocks(t):
        return t.reshape(B, N, C, H, -1).transpose(1, 0, 3, 2, 4)

    q, k, v = to_blocks(q), to_blocks(k), to_blocks(v)
    g = g.reshape(B, N, C, H).transpose(1, 0, 3, 2)
    beta = beta.reshape(B, N, C, H).transpose(1, 0, 3, 2)
    gc = jnp.cumsum(g, axis=-1)
    kb = k * beta[..., None]
    vb = v * beta[..., None]
    tri = jnp.tril(jnp.ones((C, C), dtype=bool))
    strict = jnp.tril(jnp.ones((C, C), dtype=bool), k=-1)
    decay = jnp.exp(jnp.where(tri, gc[..., :, None] - gc[..., None, :], -jnp.inf))
    L = jnp.where(strict, jnp.einsum('nbhik,nbhjk->nbhij', kb, k) * decay, 0.0)
    eye = jnp.eye(C, dtype=jnp.float32)
    Tm = lax.linalg.triangular_solve(L + eye, jnp.broadcast_to(eye, L.shape),
                                     left_side=True, lower=True)
    u = jnp.einsum('nbhij,nbhjd->nbhid', Tm, vb)
    w = jnp.einsum('nbhij,nbhjd->nbhid', Tm, kb * jnp.exp(gc)[..., None])

    def step(S, xs):
        qc, kc, uc, wc, gcc, dec = xs
        v_new = uc - jnp.einsum('bhck,bhkv->bhcv', wc, S)
        attn = jnp.einsum('bhik,bhjk->bhij', qc, kc) * dec
        o = (jnp.einsum('bhck,bhkv->bhcv', qc * jnp.exp(gcc)[..., None], S)
             + jnp.einsum('bhij,bhjv->bhiv', attn, v_new))
        glast = gcc[..., -1]
        S = (S * jnp.exp(glast)[..., None, None]
             + jnp.einsum('bhck,bhcv->bhkv', kc * jnp.exp(glast[..., None] - gcc)[..., None], v_new))
        return S, o

    S, o = lax.scan(step, S0, (q, k, u, w, gc, decay))
    o = o.transpose(1, 0, 3, 2, 4).reshape(B, N * C, H, DV)[:, :T]
    return o, S


def gated_deltanet(a, conv_buf, S0, w_in, conv_w, a_log, dt_bias, o_norm, w_out):
    B, T, _ = a.shape
    proj = a @ w_in
    qkv = proj[..., :GDN_CONV_DIM]
    z = proj[..., GDN_CONV_DIM:GDN_CONV_DIM + GDN_VAL_DIM]
    b_raw = proj[..., GDN_CONV_DIM + GDN_VAL_DIM:GDN_CONV_DIM + GDN_VAL_DIM + GDN_V_HEADS]
    a_raw = proj[..., GDN_CONV_DIM + GDN_VAL_DIM + GDN_V_HEADS:]
    qkv_c, new_buf = causal_depthwise_conv(qkv, conv_buf, conv_w)
    qkv_c = jax.nn.silu(qkv_c.astype(jnp.float32))
    q = qkv_c[..., :GDN_KEY_DIM].reshape(B, T, GDN_QK_HEADS, GDN_HEAD_K)
    k = qkv_c[..., GDN_KEY_DIM:2 * GDN_KEY_DIM].reshape(B, T, GDN_QK_HEADS, GDN_HEAD_K)
    v = qkv_c[..., 2 * GDN_KEY_DIM:].reshape(B, T, GDN_V_HEADS, GDN_HEAD_V)
    rep = GDN_V_HEADS // GDN_QK_HEADS
    q = jnp.repeat(l2_normalize(q) * (GDN_HEAD_K ** -0.5), rep, axis=2)
    k = jnp.repeat(l2_normalize(k), rep, axis=2)
    beta = jax.nn.sigmoid(b_raw.astype(jnp.float32))
    g = -jnp.exp(a_log.astype(jnp.float32)) * jax.nn.softplus(
        a_raw.astype(jnp.float32) + dt_bias.astype(jnp.float32))
    o, S = gated_delta_rule(q, k, v, g, beta, S0.astype(jnp.float32))
    zf = z.astype(jnp.float32).reshape(B, T, GDN_V_HEADS, GDN_HEAD_V)
    o = rms_norm(o, o_norm) * jax.nn.silu(zf)
    y = o.reshape(B, T, GDN_VAL_DIM).astype(a.dtype) @ w_out
    return y, new_buf, S.astype(a.dtype)


def conformer_conv(a, buf, w_in, b_in, dw_w, dw_b, ln_g, ln_b, w_out, b_out):
    h = a @ w_in + b_in
    glu = h[..., :CONF_CH] * jax.nn.sigmoid(h[..., CONF_CH:])
    c, new_buf = causal_depthwise_conv(glu, buf, dw_w)
    c = jax.nn.silu(layer_norm(c + dw_b, ln_g, ln_b))
    return c @ w_out + b_out, new_buf


def peer_ffn(xn, w_q, keys1, keys2, u, v):
    B, T, D = xn.shape
    n = B * T
    xt = xn.reshape(n, D)
    q = (xt @ w_q).reshape(n, PEER_HEADS, PEER_QDIM).astype(jnp.float32)
    s1 = jnp.einsum('nhd,kd->nhk', q[..., :PEER_HALF], keys1.astype(jnp.float32))
    s2 = jnp.einsum('nhd,kd->nhk', q[..., PEER_HALF:], keys2.astype(jnp.float32))
    v1, i1 = lax.top_k(s1, PEER_TOPK)
    v2, i2 = lax.top_k(s2, PEER_TOPK)
    cand = (v1[..., :, None] + v2[..., None, :]).reshape(n, PEER_HEADS, PEER_TOPK * PEER_TOPK)
    cand_id = (i1[..., :, None] * PEER_NKEYS + i2[..., None, :]).reshape(n, PEER_HEADS, PEER_TOPK * PEER_TOPK)
    sc, pos = lax.top_k(cand, PEER_TOPK)
    eid = jnp.take_along_axis(cand_id, pos, axis=-1)
    gate = jax.nn.softmax(sc, axis=-1)
    nb = -(-n // PEER_TOKEN_BLOCK)
    pad = nb * PEER_TOKEN_BLOCK - n
    xt_p = jnp.pad(xt, ((0, pad), (0, 0))).reshape(nb, PEER_TOKEN_BLOCK, D)
    eid_p = jnp.pad(eid, ((0, pad), (0, 0), (0, 0))).reshape(nb, PEER_TOKEN_BLOCK, PEER_HEADS, PEER_TOPK)
    gate_p = jnp.pad(gate, ((0, pad), (0, 0), (0, 0))).reshape(nb, PEER_TOKEN_BLOCK, PEER_HEADS, PEER_TOPK)

    def block(args):
        xb, eb, gb = args
        hb = jnp.einsum('thkd,td->thk', u[eb], xb).astype(jnp.float32)
        ab = (gb * jax.nn.gelu(hb, approximate=False)).astype(xb.dtype)
        return jnp.einsum('thk,thkd->td', ab, v[eb])

    out = lax.map(block, (xt_p, eid_p, gate_p))
    return out.reshape(nb * PEER_TOKEN_BLOCK, D)[:n].reshape(B, T, D)


def decoder_trunk(x, p, gdn_S, gdn_buf, conf_buf, norm_mix, norm_ffn, norm_ple, final_norm,
                  gdn_w_in, gdn_conv_w, gdn_a_log, gdn_dt_bias, gdn_o_norm, gdn_w_out,
                  conf_w_in, conf_b_in, conf_dw_w, conf_dw_b, conf_ln_g, conf_ln_b, conf_w_out, conf_b_out,
                  peer_w_q, peer_keys1, peer_keys2, peer_u, peer_v, ple_w_proj, ple_w_gate, ple_b_gate):
    h = x
    new_S, new_gbuf, new_cbuf = [], [], []
    for i in range(DEPTH):
        j = i // N_MIXERS
        a = rms_norm(h, norm_mix[i])
        if i % N_MIXERS == 0:
            y, buf, S = gated_deltanet(a, gdn_buf[j], gdn_S[j], gdn_w_in[j], gdn_conv_w[j], gdn_a_log[j],
                                       gdn_dt_bias[j], gdn_o_norm[j], gdn_w_out[j])
            new_S.append(S)
            new_gbuf.append(buf)
        else:
            y, buf = conformer_conv(a, conf_buf[j], conf_w_in[j], conf_b_in[j], conf_dw_w[j], conf_dw_b[j],
                                    conf_ln_g[j], conf_ln_b[j], conf_w_out[j], conf_b_out[j])
            new_cbuf.append(buf)
        h = h + y
        h = h + peer_ffn(rms_norm(h, norm_ffn[i]), peer_w_q[i], peer_keys1[i], peer_keys2[i],
                         peer_u[i], peer_v[i])
        gate = jax.nn.sigmoid(rms_norm(h, norm_ple[i]) @ ple_w_gate[i] + ple_b_gate[i])
        h = h + (p[i].astype(h.dtype) @ ple_w_proj[i]) * gate
    return rms_norm(h, final_norm), jnp.stack(new_S), jnp.stack(new_gbuf), jnp.stack(new_cbuf)


def setup_inputs(seed: int = 0) -> dict:
    key = jax.random.key(seed)
    ks = iter(jax.random.split(key, 64))
    f32 = jnp.float32

    def nrm(shape, scale):
        return jax.random.normal(next(ks), shape, f32) * scale

    def gain(shape):
        return 1.0 + 0.05 * jax.random.normal(next(ks), shape, f32)

    d = {}
    d['x_prompt'] = nrm((BATCH, SEQ, D_MODEL), 1.0)
    d['x_sample'] = nrm((DEC_BATCH, DEC_SEQ, D_MODEL), 1.0)
    d['p_prompt'] = nrm((DEPTH, BATCH, SEQ, PLE_DIM), 1.0)
    d['p_sample'] = nrm((DEPTH, DEC_BATCH, DEC_SEQ, PLE_DIM), 1.0)
    d['state_gdn_recurrent'] = nrm((N_A_LAYERS, DEC_BATCH, GDN_V_HEADS, GDN_HEAD_K, GDN_HEAD_V), 0.1)
    d['state_gdn_conv'] = nrm((N_A_LAYERS, DEC_BATCH, GDN_CONV - 1, GDN_CONV_DIM), 1.0)
    d['state_conf_conv'] = nrm((N_B_LAYERS, DEC_BATCH, CONF_K - 1, CONF_CH), 0.5)
    d['norm_mix'] = gain((DEPTH, D_MODEL))
    d['norm_ffn'] = gain((DEPTH, D_MODEL))
    d['norm_ple'] = gain((DEPTH, D_MODEL))
    d['final_norm'] = gain((D_MODEL,))
    d['gdn_w_in'] = nrm((N_A_LAYERS, D_MODEL, GDN_PROJ_DIM), D_MODEL ** -0.5)
    d['gdn_conv_w'] = nrm((N_A_LAYERS, GDN_CONV, GDN_CONV_DIM), GDN_CONV ** -0.5)
    d['gdn_a_log'] = jnp.log(jax.random.uniform(next(ks), (N_A_LAYERS, GDN_V_HEADS), f32, 1.0, 16.0))
    d['gdn_dt_bias'] = nrm((N_A_LAYERS, GDN_V_HEADS), 0.1)
    d['gdn_o_norm'] = gain((N_A_LAYERS, GDN_HEAD_V))
    d['gdn_w_out'] = nrm((N_A_LAYERS, GDN_VAL_DIM, D_MODEL), GDN_VAL_DIM ** -0.5)
    d['conf_w_in'] = nrm((N_B_LAYERS, D_MODEL, 2 * CONF_CH), D_MODEL ** -0.5)
    d['conf_b_in'] = nrm((N_B_LAYERS, 2 * CONF_CH), 0.02)
    d['conf_dw_w'] = nrm((N_B_LAYERS, CONF_K, CONF_CH), CONF_K ** -0.5)
    d['conf_dw_b'] = nrm((N_B_LAYERS, CONF_CH), 0.02)
    d['conf_ln_g'] = gain((N_B_LAYERS, CONF_CH))
    d['conf_ln_b'] = nrm((N_B_LAYERS, CONF_CH), 0.02)
    d['conf_w_out'] = nrm((N_B_LAYERS, CONF_CH, D_MODEL), CONF_CH ** -0.5)
    d['conf_b_out'] = nrm((N_B_LAYERS, D_MODEL), 0.02)
    d['peer_w_q'] = nrm((DEPTH, D_MODEL, PEER_HEADS * PEER_QDIM), D_MODEL ** -0.5)
    d['peer_keys1'] = nrm((DEPTH, PEER_NKEYS, PEER_HALF), PEER_HALF ** -0.5)
    d['peer_keys2'] = nrm((DEPTH, PEER_NKEYS, PEER_HALF), PEER_HALF ** -0.5)
    d['peer_u'] = nrm((DEPTH, PEER_EXPERTS, D_MODEL), D_MODEL ** -0.5)
    d['peer_v'] = nrm((DEPTH, PEER_EXPERTS, D_MODEL), 0.25)
    d['ple_w_proj'] = nrm((DEPTH, PLE_DIM, D_MODEL), PLE_DIM ** -0.5)
    d['ple_w_gate'] = nrm((DEPTH, D_MODEL, D_MODEL), D_MODEL ** -0.5)
    d['ple_b_gate'] = nrm((DEPTH, D_MODEL), 0.02)
    return d


def reference(x_prompt, x_sample, p_prompt, p_sample, state_gdn_recurrent, state_gdn_conv, state_conf_conv,
              norm_mix, norm_ffn, norm_ple, final_norm,
              gdn_w_in, gdn_conv_w, gdn_a_log, gdn_dt_bias, gdn_o_norm, gdn_w_out,
              conf_w_in, conf_b_in, conf_dw_w, conf_dw_b, conf_ln_g, conf_ln_b, conf_w_out, conf_b_out,
              peer_w_q, peer_keys1, peer_keys2, peer_u, peer_v,
              ple_w_proj, ple_w_gate, ple_b_gate):
    weights = (norm_mix, norm_ffn, norm_ple, final_norm,
               gdn_w_in, gdn_conv_w, gdn_a_log, gdn_dt_bias, gdn_o_norm, gdn_w_out,
               conf_w_in, conf_b_in, conf_dw_w, conf_dw_b, conf_ln_g, conf_ln_b, conf_w_out, conf_b_out,
               peer_w_q, peer_keys1, peer_keys2, peer_u, peer_v,
               ple_w_proj, ple_w_gate, ple_b_gate)
    bp = x_prompt.shape[0]
    dt = x_prompt.dtype
    zero_S = jnp.zeros((N_A_LAYERS, bp, GDN_V_HEADS, GDN_HEAD_K, GDN_HEAD_V), dt)
    zero_gbuf = jnp.zeros((N_A_LAYERS, bp, GDN_CONV - 1, GDN_CONV_DIM), dt)
    zero_cbuf = jnp.zeros((N_B_LAYERS, bp, CONF_K - 1, CONF_CH), dt)
    y_prompt, gS_p, gbuf_p, cbuf_p = decoder_trunk(x_prompt, p_prompt, zero_S, zero_gbuf, zero_cbuf, *weights)
    y_sample, gS_s, gbuf_s, cbuf_s = decoder_trunk(x_sample, p_sample, state_gdn_recurrent, state_gdn_conv,
                                                   state_conf_conv, *weights)
    return (y_prompt, y_sample, gS_p, gS_s, gbuf_p, gbuf_s, cbuf_p, cbuf_s)
```

```python
import contextlib
import os
import numpy as np
import concourse.bass as bass
import concourse.mybir as mybir
from concourse.bass_utils import run_bass_kernel_spmd

F32 = mybir.dt.float32
BF16 = mybir.dt.bfloat16
U32 = mybir.dt.uint32
I32 = mybir.dt.int32
ALU = mybir.AluOpType
AF = mybir.ActivationFunctionType
AX = mybir.AxisListType

ENGS = ("pe", "dve", "act", "pool", "sp")
GEN = 30000
N_DMA_SEMS = 24
SEM_POOLS = {"sp": (0, 12), "pool": (12, 8), "bulk": (20, 4)}
EPS = 1e-6

D = 2048
NTP = 2048
NT = 2176
NCH = 17
NOWN = 1184
BIG = 30000.0


class Buf:
    __slots__ = ("name", "last_w", "readers", "psum")

    def __init__(self, name, psum=False):
        self.name = name
        self.last_w = None
        self.readers = []
        self.psum = psum


class V:
    __slots__ = ("ap", "b")

    def __init__(self, ap, b):
        self.ap = ap
        self.b = b

    def __getitem__(self, k):
        return V(self.ap[k], self.b)

    def bc(self, shape):
        return V(self.ap.to_broadcast(list(shape)), self.b)

    def un(self, axis):
        return V(self.ap.unsqueeze(axis), self.b)

    def re(self, pattern, **kw):
        return V(self.ap.rearrange(pattern, **kw), self.b)

    def cast(self, dt):
        return V(self.ap.bitcast(dt), self.b)

    @property
    def shape(self):
        return self.ap.shape


WRITE_KEYS = ("out", "accum_out", "ap")


class Sched:
    def __init__(self, nc):
        self.nc = nc
        self.ops = {e: [] for e in ENGS}
        self.cnt = {e: 0 for e in ENGS}
        self.seen = {e: {} for e in ENGS}
        self.dma_cnt = [0] * N_DMA_SEMS
        self.dma_rr = {k: 0 for k in SEM_POOLS}
        self.final_tokens = []
        self.n_ops = 0

    def _need(self, eng, tok):
        if tok is None:
            return
        kind, key, val = tok
        if kind == "e":
            if key == eng and eng == "pe":
                return
            g = (val - 1) // GEN
            skey = ("e", key, g)
            v = val - g * GEN
            for gg in range(g + 1, g + 6):
                if self.seen[eng].get(("e", key, gg), 0) > 0:
                    return
        else:
            skey = ("d", key)
            v = val
        if self.seen[eng].get(skey, 0) >= v:
            return
        self.seen[eng][skey] = v
        self.ops[eng].append(("wait", skey, v))

    @staticmethod
    def _compact(toks):
        best = {}
        for t in toks:
            k = (t[0], t[1])
            if k not in best or best[k][2] < t[2]:
                best[k] = t
        return list(best.values())

    def _deps(self, eng, reads, writes):
        for b in reads:
            self._need(eng, b.last_w)
            if b.psum:
                for t in b.readers:
                    if t[0] == "e" and t[1] != eng:
                        self._need(eng, t)
        for b in writes:
            self._need(eng, b.last_w)
            for t in b.readers:
                self._need(eng, t)

    def _commit(self, tok, reads, writes):
        for b in reads:
            b.readers.append(tok)
            if len(b.readers) > 16:
                b.readers = self._compact(b.readers)
        for b in writes:
            b.last_w = tok
            b.readers = []
        self.n_ops += 1

    def op(self, eng, fn, reads=(), writes=()):
        self._deps(eng, reads, writes)
        self.cnt[eng] += 1
        tok = ("e", eng, self.cnt[eng])
        self.ops[eng].append(("op", fn, self.cnt[eng]))
        self._commit(tok, reads, writes)
        return tok

    def dma(self, eng, fn, reads=(), writes=(), final=False, bulk=False):
        self._deps(eng, reads, writes)
        pk = "bulk" if bulk else eng
        base_, n_ = SEM_POOLS[pk]
        s = base_ + self.dma_rr[pk]
        self.dma_rr[pk] = (self.dma_rr[pk] + 1) % n_
        if self.dma_cnt[s] > 0:
            self._need(eng, ("d", s, self.dma_cnt[s]))
        self.dma_cnt[s] += 16
        tok = ("d", s, self.dma_cnt[s])
        self.ops[eng].append(("dma", fn, s))
        self._commit(tok, reads, writes)
        if final:
            self.final_tokens.append(tok)
        return tok

    def fence(self):
        toks = [("e", e, self.cnt[e]) for e in ENGS if self.cnt[e] > 0]
        toks += [("d", s, self.dma_cnt[s]) for s in range(N_DMA_SEMS) if self.dma_cnt[s] > 0]
        for e in ENGS:
            for t in toks:
                if not (t[0] == "e" and t[1] == e):
                    self._need(e, t)

    def emit(self):
        nc = self.nc
        for t in self.final_tokens:
            self._need("sp", t)
        with contextlib.ExitStack() as es:
            esem = {}
            for e in ENGS:
                for g in range(max(1, (self.cnt[e] + GEN - 1) // GEN)):
                    esem[(e, g)] = es.enter_context(nc.semaphore(f"p_{e}{g}"))
            dsem = [es.enter_context(nc.semaphore(f"d{i}")) for i in range(N_DMA_SEMS)]
            block = es.enter_context(nc.Block())

            def run(eng_name, engine):
                for item in self.ops[eng_name]:
                    if item[0] == "wait":
                        skey, v = item[1], item[2]
                        if skey[0] == "e":
                            engine.wait_ge(esem[(skey[1], skey[2])], v)
                        else:
                            engine.wait_ge(dsem[skey[1]], v)
                    elif item[0] == "op":
                        ins = item[1](engine)
                        ins.then_inc(esem[(eng_name, (item[2] - 1) // GEN)], 1)
                    else:
                        ins = item[1](engine)
                        ins.then_inc(dsem[item[2]], 16)

            @block.tensor
            def _(eng):
                run("pe", eng)

            @block.vector
            def _(eng):
                run("dve", eng)

            @block.scalar
            def _(eng):
                run("act", eng)

            @block.gpsimd
            def _(eng):
                run("pool", eng)

            @block.sync
            def _(eng):
                run("sp", eng)


class EngP:
    def __init__(self, K, name):
        self.K = K
        self.name = name

    def __getattr__(self, meth):
        K, name = self.K, self.name

        def call(**kw):
            reads, writes = [], []
            for key, val in kw.items():
                if isinstance(val, V):
                    (writes if key in WRITE_KEYS else reads).append(val.b)

            def fn(e):
                args = {k: (v.ap if isinstance(v, V) else v) for k, v in kw.items()}
                return getattr(e, meth)(**args)

            return K.S.op(name, fn, reads, writes)

        return call


class Kern:
    ARENA_F32 = 53000

    def __init__(self):
        self.nc = bass.Bass("TRN2", target_bir_lowering=False)
        self.S = Sched(self.nc)
        self.es = contextlib.ExitStack()
        self.pe = EngP(self, "pe")
        self.dve = EngP(self, "dve")
        self.act = EngP(self, "act")
        self.pool = EngP(self, "pool")
        self.arena = self.es.enter_context(self.nc.sbuf_tensor("arena", [128, self.ARENA_F32], F32))
        self.top = 0
        self.banks = [V(self.es.enter_context(self.nc.psum_tensor(f"bank{i}", [128, 512], F32))[:, :],
                        Buf(f"bank{i}", psum=True)) for i in range(8)]
        self.ins = {}
        self.outs = {}

    def din(self, name, shape, dt=F32):
        ap = self.nc.dram_tensor(name, list(shape), dt, kind="ExternalInput").ap()
        self.ins[name] = (tuple(shape), dt)
        return ap

    def dout(self, name, shape, dt=F32):
        ap = self.nc.dram_tensor(name, list(shape), dt, kind="ExternalOutput").ap()
        self.outs[name] = (tuple(shape), dt)
        return ap

    def dscr(self, name, shape, dt):
        ap = self.nc.dram_tensor(name, list(shape), dt, kind="Internal").ap()
        return V(ap, Buf(name))

    def alloc(self, name, shape, dt=F32):
        n = int(np.prod(shape[1:]))
        words = n if dt in (F32, U32, I32) else (n + 1) // 2
        words = (words + 1) // 2 * 2
        off = self.top
        self.top += words
        assert self.top <= self.ARENA_F32, f"arena overflow at {name}: {self.top}"
        ap = self.arena[0:shape[0], off:off + words]
        if dt != F32:
            ap = ap.bitcast(dt)
        ap = ap[:, 0:n]
        if len(shape) == 3:
            ap = ap.rearrange("p (a b) -> p a b", a=shape[1])
        elif len(shape) == 4:
            ap = ap.rearrange("p (a b c) -> p a b c", a=shape[1], b=shape[2])
        return V(ap, Buf(name))

    def mark(self):
        return self.top

    def release(self, m):
        self.S.fence()
        self.top = m

    def bank(self, i, dt=F32, shape=None):
        v = self.banks[i]
        ap = v.ap
        if dt != F32:
            ap = ap.bitcast(dt)
        if shape is not None:
            n = int(np.prod(shape[1:]))
            ap = ap[0:shape[0], 0:n]
            if len(shape) == 3:
                ap = ap.rearrange("p (a b) -> p a b", a=shape[1])
        return V(ap, v.b)

    def dma(self, q, out, in_, final=False, bulk=False, **kw):
        reads = [in_.b] if isinstance(in_, V) else []
        writes = [out.b] if isinstance(out, V) else []
        o = out.ap if isinstance(out, V) else out
        i = in_.ap if isinstance(in_, V) else in_
        return self.S.dma(q, lambda e: e.dma_start(out=o, in_=i, **kw), reads, writes, final=final, bulk=bulk)

    def gather(self, out, table_ap, idx, extra_reads=()):
        o, ia = out.ap, idx.ap
        return self.S.dma(
            "pool",
            lambda e: e.indirect_dma_start(out=o, out_offset=None, in_=table_ap,
                                           in_offset=bass.IndirectOffsetOnAxis(ap=ia, axis=0)),
            reads=[idx.b] + list(extra_reads), writes=[out.b])


def build_consts(K):
    C = {}
    pool, dve = K.pool, K.dve

    def mask(name, steps, op, base, cm, shape=(128, 128)):
        t = K.alloc(name, list(shape), F32)
        pool.memset(ap=t, constant=1.0)
        pool.affine_select(out=t, in_=t, pattern=steps, compare_op=op, fill=0.0, base=base,
                           channel_multiplier=cm)
        return t

    C["ident"] = mask("ident", [[-1, 128]], ALU.is_equal, 0, 1)
    C["ones"] = K.alloc("ones", [128, 128], F32)
    pool.memset(ap=C["ones"], constant=1.0)
    C["identb"] = K.alloc("identb", [128, 128], BF16)
    dve.tensor_copy(out=C["identb"], in_=C["ident"])
    C["Lst_p"] = mask("Lst_p", [[-1, 128]], ALU.is_gt, 0, 1)
    C["Uin_p"] = mask("Uin_p", [[1, 128]], ALU.is_ge, 0, -1)
    C["LS_p"] = mask("LS_p", [[0, 128]], ALU.is_equal, -127, 1)
    blk = K.alloc("blk", [128, 128], F32)
    pool.memset(ap=blk, constant=1.0)
    pool.affine_select(out=blk, in_=blk, pattern=[[-8, 16], [0, 8]], compare_op=ALU.is_ge, fill=0.0,
                       base=0, channel_multiplier=1)
    pool.affine_select(out=blk, in_=blk, pattern=[[8, 16], [0, 8]], compare_op=ALU.is_ge, fill=0.0,
                       base=7, channel_multiplier=-1)
    C["blk"] = blk
    C["Lst_s"] = K.alloc("Lst_s", [128, 128], F32)
    dve.tensor_tensor(out=C["Lst_s"], in0=C["Lst_p"], in1=blk, op=ALU.mult)
    C["Uin_s"] = K.alloc("Uin_s", [128, 128], F32)
    dve.tensor_tensor(out=C["Uin_s"], in0=C["Uin_p"], in1=blk, op=ALU.mult)
    C["LS_s"] = mask("LS_s", [[-8, 16], [0, 8]], ALU.is_equal, -7, 1)
    rm = K.alloc("rowmask", [128, 16], F32)
    pool.memset(ap=rm, constant=1.0)
    pool.affine_select(out=rm, in_=rm, pattern=[[-8, 16]], compare_op=ALU.is_ge, fill=0.0, base=0,
                       channel_multiplier=1)
    pool.affine_select(out=rm, in_=rm, pattern=[[8, 16]], compare_op=ALU.is_ge, fill=0.0, base=7,
                       channel_multiplier=-1)
    C["rowmask"] = rm
    C["lastmask"] = mask("lastmask", [[-8, 16]], ALU.is_equal, -7, 1, shape=(128, 16))
    return C


def rmsnorm_tok(K, x, g, out, sq, ss, n=D):
    K.act.activation(out=sq, in_=x, func=AF.Square, accum_out=ss)
    K.act.activation(out=ss, in_=ss, func=AF.Sqrt, scale=1.0 / n, bias=EPS)
    K.dve.reciprocal(out=ss, in_=ss)
    K.dve.scalar_tensor_tensor(out=out, in0=x, scalar=ss, in1=g, op0=ALU.mult, op1=ALU.mult)


_flip = [0]


def evac(K, out, in_):
    _flip[0] ^= 1
    if _flip[0]:
        K.act.activation(out=out, in_=in_, func=AF.Copy)
    else:
        K.dve.tensor_copy(out=out, in_=in_)


def transpose_chunks(K, C, src, dst, nk, banks=(2, 3), npart=128):
    for g0 in range(0, nk, 4):
        n = min(4, nk - g0)
        pb = K.bank(banks[(g0 // 4) % 2], BF16, [128, 4, 128])
        for j in range(n):
            K.pe.transpose(out=pb[:, j, 0:npart], in_=src[0:npart, (g0 + j) * 128:(g0 + j + 1) * 128],
                           identity=C["identb"][0:npart, 0:npart])
        evac(K, dst[:, g0:g0 + n, 0:npart], pb[:, 0:n, 0:npart])


def stage_gdn(K, C, I, O, osel_scr, conv_chunks=None):
    dve, act, pe, pool = K.dve, K.act, K.pe, K.pool
    aT_scr = K.dscr("aT_scr", [128, 16, NT], BF16)
    w_in = I["gdn_w_in"]

    m0 = K.mark()
    gmix = K.alloc("gmix", [128, D])
    K.dma("sp", gmix, I["norm_mix"][0].partition_broadcast(128))
    xts = [K.alloc(f"xt{i}", [128, D]) for i in range(2)]
    xns = [K.alloc(f"xn{i}", [128, D], BF16) for i in range(2)]
    xTs = [K.alloc(f"xT{i}", [128, 16, 128], BF16) for i in range(2)]
    sq = K.alloc("sq", [128, D])
    sss = [K.alloc(f"ss{i}", [128, 1]) for i in range(2)]
    for i in range(NCH):
        xt, xn, xT, ss = xts[i % 2], xns[i % 2], xTs[i % 2], sss[i % 2]
        src = I["xp"][i * 128:(i + 1) * 128, :] if i < 16 else I["xs"][:, :]
        K.dma("sp", xt, src)
        rmsnorm_tok(K, xt, gmix, xn, sq, ss)
        transpose_chunks(K, C, xn, xT, 16)
        K.dma("sp", aT_scr[:, :, i * 128:(i + 1) * 128], xT)
    K.release(m0)
    STOP = os.environ.get("GDN_STOP", "")
    NHQ = int(os.environ.get("GDN_NHQ", "16"))
    if STOP == "g0":
        return

    P = {}
    for nm in ("negbeta", "gc", "kdec", "bge", "decS"):
        P[nm] = K.alloc(nm, [128, NCH, 32])
    P["decSs"] = K.alloc("decSs", [128, 32, 16])
    cw = K.alloc("cw", [128, 64, 4])
    for j in range(4):
        K.dma("sp", cw[:, :, j], I["gdn_conv_w"][j].rearrange("(c p) -> p c", p=128), allow_slow_non_contiguous=True)
    onorm = K.alloc("onorm", [128, 1])
    K.dma("sp", onorm, I["gdn_o_norm"].rearrange("o p -> p o"), allow_slow_non_contiguous=True)
    sel = K.alloc("sel", [128, 4])
    K.dma("sp", sel, I["sel"][:, :])

    m1 = K.mark()
    wbd = K.alloc("wbd", [128, 16, 64], BF16)
    K.dma("pool", wbd, w_in[:, 12288:12352].rearrange("(k p) n -> p k n", p=128))
    bd = K.alloc("bd", [128, NCH, 64])
    ab = K.alloc("aTbd", [128, 16, 1024], BF16)
    for c0 in range(0, NCH, 8):
        nchk = min(8, NCH - c0)
        K.dma("sp", ab[:, :, 0:nchk * 128], aT_scr[:, :, c0 * 128:(c0 + nchk) * 128])
        pb = K.bank(0, F32, [128, 8, 64])
        for j in range(nchk):
            for k in range(16):
                pe.matmul(out=pb[:, j, :], lhsT=ab[:, k, j * 128:(j + 1) * 128], rhs=wbd[:, k, :],
                          start=(k == 0), stop=(k == 15))
        evac(K, bd[:, c0:c0 + nchk, :], pb[:, 0:nchk, :])
    alog = K.alloc("alog", [128, 32])
    dtb = K.alloc("dtb", [128, 32])
    K.dma("sp", alog, I["gdn_a_log"][0].partition_broadcast(128))
    K.dma("sp", dtb, I["gdn_dt_bias"][0].partition_broadcast(128))
    act.activation(out=alog, in_=alog, func=AF.Exp)
    dve.tensor_scalar(out=alog, in0=alog, scalar1=-1.0, scalar2=None, op0=ALU.mult)
    beta = K.alloc("beta", [128, NCH, 32])
    act.activation(out=beta, in_=bd[:, :, 0:32], func=AF.Sigmoid)
    dve.tensor_scalar(out=P["negbeta"], in0=beta, scalar1=-1.0, scalar2=None, op0=ALU.mult)
    g = K.alloc("g", [128, NCH, 32])
    dve.tensor_tensor(out=g, in0=bd[:, :, 32:64], in1=dtb.un(1).bc([128, NCH, 32]), op=ALU.add)
    act.activation(out=g, in_=g, func=AF.Exp)
    act.activation(out=g, in_=g, func=AF.Ln, bias=1.0)
    dve.tensor_tensor(out=g, in0=g, in1=alog.un(1).bc([128, NCH, 32]), op=ALU.mult)
    pg = K.bank(1, F32, [128, 16, 32])
    pe.matmul(out=pg, lhsT=C["Uin_p"], rhs=g[:, 0:16, :], start=True, stop=True)
    evac(K, P["gc"][:, 0:16, :], pg)
    pg2 = K.bank(0, F32, [128, 1, 32])
    pe.matmul(out=pg2, lhsT=C["Uin_s"], rhs=g[:, 16:17, :], start=True, stop=True)
    evac(K, P["gc"][:, 16:17, :], pg2)
    gl = K.alloc("gl", [128, NCH, 32])
    pl = K.bank(1, F32, [128, 16, 32])
    pe.matmul(out=pl, lhsT=C["LS_p"], rhs=P["gc"][:, 0:16, :], start=True, stop=True)
    evac(K, gl[:, 0:16, :], pl)
    pl2 = K.bank(0, F32, [128, 1, 32])
    pe.matmul(out=pl2, lhsT=C["LS_s"], rhs=P["gc"][:, 16:17, :], start=True, stop=True)
    evac(K, gl[:, 16:17, :], pl2)
    act.activation(out=P["decS"], in_=gl, func=AF.Exp)
    dve.tensor_tensor(out=gl, in0=gl, in1=P["gc"], op=ALU.subtract)
    act.activation(out=P["kdec"], in_=gl, func=AF.Exp)
    eg = K.alloc("eg", [128, NCH, 32])
    act.activation(out=eg, in_=P["gc"], func=AF.Exp)
    dve.tensor_tensor(out=P["bge"], in0=eg, in1=beta, op=ALU.mult)
    lm = K.alloc("lm", [128, 32, 16])
    dve.tensor_tensor(out=lm, in0=P["gc"][:, 16, :].un(2).bc([128, 32, 16]),
                      in1=C["lastmask"].un(1).bc([128, 32, 16]), op=ALU.mult)
    pls = K.bank(1, F32, [128, 32, 16])
    pe.matmul(out=pls, lhsT=C["ones"], rhs=lm, start=True, stop=True)
    act.activation(out=P["decSs"], in_=pls, func=AF.Exp)
    K.release(m1)
    if STOP == "g1":
        return

    gbp_sb = K.alloc("gbp_sb", [128, 64, 3])
    aTb = [K.alloc(f"aTb{i}", [128, 16, 256], BF16) for i in range(2)]
    w6 = [K.alloc(f"w6_{i}", [128, 16, 128], BF16) for i in range(6)]
    raw = [K.alloc(f"raw{i}", [128, 2051 + 176], BF16) for i in range(4)]
    cv = K.alloc("cv", [128, NT])
    tmp1 = K.alloc("tmp1", [128, NT])
    rinv = K.alloc("rinv", [128, 512])
    qT = K.alloc("qT", [128, NT], BF16)
    kT = K.alloc("kT", [128, NT], BF16)
    zs = [K.alloc(f"zs{i}", [128, NT], BF16) for i in range(2)]
    vTb = tmp1.cast(BF16)[:, 0:NT]
    k_tok = K.alloc("k_tok", [128, NCH, 128], BF16)
    v_tok = [K.alloc(f"v_tok{i}", [128, NCH, 128], BF16) for i in range(2)]
    oTr = [K.alloc(f"oTr{i}", [128, NT], BF16) for i in range(2)]
    osel = K.alloc("osel", [128, NOWN], BF16)
    st48 = K.alloc("st48", [48, 4, 128])
    smp = K.alloc("smp", [128, 128])
    gb48 = K.alloc("gb48", [128, 128])
    dve.memset(ap=gb48, constant=0.0)
    gbs_sb = K.alloc("gbs_sb", [48, 128])
    Gs = K.alloc("Gs", [128, 4, 128])
    Atm = K.alloc("Atm", [128, 4, 128])
    dg = K.alloc("dg", [128, 4, 128])
    t1 = K.alloc("t1", [128, 4, 128])
    Dm = K.alloc("Dm", [128, 4, 128])
    Egm = K.alloc("Egm", [128, 4, 128])
    N0s = [K.alloc(f"N0_{i}", [128, 8, 128]) for i in range(2)]
    Pn = [K.alloc(f"Pn{i}", [128, 4, 128]) for i in range(2)]
    Qn = [K.alloc(f"Qn{i}", [128, 4, 128]) for i in range(2)]
    Xn = [K.alloc(f"Xn{i}", [128, 4, 128]) for i in range(2)]
    TmT = K.alloc("TmT", [128, 8, 128], BF16)
    vbs = [K.alloc(f"vb{i}", [128, 8, 128], BF16) for i in range(2)]
    kbgs = [K.alloc(f"kbg{i}", [128, 8, 128], BF16) for i in range(2)]
    CH = []
    for i in range(2):
        CH.append(dict(wT=K.alloc(f"wT{i}", [128, 8, 128], BF16), u=K.alloc(f"u{i}", [128, 8, 128], BF16),
                       qg=K.alloc(f"qg{i}", [128, 8, 128], BF16), A=K.alloc(f"A{i}", [128, 8, 128], BF16),
                       kd=K.alloc(f"kd{i}", [128, 8, 128], BF16)))
    Sf = [K.alloc(f"Sf{i}", [128, 128]) for i in range(2)]
    Sb = [K.alloc(f"Sb{i}", [128, 128], BF16) for i in range(2)]
    vnew = [K.alloc(f"vnew{i}", [128, 128], BF16) for i in range(2)]
    print("GDN arena top (f32 words):", K.top)

    def colsel(hq):
        return [hq * 128, 2048 + hq * 128, 4096 + (2 * hq) * 128, 4096 + (2 * hq + 1) * 128,
                8192 + (2 * hq) * 128, 8192 + (2 * hq + 1) * 128]

    blocks = [(i * 256, 256) for i in range(8)] + [(2048, 128)]
    xdbg = None

    def neumann(nchain, nsteps, N0, hook=None):
        pT = K.bank(3, F32, [128, 4, 128])
        pA = K.bank(4, F32, [128, 4, 128])
        pB = K.bank(5, F32, [128, 4, 128])
        pC = K.bank(6, F32, [128, 4, 128])
        for h0 in range(0, nchain, 4):
            n = min(4, nchain - h0)
            sl = slice(h0, h0 + n)
            for j in range(n):
                pe.transpose(out=pT[:, j, :], in_=N0[:, h0 + j, :], identity=C["ident"])
            act.activation(out=Qn[0][:, 0:n, :], in_=pT[:, 0:n, :], func=AF.Copy)
            dve.tensor_tensor(out=Xn[1][:, 0:n, :], in0=pT[:, 0:n, :],
                              in1=C["ident"].un(1).bc([128, n, 128]), op=ALU.add)
            for k in range(1, nsteps):
                a, b = (k - 1) % 2, k % 2
                last = (k == nsteps - 1)
                Pa = (lambda j: N0[:, h0 + j, :]) if k == 1 else (lambda j, a=a: Pn[a][:, j, :])
                for j in range(n):
                    pe.matmul(out=pA[:, j, :], lhsT=Qn[a][:, j, :], rhs=Pa(j), start=True, stop=True)
                if not last:
                    for j in range(n):
                        pe.matmul(out=pB[:, j, :], lhsT=Pa(j), rhs=Qn[a][:, j, :], start=True, stop=True)
                act.activation(out=Pn[b][:, 0:n, :], in_=pA[:, 0:n, :], func=AF.Copy)
                if not last:
                    dve.tensor_copy(out=Qn[b][:, 0:n, :], in_=pB[:, 0:n, :])
                for j in range(n):
                    pe.matmul(out=pC[:, j, :], lhsT=Pn[b][:, j, :], rhs=Xn[b][:, j, :], start=True, stop=True)
                if last:
                    dve.tensor_tensor(out=TmT[:, sl, :], in0=pC[:, 0:n, :], in1=Xn[b][:, 0:n, :], op=ALU.add)
                else:
                    dve.tensor_tensor(out=Xn[1 - b][:, 0:n, :], in0=pC[:, 0:n, :], in1=Xn[b][:, 0:n, :],
                                      op=ALU.add)
                if hook is not None:
                    hook()

    def elem(hq, chunks, ch, sample, N0, vb, kbg):
        nc_ = len(chunks)
        Lst = C["Lst_s"] if sample else C["Lst_p"]
        Uin = C["Uin_s"] if sample else C["Uin_p"]
        pG = K.bank(0, F32, [128, 4, 128])
        pAt = K.bank(1, F32, [128, 4, 128])
        for ci, c in enumerate(chunks):
            cs = slice(c * 128, (c + 1) * 128)
            pe.matmul(out=pG[:, ci, :], lhsT=kT[:, cs], rhs=kT[:, cs], start=True, stop=True)
            pe.matmul(out=pAt[:, ci, :], lhsT=kT[:, cs], rhs=qT[:, cs], start=True, stop=True)
        dve.tensor_tensor(out=Gs[:, 0:nc_, :], in0=pG[:, 0:nc_, :], in1=Lst.un(1).bc([128, nc_, 128]), op=ALU.mult)
        dve.tensor_tensor(out=Atm[:, 0:nc_, :], in0=pAt[:, 0:nc_, :], in1=Uin.un(1).bc([128, nc_, 128]), op=ALU.mult)
        yield
        pR = [K.bank(2, F32, [128, 4, 128]), K.bank(7, F32, [128, 4, 128])]
        for ci, c in enumerate(chunks):
            for e in range(2):
                x = ci * 2 + e
                hv = 2 * hq + e
                gcol = P["gc"][:, c, hv:hv + 1]
                dve.tensor_scalar(out=dg[:, x % 4, :], in0=C["ident"], scalar1=gcol, scalar2=None, op0=ALU.mult)
                pr = pR[x // 4][:, x % 4, :]
                pe.matmul(out=pr, lhsT=C["ones"], rhs=dg[:, x % 4, :], start=True, stop=True)
                dve.tensor_scalar(out=t1[:, x % 4, :], in0=pr, scalar1=gcol, scalar2=0.0, op0=ALU.subtract, op1=ALU.max)
                act.activation(out=Dm[:, x % 4, :], in_=t1[:, x % 4, :], func=AF.Exp, scale=-1.0)
                dve.scalar_tensor_tensor(out=N0[:, x, :], in0=Gs[:, ci, :], scalar=P["negbeta"][:, c, hv:hv + 1],
                                         in1=Dm[:, x % 4, :], op0=ALU.mult, op1=ALU.mult)
                dve.tensor_scalar(out=t1[:, x % 4, :], in0=pr, scalar1=gcol, scalar2=0.0, op0=ALU.subtract, op1=ALU.min)
                act.activation(out=Dm[:, x % 4, :], in_=t1[:, x % 4, :], func=AF.Exp)
                dve.tensor_tensor(out=ch["A"][:, x, :], in0=Atm[:, ci, :], in1=Dm[:, x % 4, :], op=ALU.mult)
                act.activation(out=Egm[:, x % 4, :], in_=pr, func=AF.Exp)
                dve.tensor_tensor(out=ch["qg"][:, x, :], in0=qT[:, c * 128:(c + 1) * 128], in1=Egm[:, x % 4, :], op=ALU.mult)
                dve.tensor_scalar(out=vb[:, x, :], in0=v_tok[e][:, c, :], scalar1=P["negbeta"][:, c, hv:hv + 1],
                                  scalar2=-1.0, op0=ALU.mult, op1=ALU.mult)
                pool.tensor_scalar(out=kbg[:, x, :], in0=k_tok[:, c, :], scalar1=P["bge"][:, c, hv:hv + 1],
                                   scalar2=1.0, op0=ALU.mult, op1=ALU.mult)
                pool.tensor_scalar(out=ch["kd"][:, x, :], in0=k_tok[:, c, :], scalar1=P["kdec"][:, c, hv:hv + 1],
                                   scalar2=1.0, op0=ALU.mult, op1=ALU.mult)
                yield

    def exhaust(g_):
        for _ in g_:
            pass

    def solve(nc_, ch, sample, N0, vb, kbg, hook=None):
        neumann(2 * nc_, 3 if sample else 7, N0, hook)
        pU = [K.bank(0, F32, [128, 4, 128]), K.bank(1, F32, [128, 4, 128])]
        pW = [K.bank(2, F32, [128, 4, 128]), K.bank(7, F32, [128, 4, 128])]
        for x in range(2 * nc_):
            if sample:
                pe.matmul(out=pU[x // 4][:, x % 4, :], lhsT=vb[:, x, :], rhs=TmT[:, x, :], start=True, stop=True)
            else:
                pe.matmul(out=pU[x // 4][:, x % 4, :], lhsT=TmT[:, x, :], rhs=vb[:, x, :], start=True, stop=True)
            pe.matmul(out=pW[x // 4][:, x % 4, :], lhsT=kbg[:, x, :], rhs=TmT[:, x, :], start=True, stop=True)
        for h0 in range(0, 2 * nc_, 4):
            n = min(4, 2 * nc_ - h0)
            act.activation(out=ch["u"][:, h0:h0 + n, :], in_=pU[h0 // 4][:, 0:n, :], func=AF.Copy)
            dve.tensor_copy(out=ch["wT"][:, h0:h0 + n, :], in_=pW[h0 // 4][:, 0:n, :])

    def load_w6(hq_):
        cols_ = colsel(hq_)
        for i in range(6):
            K.dma("pool", w6[i], w_in[:, cols_[i]:cols_[i] + 128].rearrange("(k p) n -> p k n", p=128))

    load_w6(0)
    for hq in range(NHQ):
        cols = colsel(hq)
        if STOP == "w6":
            continue
        for i in range(4):
            K.dma("sp", st48[:, i, :], I["sgc"].rearrange("s t c -> (s t) c")[:, cols[i]:cols[i] + 128])
        pst = K.bank(7, F32, [128, 4, 48])
        for i in range(4):
            pe.transpose(out=pst[:, i, :], in_=st48[:, i, :], identity=C["ident"][0:48, 0:48])
        for i in range(4):
            rs_ = raw[i][:, 2051:2051 + 176].re("p (s t) -> p s t", t=11)
            evac(K, rs_[:, :, 0:3], pst[:, i, :].re("p (s t) -> p s t", t=3))
            dve.memset(ap=raw[i][:, 0:3], constant=0.0)
        if STOP == "st":
            continue
        for bi, (c0, nb) in enumerate(blocks):
            if STOP == "blk0" and bi > 0:
                continue
            if (STOP == "blkp" or "nosamp" in os.environ.get("DBG", "")) and bi == 8:
                continue
            ab_ = aTb[bi % 2]
            for kh in range(2):
                K.dma("sp", ab_[:, 8 * kh:8 * kh + 8, 0:nb], aT_scr[:, 8 * kh:8 * kh + 8, c0:c0 + nb])
            DBG = os.environ.get("DBG", "")
            for i in range(6):
                pb = K.bank(i // 2, F32, [128, 2, 256])[:, i % 2, 0:nb]
                if "nomm" not in DBG:
                    for k in range(16):
                        pe.matmul(out=pb, lhsT=(C["identb"] if "idw" in DBG else w6[i][:, k, :]),
                                  rhs=(xdbg[:, 0:nb] if "xd" in DBG else ab_[:, k, 0:nb]), start=(k == 0), stop=(k == 15))
                if "noev" in DBG:
                    continue
                if i < 4:
                    if c0 < 2048:
                        evac(K, raw[i][:, 3 + c0:3 + c0 + nb], pb)
                        if c0 == 1792 and "nogbp" not in DBG:
                            dve.tensor_copy(out=gbp_sb[:, cols[i] // 128, :], in_=pb[:, 253:256])
                    else:
                        rs_ = raw[i][:, 2051:2051 + 176].re("p (s t) -> p s t", t=11)
                        act.activation(out=smp, in_=pb, func=AF.Copy)
                        dve.tensor_copy(out=rs_[:, :, 3:11], in_=smp.re("p (s t) -> p s t", t=8))
                        dve.tensor_copy(out=gb48[:, 0:48].re("p (s t) -> p s t", t=3),
                                        in_=smp.re("p (s t) -> p s t", t=8)[:, :, 5:8])
                        pgb = K.bank(3, F32, [128, 128])
                        pe.transpose(out=pgb, in_=gb48, identity=C["ident"])
                        act.activation(out=gbs_sb, in_=pgb[0:48, :], func=AF.Copy)
                        K.dma("sp", O["gbs"].rearrange("s t c -> (s t) c")[:, cols[i]:cols[i] + 128], gbs_sb, final=True)
                elif "nozs" not in DBG:
                    act.activation(out=zs[i - 4][:, c0:c0 + nb], in_=pb,
                                   func=(AF.Copy if os.environ.get("NOSILU") else AF.Silu))
        if STOP == "proj":
            continue
        for i in range(4):
            cc = cols[i] // 128
            rp = raw[i][:, 0:2051]
            rs_ = raw[i][:, 2051:2051 + 176].re("p (s t) -> p s t", t=11)
            cvp = cv[:, 0:2048]
            cvs = cv[:, 2048:NT].re("p (s t) -> p s t", t=8)
            dve.tensor_scalar(out=cvp, in0=rp[:, 3:2051], scalar1=cw[:, cc, 3:4], scalar2=None, op0=ALU.mult)
            dve.tensor_scalar(out=cvs, in0=rs_[:, :, 3:11], scalar1=cw[:, cc, 3:4], scalar2=None, op0=ALU.mult)
            for j in range(3):
                dve.scalar_tensor_tensor(out=cvp, in0=rp[:, j:j + 2048], scalar=cw[:, cc, j:j + 1], in1=cvp,
                                         op0=ALU.mult, op1=ALU.add)
                dve.scalar_tensor_tensor(out=cvs, in0=rs_[:, :, j:j + 8], scalar=cw[:, cc, j:j + 1], in1=cvs,
                                         op0=ALU.mult, op1=ALU.add)
            if i < 2:
                act.activation(out=cv, in_=cv, func=AF.Silu)
                act.activation(out=tmp1, in_=cv, func=AF.Square)
                dst = qT if i == 0 else kT
                for b0 in range(0, NT, 512):
                    nb = min(512, NT - b0)
                    pss = K.bank(3, F32, [128, 512])[:, 0:nb]
                    pe.matmul(out=pss, lhsT=C["ones"], rhs=tmp1[:, b0:b0 + nb], start=True, stop=True)
                    act.activation(out=rinv[:, 0:nb], in_=pss, func=AF.Sqrt, bias=EPS)
                    dve.reciprocal(out=rinv[:, 0:nb], in_=rinv[:, 0:nb])
                    dve.scalar_tensor_tensor(out=dst[:, b0:b0 + nb], in0=cv[:, b0:b0 + nb],
                                             scalar=(128.0 ** -0.5 if i == 0 else 1.0), in1=rinv[:, 0:nb],
                                             op0=ALU.mult, op1=ALU.mult)
                if i == 1:
                    transpose_chunks(K, C, kT, k_tok, NCH)
            else:
                act.activation(out=vTb, in_=cv, func=AF.Silu)
                transpose_chunks(K, C, vTb, v_tok[i - 2], NCH)
        if STOP == "conv":
            continue
        if conv_chunks:
            for _ in range(min(4, len(conv_chunks))):
                dst, src = conv_chunks.pop(0)
                K.dma("pool", dst, src, bulk=True)
        if hq + 1 < NHQ:
            load_w6(hq + 1)
        for e in range(2):
            dve.memset(ap=Sf[e], constant=0.0)
            dve.memset(ap=Sb[e], constant=0.0)
        exhaust(elem(hq, [0, 1, 2, 3], CH[0], False, N0s[0], vbs[0], kbgs[0]))
        for gi in range(4):
            chunks = list(range(gi * 4, gi * 4 + 4))
            ch = CH[gi % 2]
            p_ = gi % 2
            if gi + 1 < 4:
                nxt = elem(hq, list(range(gi * 4 + 4, gi * 4 + 8)), CH[1 - p_], False, N0s[1 - p_], vbs[1 - p_], kbgs[1 - p_])
            else:
                nxt = elem(hq, [16], CH[0], True, N0s[0], vbs[0], kbgs[0])
            solve(4, ch, False, N0s[p_], vbs[p_], kbgs[p_], hook=lambda g_=nxt: next(g_, None))
            exhaust(nxt)
            for ci, c in enumerate(chunks):
                for e in range(2):
                    x = ci * 2 + e
                    hv = 2 * hq + e
                    pv = K.bank(0 + e, F32, [128, 128])
                    pe.matmul(out=pv, lhsT=ch["wT"][:, x, :], rhs=Sb[e], start=True, stop=True)
                    dve.tensor_tensor(out=vnew[e], in0=ch["u"][:, x, :], in1=pv, op=ALU.subtract)
                    po = K.bank(2 + e, F32, [128, 128])
                    pe.matmul(out=po, lhsT=Sb[e], rhs=ch["qg"][:, x, :], start=True, stop=False)
                    pe.matmul(out=po, lhsT=vnew[e], rhs=ch["A"][:, x, :], start=False, stop=True)
                    pd = K.bank(4 + e, F32, [128, 128])
                    pe.matmul(out=pd, lhsT=ch["kd"][:, x, :], rhs=vnew[e], start=True, stop=True)
                    dve.scalar_tensor_tensor(out=Sf[e], in0=Sf[e], scalar=P["decS"][:, c, hv:hv + 1], in1=pd,
                                             op0=ALU.mult, op1=ALU.add)
                    act.activation(out=Sb[e], in_=Sf[e], func=AF.Copy)
                    act.activation(out=oTr[e][:, c * 128:(c + 1) * 128], in_=po, func=AF.Copy)
        for e in range(2):
            K.dma("sp", O["gSp"][2 * hq + e], Sf[e], final=True)
        ch = CH[0]
        solve(1, ch, True, N0s[0], vbs[0], kbgs[0])
        Sall = cv.re("p (s v) -> p s v", s=17)[:, 0:16, :]
        Snew = tmp1.re("p (s v) -> p s v", s=17)[:, 0:16, :]
        Sab = raw[0][:, 0:2048].re("p (s v) -> p s v", s=16)
        Vblk = raw[1][:, 0:2048].re("p (s v) -> p s v", s=16)
        cs = slice(2048, NT)
        for e in range(2):
            hv = 2 * hq + e
            for sh in range(2):
                K.dma("sp", Sall[:, 8 * sh:8 * sh + 8, :], I["sgr"][8 * sh:8 * sh + 8, hv].rearrange("s k v -> k s v"))
            act.activation(out=Sab, in_=Sall, func=AF.Copy)
            pws = K.bank(0, F32, [128, 128])
            pos = K.bank(1, F32, [128, 128])
            for s in range(16):
                pe.matmul(out=pws[:, 8 * s:8 * s + 8], lhsT=Sab[:, s, :], rhs=ch["wT"][:, e, 8 * s:8 * s + 8],
                          start=True, stop=True)
                pe.matmul(out=pos[:, 8 * s:8 * s + 8], lhsT=Sab[:, s, :], rhs=ch["qg"][:, e, 8 * s:8 * s + 8],
                          start=True, stop=True)
            vnT = Egm[:, 0, :]
            dve.tensor_tensor(out=vnT, in0=ch["u"][:, e, :], in1=pws, op=ALU.subtract)
            vnTb = TmT[:, 7, :]
            act.activation(out=vnTb, in_=vnT, func=AF.Copy)
            pvt = K.bank(2, BF16, [128, 128])
            pe.transpose(out=pvt, in_=vnTb, identity=C["identb"])
            act.activation(out=vnew[e], in_=pvt, func=AF.Copy)
            poa = K.bank(3, F32, [128, 128])
            pe.matmul(out=poa, lhsT=vnew[e], rhs=ch["A"][:, e, :], start=True, stop=True)
            osb = Egm[:, 1, :]
            act.activation(out=osb, in_=pos, func=AF.Copy)
            dve.tensor_tensor(out=oTr[e][:, cs], in0=osb, in1=poa, op=ALU.add)
            dve.tensor_tensor(out=Vblk, in0=vnew[e].un(1).bc([128, 16, 128]),
                              in1=C["rowmask"].un(2).bc([128, 16, 128]), op=ALU.mult)
            for q4 in range(4):
                psd = K.bank(4 + q4, F32, [128, 4, 128])
                pe.matmul(out=psd, lhsT=ch["kd"][:, e, :], rhs=Vblk[:, 4 * q4:4 * q4 + 4, :], start=True, stop=True)
                dve.tensor_tensor(out=Snew[:, 4 * q4:4 * q4 + 4, :], in0=Sall[:, 4 * q4:4 * q4 + 4, :],
                                  in1=P["decSs"][:, hv, 4 * q4:4 * q4 + 4].un(2).bc([128, 4, 128]), op=ALU.mult)
                dve.tensor_tensor(out=Snew[:, 4 * q4:4 * q4 + 4, :], in0=Snew[:, 4 * q4:4 * q4 + 4, :],
                                  in1=psd, op=ALU.add)
            K.dma("sp", O["gSs"][:, hv].rearrange("s k v -> k s v"), Snew, final=True)
        if STOP == "samp":
            continue
        for e in range(2):
            hv = 2 * hq + e
            og = cv
            act.activation(out=tmp1, in_=oTr[e], func=AF.Square)
            for b0 in range(0, NT, 512):
                nb = min(512, NT - b0)
                pss = K.bank(3, F32, [128, 512])[:, 0:nb]
                pe.matmul(out=pss, lhsT=C["ones"], rhs=tmp1[:, b0:b0 + nb], start=True, stop=True)
                act.activation(out=rinv[:, 0:nb], in_=pss, func=AF.Sqrt, scale=1.0 / 128, bias=EPS)
                dve.reciprocal(out=rinv[:, 0:nb], in_=rinv[:, 0:nb])
                dve.scalar_tensor_tensor(out=og[:, b0:b0 + nb], in0=oTr[e][:, b0:b0 + nb], scalar=onorm,
                                         in1=rinv[:, 0:nb], op0=ALU.mult, op1=ALU.mult)
            dve.tensor_tensor(out=og, in0=og, in1=zs[e], op=ALU.mult)
            dve.tensor_scalar(out=osel[:, 0:32], in0=og[:, 992:1024], scalar1=sel[:, 1:2], scalar2=None, op0=ALU.mult)
            dve.tensor_scalar(out=tmp1[:, 0:1024], in0=og[:, 0:1024], scalar1=sel[:, 0:1], scalar2=None, op0=ALU.mult)
            dve.scalar_tensor_tensor(out=osel[:, 32:1056], in0=og[:, 1024:2048], scalar=sel[:, 1:2],
                                     in1=tmp1[:, 0:1024], op0=ALU.mult, op1=ALU.add)
            act.activation(out=osel[:, 1056:NOWN], in_=og[:, 2048:NT], func=AF.Copy)
            K.dma("sp", osel_scr[hv], osel)
    for t in range(3):
        K.dma("sp", O["gbp"][t].rearrange("(c p) -> p c", p=128), gbp_sb[:, :, t], final=True,
              allow_slow_non_contiguous=True)


def stage_oproj(K, C, I, H, osel_scr):
    dve, act, pe = K.dve, K.act, K.pe
    w_out = I["gdn_w_out"]
    m = K.mark()
    ot = K.alloc("ot", [128, 32, NOWN], BF16)
    for hv in range(32):
        K.dma("sp", ot[:, hv, :], osel_scr[hv])
    dve.memset(ap=H[0], constant=0.0)
    K.dma("sp", H[0][0:32, :], I["xo"][0:32, :])
    for t in range(1, 9):
        K.dma("sp", H[t], I["xo"][32 + (t - 1) * 128:32 + t * 128, :])
    K.dma("sp", H[9], I["xs"][:, :])
    wb = [K.alloc(f"wob{i}", [128, 32, 256], BF16) for i in range(2)]
    n = 0
    for blk in range(8):
        w = wb[blk % 2]
        K.dma("pool", w, w_out[:, blk * 256:(blk + 1) * 256].rearrange("(h p) n -> p h n", p=128))
        for t in range(10):
            np_ = 32 if t == 0 else 128
            c0 = 0 if t == 0 else 32 + (t - 1) * 128
            ps = K.bank(n % 8, F32, [128, 2, 256])[0:np_, (n // 8) % 2, :]
            n += 1
            for hv in range(32):
                pe.matmul(out=ps, lhsT=ot[:, hv, c0:c0 + np_], rhs=w[:, hv, :], start=(hv == 0), stop=(hv == 31))
            hs = H[t][0:np_, blk * 256:(blk + 1) * 256]
            dve.tensor_tensor(out=hs, in0=hs, in1=ps, op=ALU.add)
    K.release(m)


def stage_peer(K, C, I, H, layer, tiles, TB):
    dve, act, pe, pool = K.dve, K.act, K.pe, K.pool
    NEG = -1.0e30
    m = K.mark()
    w_q = I["peer_w_q"][layer]
    uv_tab, uv_bufs = TB[0], TB[1][layer]
    gffn = K.alloc("gffn", [128, D])
    K.dma("sp", gffn, I["norm_ffn"][layer].partition_broadcast(128))
    kT = K.alloc("kTk", [128, 2, 128])
    ktmp = K.alloc("ktmp", [128, 2, 128])
    K.dma("sp", ktmp[:, 0, :], I["peer_keys1"][layer])
    K.dma("sp", ktmp[:, 1, :], I["peer_keys2"][layer])
    pk = K.bank(0, F32, [128, 2, 128])
    for hf in range(2):
        pe.transpose(out=pk[:, hf, :], in_=ktmp[:, hf, :], identity=C["ident"])
    dve.tensor_copy(out=kT, in_=pk)
    iota = K.alloc("iota", [128, 256])
    pool.iota(out=iota, pattern=[[1, 256]], base=0, channel_multiplier=0, allow_small_or_imprecise_dtypes=True)
    xnbs = [K.alloc(f"xnb{i}", [128, D], BF16) for i in range(2)]
    xnT = K.alloc("xnT", [128, 16, 128], BF16)
    ss = K.alloc("ssp", [128, 1])
    NW = 2
    wq = [K.alloc(f"wq{i}", [128, 16, 128], BF16) for i in range(NW)]
    tv = K.alloc("tv", [128, 16, 16])
    ti = K.alloc("ti", [128, 16, 16], U32)
    tif = K.alloc("tif", [128, 16, 16])
    cand = K.alloc("cand", [128, 8, 256])
    scr1 = K.alloc("scr1", [128, 256])
    cid = K.alloc("cid", [128, 8, 256])
    qT = cand.re("p h (a b) -> p (h a) b", a=2)
    sc0 = cid.re("p h (a b) -> p (h a) b", a=2)
    scv = K.alloc("scv", [128, 8, 16])
    pos = K.alloc("pos", [128, 8, 16], U32)
    posf = K.alloc("posf", [128, 8, 16])
    junk = K.alloc("junk", [128, 256])
    eidf = K.alloc("eidf", [128, 128])
    eids = [K.alloc(f"eid{i}", [128, 128], U32) for i in range(2)]
    negm = K.alloc("negm", [128, 8])
    zsum = K.alloc("zsum", [128, 8])
    gates = [K.alloc(f"gate{i}", [128, 8, 16]) for i in range(2)]
    NR = 8
    hvs = [K.alloc(f"hv{i}", [128, 1]) for i in range(NR)]
    acs = [K.alloc(f"ac{i}", [128, 1]) for i in range(NR)]
    NG = 8
    gb = [K.alloc(f"gb{i}", [128, 2 * D], BF16) for i in range(NG)]
    dgs = [K.alloc(f"dgd{i}", [128, 128], BF16) for i in range(3)]
    accb = [K.bank(b_, F32, [128, 512]) for b_ in (0, 1, 6, 7)]
    sq = xnT.re("p a b -> p (a b)")
    print("PEER arena top:", K.top)

    def load_wq(blk):
        K.dma("pool", wq[blk % NW], w_q[:, blk * 128:(blk + 1) * 128].rearrange("(k p) n -> p k n", p=128))

    def front(t, par):
        h = H[t]
        eid, gate, xnb = eids[par], gates[par], xnbs[par]
        load_wq(0)
        rmsnorm_tok(K, h, gffn, xnb, sq, ss)
        yield
        transpose_chunks(K, C, xnb, xnT, 16)
        yield
        for blk in range(16):
            if blk + 1 < 16:
                load_wq(blk + 1)
            w = wq[blk % NW]
            pq = K.bank(4 + (blk // 2) % 2, F32, [128, 2, 128])
            for k in range(16):
                pe.matmul(out=pq[:, blk % 2, :], lhsT=w[:, k, :], rhs=xnT[:, k, :], start=(k == 0), stop=(k == 15))
            if blk % 2 == 1:
                evac(K, qT[:, blk - 1:blk + 1, :], pq)
                yield
        for g4 in range(4):
            psc = K.bank(4 + g4 % 2, F32, [128, 4, 128])
            for j in range(4):
                hc = g4 * 4 + j
                pe.matmul(out=psc[:, j, :], lhsT=qT[:, hc, :], rhs=kT[:, hc % 2, :], start=True, stop=True)
            evac(K, sc0[:, g4 * 4:(g4 + 1) * 4, :], psc)
        yield
        for hc in range(16):
            dve.max(out=tv[:, hc, 0:8], in_=sc0[:, hc, :])
            dve.max_index(out=ti[:, hc, 0:8], in_max=tv[:, hc, 0:8], in_values=sc0[:, hc, :])
            dve.match_replace(out=scr1[:, 0:128], in_to_replace=tv[:, hc, 0:8], in_values=sc0[:, hc, :], imm_value=NEG)
            dve.max(out=tv[:, hc, 8:16], in_=scr1[:, 0:128])
            dve.max_index(out=ti[:, hc, 8:16], in_max=tv[:, hc, 8:16], in_values=scr1[:, 0:128])
            if hc % 4 == 3:
                yield
        dve.tensor_copy(out=tif, in_=ti)
        tv4 = tv.re("p (h f) k -> p h f k", f=2)
        ti4 = tif.re("p (h f) k -> p h f k", f=2)
        c4 = cand.re("p h (i j) -> p h i j", j=16)
        d4 = cid.re("p h (i j) -> p h i j", j=16)
        for hh in range(8):
            dve.tensor_tensor(out=c4[:, hh], in0=tv4[:, hh, 0, :].un(2).bc([128, 16, 16]),
                              in1=tv4[:, hh, 1, :].un(1).bc([128, 16, 16]), op=ALU.add)
            dve.scalar_tensor_tensor(out=d4[:, hh], in0=ti4[:, hh, 0, :].un(2).bc([128, 16, 16]), scalar=128.0,
                                     in1=ti4[:, hh, 1, :].un(1).bc([128, 16, 16]), op0=ALU.mult, op1=ALU.add)
        yield
        for hh in range(8):
            dve.max(out=scv[:, hh, 0:8], in_=cand[:, hh, :])
            dve.max_index(out=pos[:, hh, 0:8], in_max=scv[:, hh, 0:8], in_values=cand[:, hh, :])
            dve.match_replace(out=scr1, in_to_replace=scv[:, hh, 0:8], in_values=cand[:, hh, :], imm_value=NEG)
            dve.max(out=scv[:, hh, 8:16], in_=scr1)
            dve.max_index(out=pos[:, hh, 8:16], in_max=scv[:, hh, 8:16], in_values=scr1)
            if hh % 4 == 3:
                yield
        dve.tensor_copy(out=posf, in_=pos)
        for hh in range(8):
            for k in range(16):
                sl = hh * 16 + k
                dve.scalar_tensor_tensor(out=junk, in0=iota, scalar=posf[:, hh, k:k + 1], in1=cid[:, hh, :],
                                         op0=ALU.is_equal, op1=ALU.mult, accum_out=eidf[:, sl:sl + 1])
            yield
        if layer:
            dve.tensor_scalar(out=eidf, in0=eidf, scalar1=float(layer * 16384), scalar2=None, op0=ALU.add)
        dve.tensor_copy(out=eid, in_=eidf)
        dve.tensor_scalar(out=negm, in0=scv[:, :, 0], scalar1=-1.0, scalar2=None, op0=ALU.mult)
        for hh in range(8):
            act.activation(out=gate[:, hh, :], in_=scv[:, hh, :], func=AF.Exp, bias=negm[:, hh:hh + 1],
                           accum_out=zsum[:, hh:hh + 1])
        dve.reciprocal(out=zsum, in_=zsum)
        dve.tensor_tensor(out=gate, in0=gate, in1=zsum.un(2).bc([128, 8, 16]), op=ALU.mult)
        yield

    def exhaust(g):
        for _ in g:
            pass

    gi = 0
    exhaust(front(tiles[0], 0))
    for idx, t in enumerate(tiles):
        par = idx % 2
        h = H[t]
        eid, gate, xnb = eids[par], gates[par], xnbs[par]
        gflat = gate.re("p h k -> p (h k)")
        npr = 32 if (layer == 0 and t == 0) else 128
        nxt = front(tiles[idx + 1], 1 - par) if idx + 1 < len(tiles) else None
        for sl in range(128):
            g = gb[gi % NG]
            gi += 1
            K.gather(g[0:npr], uv_tab, eid[0:npr, sl:sl + 1], extra_reads=uv_bufs)
            hv, ac = hvs[sl % NR], acs[sl % NR]
            dve.scalar_tensor_tensor(out=g[0:npr, 0:D], in0=g[0:npr, 0:D], scalar=1.0, in1=xnb[0:npr], op0=ALU.mult,
                                     op1=ALU.mult, accum_out=hv[0:npr])
            act.activation(out=ac[0:npr], in_=hv[0:npr], func=AF.Gelu)
            act.activation(out=ac[0:npr], in_=ac[0:npr], func=AF.Copy, scale=gflat[0:npr, sl:sl + 1])
            dgt = dgs[sl % 3]
            act.activation(out=dgt[0:npr, 0:npr], in_=C["ident"][0:npr, 0:npr], func=AF.Copy, scale=ac[0:npr, 0:1])
            for q in range(4):
                pe.matmul(out=accb[q][0:npr], lhsT=dgt[0:npr, 0:npr], rhs=g[0:npr, D + q * 512:D + (q + 1) * 512],
                          start=(sl == 0), stop=(sl == 127))
            if nxt is not None and sl % 4 == 3:
                next(nxt, None)
        if nxt is not None:
            exhaust(nxt)
        for q in range(4):
            hq_ = h[0:npr, q * 512:(q + 1) * 512]
            dve.tensor_tensor(out=hq_, in0=hq_, in1=accb[q][0:npr], op=ALU.add)
    K.release(m)


def stage_ple(K, C, I, H, layer, tiles):
    dve, act, pe = K.dve, K.act, K.pe
    m = K.mark()
    gple = K.alloc("gple", [128, D])
    K.dma("sp", gple, I["norm_ple"][layer].partition_broadcast(128))
    bg = K.alloc("bg", [128, D])
    K.dma("sp", bg, I["ple_b_gate"][layer].partition_broadcast(128))
    wp = K.alloc("wp", [128, 2, D], BF16)
    K.dma("pool", wp, I["ple_w_proj"][layer].rearrange("(k p) n -> p k n", p=128))
    pnT = [K.alloc(f"pnT{t}", [128, 16, 128], BF16) for t in range(len(tiles))]
    pT = [K.alloc(f"pT{t}", [128, 2, 128], BF16) for t in range(len(tiles))]
    pn = K.alloc("pn", [128, D], BF16)
    pt = K.alloc("pt", [128, 256])
    ptb = K.alloc("ptb", [128, 256], BF16)
    sq = K.alloc("sqe", [128, D])
    ss = K.alloc("sse", [128, 1])
    gs = K.alloc("gs", [128, 256])
    wg = [K.alloc(f"wg{i}", [128, 16, 256], BF16) for i in range(2)]
    print("PLE arena top:", K.top)
    for ti_, t in enumerate(tiles):
        rmsnorm_tok(K, H[t], gple, pn, sq, ss)
        transpose_chunks(K, C, pn, pnT[ti_], 16)
        if t == 0:
            dve.memset(ap=pt, constant=0.0)
            K.dma("sp", pt[0:32, :], I["pp"][layer, 0:32, :])
        elif t == 9:
            K.dma("sp", pt, I["psm"][layer])
        else:
            K.dma("sp", pt, I["pp"][layer, 32 + (t - 1) * 128:32 + t * 128, :])
        act.activation(out=ptb, in_=pt, func=AF.Copy)
        transpose_chunks(K, C, ptb, pT[ti_], 2)
    n = 0
    for blk in range(8):
        w = wg[blk % 2]
        cs_ = slice(blk * 256, (blk + 1) * 256)
        K.dma("pool", w, I["ple_w_gate"][layer][:, cs_].rearrange("(k p) n -> p k n", p=128))
        for ti_, t in enumerate(tiles):
            pg = K.bank(n % 4, F32, [128, 256])
            pp_ = K.bank(4 + n % 4, F32, [128, 256])
            n += 1
            for k in range(16):
                pe.matmul(out=pg, lhsT=pnT[ti_][:, k, :], rhs=w[:, k, :], start=(k == 0), stop=(k == 15))
            for k in range(2):
                pe.matmul(out=pp_, lhsT=pT[ti_][:, k, :], rhs=wp[:, k, cs_], start=(k == 0), stop=(k == 1))
            dve.tensor_tensor(out=gs, in0=pg, in1=bg[:, cs_], op=ALU.add)
            act.activation(out=gs, in_=gs, func=AF.Sigmoid)
            dve.tensor_tensor(out=gs, in0=gs, in1=pp_, op=ALU.mult)
            hs = H[t][:, cs_]
            dve.tensor_tensor(out=hs, in0=hs, in1=gs, op=ALU.add)
    K.release(m)


NCT = 1184


def stage_conf(K, C, I, O, H):
    dve, act, pe, pool = K.dve, K.act, K.pe, K.pool
    w_in = I["conf_w_in"]
    mA = K.mark()
    cbf = K.alloc("cbf", [128, 16, 1152], BF16)
    mB = K.mark()
    def chan(name, src_row):
        t = K.alloc(name, [128, 16])
        K.dma("sp", t, src_row.rearrange("(c p) -> p c", p=128), allow_slow_non_contiguous=True)
        return t
    b_v = chan("b_v", I["conf_b_in"][0, 0:2048])
    b_g = chan("b_g", I["conf_b_in"][0, 2048:4096])
    dwb = chan("dwb", I["conf_dw_b"][0])
    dww = K.alloc("dww", [128, 16, 31])
    for j in range(31):
        K.dma("sp", dww[:, :, j], I["conf_dw_w"][j].rearrange("(c p) -> p c", p=128), allow_slow_non_contiguous=True)
    sel = K.alloc("selc", [128, 4])
    K.dma("sp", sel, I["sel"][:, :])
    aT = K.alloc("aTc", [128, 16, NCT], BF16)
    mT = K.mark()
    gmix = K.alloc("gmix1", [128, D])
    K.dma("sp", gmix, I["norm_mix"][1].partition_broadcast(128))
    xn = K.alloc("xnc", [128, D], BF16)
    xT = K.alloc("xTc", [128, 16, 128], BF16)
    sq = K.alloc("sqc", [128, D])
    ss = K.alloc("ssc", [128, 1])
    for t in range(10):
        rmsnorm_tok(K, H[t], gmix, xn, sq, ss)
        transpose_chunks(K, C, xn, xT, 16)
        if t == 0:
            dve.tensor_copy(out=aT[:, :, 0:32], in_=xT[:, :, 0:32])
        else:
            dve.tensor_copy(out=aT[:, :, 32 + (t - 1) * 128:32 + t * 128], in_=xT)
    K.release(mT)
    st = K.alloc("stc", [128, 4, 128])
    dve.memset(ap=st, constant=0.0)
    scc2 = I["scc"].rearrange("s t c -> (s t) c")
    K.dma("sp", O["cbs"][:, 0:22, :], I["scc"][:, 8:30, :], final=True)
    ext_p = K.alloc("ext_p", [128, 30 + 1024])
    ext_s = K.alloc("ext_s", [128, 16, 38])
    exs = K.alloc("exs", [128, 512])
    glu = K.alloc("glu", [128, NCT])
    sg = K.alloc("sg", [128, 512])
    cacc = K.alloc("cacc", [128, 1152])
    cb_sb = K.alloc("cb_sb", [128, 2, 128])
    wv = [K.alloc(f"wv{i}", [128, 16, 128], BF16) for i in range(1)]
    wgt = [K.alloc(f"wgt{i}", [128, 16, 128], BF16) for i in range(1)]
    print("CONF arena top:", K.top)
    blocks = [(0, 512), (512, 512), (1024, 160)]
    for c in range(16):
        wv_, wg_ = wv[0], wgt[0]
        for r in range(4):
            nr = 128 if r < 3 else 96
            K.dma("sp", st[0:nr, r, :], scc2[r * 128:r * 128 + nr, c * 128:(c + 1) * 128])
        K.dma("pool", wv_, w_in[:, c * 128:(c + 1) * 128].rearrange("(k p) n -> p k n", p=128))
        K.dma("pool", wg_, w_in[:, 2048 + c * 128:2048 + (c + 1) * 128].rearrange("(k p) n -> p k n", p=128))
        pst = K.bank(7, F32, [128, 512])
        for r in range(4):
            pe.transpose(out=pst[:, r * 128:(r + 1) * 128], in_=st[:, r, :], identity=C["ident"])
        act.activation(out=exs, in_=pst, func=AF.Copy)
        dve.tensor_copy(out=ext_s[:, :, 0:30], in_=exs[:, 0:480].re("p (s t) -> p s t", t=30))
        for bi, (c0, nb) in enumerate(blocks):
            pv = K.bank(2 * (bi % 2), F32, [128, 512])[:, 0:nb]
            pg = K.bank(2 * (bi % 2) + 1, F32, [128, 512])[:, 0:nb]
            for k in range(16):
                pe.matmul(out=pv, lhsT=wv_[:, k, :], rhs=aT[:, k, c0:c0 + nb], start=(k == 0), stop=(k == 15))
            for k in range(16):
                pe.matmul(out=pg, lhsT=wg_[:, k, :], rhs=aT[:, k, c0:c0 + nb], start=(k == 0), stop=(k == 15))
            act.activation(out=sg[:, 0:nb], in_=pg, func=AF.Sigmoid, bias=b_g[:, c:c + 1])
            dve.scalar_tensor_tensor(out=glu[:, c0:c0 + nb], in0=pv, scalar=b_v[:, c:c + 1], in1=sg[:, 0:nb],
                                     op0=ALU.add, op1=ALU.mult)
        dve.tensor_scalar(out=ext_p[:, 0:30], in0=glu[:, 2:32], scalar1=sel[:, 1:2], scalar2=None, op0=ALU.mult)
        act.activation(out=ext_p[:, 30:1054], in_=glu[:, 32:1056], func=AF.Copy)
        act.activation(out=ext_s[:, :, 30:38], in_=glu[:, 1056:1184].re("p (s t) -> p s t", t=8), func=AF.Copy)
        pcb = K.bank(6, F32, [128, 2, 128])
        pe.transpose(out=pcb[:, 0, :], in_=ext_p[:, 926:1054], identity=C["ident"])
        pe.transpose(out=pcb[:, 1, :], in_=glu[:, 1056:1184], identity=C["ident"])
        act.activation(out=cb_sb, in_=pcb, func=AF.Copy)
        K.dma("sp", O["cbp"][:, c * 128:(c + 1) * 128], cb_sb[98:128, 0, :], final=True)
        for s_ in range(16):
            K.dma("sp", O["cbs"][s_, 22:30, c * 128:(c + 1) * 128], cb_sb[8 * s_:8 * s_ + 8, 1, :], final=True)
        cp = cacc[:, 0:1024]
        cs = cacc[:, 1024:1152].re("p (s t) -> p s t", t=8)
        dve.tensor_scalar(out=cp, in0=ext_p[:, 0:1024], scalar1=dww[:, c, 0:1], scalar2=dwb[:, c:c + 1],
                          op0=ALU.mult, op1=ALU.add)
        dve.tensor_scalar(out=cs, in0=ext_s[:, :, 0:8], scalar1=dww[:, c, 0:1], scalar2=dwb[:, c:c + 1],
                          op0=ALU.mult, op1=ALU.add)
        for j in range(1, 31):
            dve.scalar_tensor_tensor(out=cp, in0=ext_p[:, j:j + 1024], scalar=dww[:, c, j:j + 1], in1=cp,
                                     op0=ALU.mult, op1=ALU.add)
            dve.scalar_tensor_tensor(out=cs, in0=ext_s[:, :, j:j + 8], scalar=dww[:, c, j:j + 1], in1=cs,
                                     op0=ALU.mult, op1=ALU.add)
        act.activation(out=cbf[:, c, :], in_=cacc, func=AF.Copy)
    K.release(mB)
    mC = K.mark()
    lng = K.alloc("lng", [128, 16])
    lnb = K.alloc("lnb", [128, 16])
    K.dma("sp", lng, I["conf_ln_g"][0].rearrange("(c p) -> p c", p=128), allow_slow_non_contiguous=True)
    K.dma("sp", lnb, I["conf_ln_b"][0].rearrange("(c p) -> p c", p=128), allow_slow_non_contiguous=True)
    cf = K.alloc("cf", [128, 512])
    c2 = K.alloc("c2", [128, 512])
    mu = K.alloc("mu", [128, 512])
    rstd = K.alloc("rstd", [128, 512])
    tmpn = K.alloc("tmpn", [128, 512])
    for (c0, nb) in [(0, 512), (512, 512), (1024, 128)]:
        psum_s = K.bank(0, F32, [128, 512])[:, 0:nb]
        psum_q = K.bank(1, F32, [128, 512])[:, 0:nb]
        for c in range(16):
            act.activation(out=cf[:, 0:nb], in_=cbf[:, c, c0:c0 + nb], func=AF.Copy)
            act.activation(out=c2[:, 0:nb], in_=cbf[:, c, c0:c0 + nb], func=AF.Square)
            pe.matmul(out=psum_s, lhsT=C["ones"], rhs=cf[:, 0:nb], start=(c == 0), stop=(c == 15))
            pe.matmul(out=psum_q, lhsT=C["ones"], rhs=c2[:, 0:nb], start=(c == 0), stop=(c == 15))
        act.activation(out=mu[:, 0:nb], in_=psum_s, func=AF.Copy, scale=1.0 / D)
        dve.tensor_tensor(out=tmpn[:, 0:nb], in0=mu[:, 0:nb], in1=mu[:, 0:nb], op=ALU.mult)
        dve.scalar_tensor_tensor(out=rstd[:, 0:nb], in0=psum_q, scalar=1.0 / D, in1=tmpn[:, 0:nb],
                                 op0=ALU.mult, op1=ALU.subtract)
        act.activation(out=rstd[:, 0:nb], in_=rstd[:, 0:nb], func=AF.Sqrt, bias=EPS)
        dve.reciprocal(out=rstd[:, 0:nb], in_=rstd[:, 0:nb])
        for c in range(16):
            dve.tensor_tensor(out=tmpn[:, 0:nb], in0=cbf[:, c, c0:c0 + nb], in1=mu[:, 0:nb], op=ALU.subtract)
            dve.tensor_tensor(out=tmpn[:, 0:nb], in0=tmpn[:, 0:nb], in1=rstd[:, 0:nb], op=ALU.mult)
            act.activation(out=cbf[:, c, c0:c0 + nb], in_=tmpn[:, 0:nb], func=AF.Silu,
                           scale=lng[:, c:c + 1], bias=lnb[:, c:c + 1])
    bo = K.alloc("bo", [128, D])
    K.dma("sp", bo, I["conf_b_out"][0].partition_broadcast(128))
    wo = [K.alloc(f"wo{i}", [128, 16, 512], BF16) for i in range(2)]
    ysb = K.alloc("ysb", [128, 512])
    n = 0
    for blk in range(4):
        w = wo[blk % 2]
        K.dma("pool", w, I["conf_w_out"][:, blk * 512:(blk + 1) * 512].rearrange("(k p) n -> p k n", p=128))
        for t in range(1, 10):
            ps = K.bank(2 + n % 6, F32, [128, 512])
            n += 1
            for c in range(16):
                pe.matmul(out=ps, lhsT=cbf[:, c, (t - 1) * 128:t * 128], rhs=w[:, c, :], start=(c == 0), stop=(c == 15))
            dve.tensor_tensor(out=ysb, in0=ps, in1=bo[:, blk * 512:(blk + 1) * 512], op=ALU.add)
            hs = H[t][:, blk * 512:(blk + 1) * 512]
            dve.tensor_tensor(out=hs, in0=hs, in1=ysb, op=ALU.add)
    K.release(mA)


def stage_final(K, C, I, O, H):
    m = K.mark()
    gf = K.alloc("gf", [128, D])
    K.dma("sp", gf, I["final_norm"][0].partition_broadcast(128))
    yo = [K.alloc(f"yo{i}", [128, D]) for i in range(2)]
    sq = K.alloc("sqf", [128, D])
    ss = K.alloc("ssf", [128, 1])
    for t in range(1, 10):
        y = yo[t % 2]
        rmsnorm_tok(K, H[t], gf, y, sq, ss)
        if t < 9:
            K.dma("sp", O["yp"][(t - 1) * 128:t * 128, :], y, final=True)
        else:
            K.dma("sp", O["ys"][:, :], y, final=True)
    K.release(m)

W_SHAPES = {
    "norm_mix": (2, D), "norm_ffn": (2, D), "norm_ple": (2, D), "final_norm": (1, D),
    "gdn_w_in": (D, 12352), "gdn_conv_w": (4, 8192), "gdn_a_log": (1, 32), "gdn_dt_bias": (1, 32),
    "gdn_o_norm": (1, 128), "gdn_w_out": (4096, D),
    "conf_w_in": (D, 4096), "conf_b_in": (1, 4096), "conf_dw_w": (31, D), "conf_dw_b": (1, D),
    "conf_ln_g": (1, D), "conf_ln_b": (1, D), "conf_w_out": (D, D), "conf_b_out": (1, D),
    "peer_w_q": (2, D, D), "peer_keys1": (2, 128, 128), "peer_keys2": (2, 128, 128),
    "peer_u": (2 * 16384, D), "peer_v": (2 * 16384, D),
    "ple_w_proj": (2, 256, D), "ple_w_gate": (2, D, D), "ple_b_gate": (2, D),
}
ACT_SHAPES = {
    "xp": (2048, D), "xo": (1056, D), "pp": (2, 1056, 256), "xs": (128, D), "psm": (2, 128, 256),
    "sel": (128, 4), "sgr": (16, 32, 128, 128), "sgc": (16, 3, 8192), "scc": (16, 30, D),
}
OUT_SHAPES = {
    "yp": (1024, D), "ys": (128, D), "gSp": (32, 128, 128), "gSs": (16, 32, 128, 128),
    "gbp": (3, 8192), "gbs": (16, 3, 8192), "cbp": (30, D), "cbs": (16, 30, D),
}
STAGE_INPUTS = {
    "gdn": ["xp", "xs", "sel", "sgr", "sgc", "norm_mix", "gdn_w_in", "gdn_conv_w", "gdn_a_log", "gdn_dt_bias",
            "gdn_o_norm"],
    "oproj": ["xo", "xs", "gdn_w_out"],
    "peer0": ["norm_ffn", "peer_w_q", "peer_keys1", "peer_keys2", "peer_u", "peer_v"],
    "ple0": ["norm_ple", "ple_w_proj", "ple_w_gate", "ple_b_gate", "pp", "psm"],
    "conf": ["norm_mix", "sel", "scc", "conf_w_in", "conf_b_in", "conf_dw_w", "conf_dw_b", "conf_ln_g", "conf_ln_b",
             "conf_w_out", "conf_b_out"],
    "peer1": ["norm_ffn", "peer_w_q", "peer_keys1", "peer_keys2", "peer_u", "peer_v"],
    "ple1": ["norm_ple", "ple_w_proj", "ple_w_gate", "ple_b_gate", "pp", "psm"],
    "final": ["final_norm"],
}
STAGE_OUTPUTS = {"gdn": ["gSp", "gSs", "gbp", "gbs"], "conf": ["cbp", "cbs"], "final": ["yp", "ys"]}
ALL_STAGES = ("gdn", "oproj", "peer0", "ple0", "conf", "peer1", "ple1", "final")


def build(stages=("gdn",)):
    K = Kern()
    need_in, need_out = [], []
    for s in stages:
        need_in += [n for n in STAGE_INPUTS[s] if n not in need_in]
        need_out += [n for n in STAGE_OUTPUTS.get(s, []) if n not in need_out]
    I = {n: K.din(n, ACT_SHAPES.get(n) or W_SHAPES[n]) for n in need_in}
    O = {n: K.dout(n, OUT_SHAPES[n]) for n in need_out}
    C = build_consts(K)
    osel_scr = K.dscr("osel_scr", [32, 128, NOWN], BF16)
    TB, conv_chunks = None, []
    if "peer0" in stages or "peer1" in stages:
        scr = K.nc.dram_tensor("peer_uvb", [2 * 16384, 2 * D], BF16, kind="Internal").ap()
        TB = (scr, {0: [], 1: []})
        for layer in range(2):
            for ci_, nm in enumerate(("u", "v")):
                for r0 in range(layer * 16384, (layer + 1) * 16384, 1024):
                    b_ = Buf(f"{nm}b{r0}")
                    TB[1][layer].append(b_)
                    conv_chunks.append((V(scr[r0:r0 + 1024, ci_ * D:(ci_ + 1) * D], b_),
                                        I[f"peer_{nm}"][r0:r0 + 1024, :]))
    base = K.mark()
    if "gdn" in stages:
        stage_gdn(K, C, I, O, osel_scr, conv_chunks)
        K.release(base)
    for dst, src in conv_chunks:
        K.dma("pool", dst, src, bulk=True)
    H = [K.alloc(f"H{t}", [128, D]) for t in range(10)]
    if "oproj" in stages:
        stage_oproj(K, C, I, H, osel_scr)
    if "peer0" in stages:
        stage_peer(K, C, I, H, 0, list(range(10)), TB)
    if "ple0" in stages:
        stage_ple(K, C, I, H, 0, list(range(10)))
    if "conf" in stages:
        stage_conf(K, C, I, O, H)
    if "peer1" in stages:
        stage_peer(K, C, I, H, 1, list(range(1, 10)), TB)
    if "ple1" in stages:
        stage_ple(K, C, I, H, 1, list(range(1, 10)))
    if "final" in stages:
        stage_final(K, C, I, O, H)
    K.S.emit()
    K.es.close()
    return K


def prep_inputs(inp, names):
    f = lambda a: np.ascontiguousarray(a, dtype=np.float32)
    shared = {}
    for n, shp in W_SHAPES.items():
        if n in names:
            shared[n] = f(np.asarray(inp[n]).reshape(shp))
    maps = []
    for c in range(8):
        b, half = c // 2, c % 2
        m = dict(shared)
        xp = np.asarray(inp["x_prompt"][b])
        if "xp" in names:
            m["xp"] = f(xp)
        lo = half * 1024 - 32
        if "xo" in names:
            xo = np.zeros((1056, D), np.float32)
            if lo >= 0:
                xo[:] = xp[lo:lo + 1056]
            else:
                xo[32:] = xp[0:1024]
            m["xo"] = xo
        if "pp" in names:
            pp = np.zeros((2, 1056, 256), np.float32)
            pr = np.asarray(inp["p_prompt"][:, b])
            if lo >= 0:
                pp[:] = pr[:, lo:lo + 1056]
            else:
                pp[:, 32:] = pr[:, 0:1024]
            m["pp"] = pp
        if "xs" in names:
            m["xs"] = f(np.asarray(inp["x_sample"][16 * c:16 * c + 16]).reshape(128, D))
        if "psm" in names:
            m["psm"] = f(np.asarray(inp["p_sample"][:, 16 * c:16 * c + 16]).reshape(2, 128, 256))
        if "sel" in names:
            s = np.zeros((128, 4), np.float32)
            s[:, 0] = 1.0 - half
            s[:, 1] = float(half)
            m["sel"] = s
        if "sgr" in names:
            m["sgr"] = f(inp["state_gdn_recurrent"][0, 16 * c:16 * c + 16])
        if "sgc" in names:
            m["sgc"] = f(inp["state_gdn_conv"][0, 16 * c:16 * c + 16])
        if "scc" in names:
            m["scc"] = f(inp["state_conf_conv"][0, 16 * c:16 * c + 16])
        maps.append(m)
    return maps


_PROG = {}


def _program():
    if "K" not in _PROG:
        _PROG["K"] = build(ALL_STAGES)
    return _PROG["K"]


def assemble(results, n_cores=8):
    nb = n_cores // 2
    y_p = np.zeros((4, 2048, D), np.float32)
    y_s = np.zeros((128, 8, D), np.float32)
    gS_p = np.zeros((1, 4, 32, 128, 128), np.float32)
    gS_s = np.zeros((1, 128, 32, 128, 128), np.float32)
    gb_p = np.zeros((1, 4, 3, 8192), np.float32)
    gb_s = np.zeros((1, 128, 3, 8192), np.float32)
    cb_p = np.zeros((1, 4, 30, D), np.float32)
    cb_s = np.zeros((1, 128, 30, D), np.float32)
    for c in range(n_cores):
        r = results[c]
        b, half = c // 2, c % 2
        y_p[b, half * 1024:(half + 1) * 1024] = r["yp"]
        y_s[16 * c:16 * c + 16] = r["ys"].reshape(16, 8, D)
        gS_s[0, 16 * c:16 * c + 16] = r["gSs"]
        gb_s[0, 16 * c:16 * c + 16] = r["gbs"]
        cb_s[0, 16 * c:16 * c + 16] = r["cbs"]
        if half == 0:
            gS_p[0, b] = r["gSp"]
            gb_p[0, b] = r["gbp"]
        else:
            cb_p[0, b] = r["cbp"]
    return (y_p, y_s, gS_p, gS_s, gb_p, gb_s, cb_p, cb_s)


def kernel(**inputs):
    K = _program()
    maps = prep_inputs(inputs, list(K.ins.keys()))
    res = run_bass_kernel_spmd(K.nc, maps, core_ids=list(range(8)))
    return assemble(res.results, 8)
```

```python
import contextlib
import os
import numpy as np
import concourse.bass as bass
import concourse.mybir as mybir
from concourse.bass_utils import run_bass_kernel_spmd

F32 = mybir.dt.float32
BF16 = mybir.dt.bfloat16
U32 = mybir.dt.uint32
I32 = mybir.dt.int32
ALU = mybir.AluOpType
AF = mybir.ActivationFunctionType
AX = mybir.AxisListType

ENGS = ("pe", "dve", "act", "pool", "sp")
GEN = 30000
N_DMA_SEMS = 24
SEM_POOLS = {"sp": (0, 12), "pool": (12, 8), "bulk": (20, 4)}
EPS = 1e-6

D = 2048
NTP = 2048
NT = 2176
NCH = 17
NOWN = 1184
BIG = 30000.0


class Buf:
    __slots__ = ("name", "last_w", "readers", "psum")

    def __init__(self, name, psum=False):
        self.name = name
        self.last_w = None
        self.readers = []
        self.psum = psum


class V:
    __slots__ = ("ap", "b")

    def __init__(self, ap, b):
        self.ap = ap
        self.b = b

    def __getitem__(self, k):
        return V(self.ap[k], self.b)

    def bc(self, shape):
        return V(self.ap.to_broadcast(list(shape)), self.b)

    def un(self, axis):
        return V(self.ap.unsqueeze(axis), self.b)

    def re(self, pattern, **kw):
        return V(self.ap.rearrange(pattern, **kw), self.b)

    def cast(self, dt):
        return V(self.ap.bitcast(dt), self.b)

    @property
    def shape(self):
        return self.ap.shape


WRITE_KEYS = ("out", "accum_out", "ap")


class Sched:
    def __init__(self, nc):
        self.nc = nc
        self.ops = {e: [] for e in ENGS}
        self.cnt = {e: 0 for e in ENGS}
        self.seen = {e: {} for e in ENGS}
        self.dma_cnt = [0] * N_DMA_SEMS
        self.dma_rr = {k: 0 for k in SEM_POOLS}
        self.final_tokens = []
        self.n_ops = 0

    def _need(self, eng, tok):
        if tok is None:
            return
        kind, key, val = tok
        if kind == "e":
            if key == eng and eng == "pe":
                return
            g = (val - 1) // GEN
            skey = ("e", key, g)
            v = val - g * GEN
            for gg in range(g + 1, g + 6):
                if self.seen[eng].get(("e", key, gg), 0) > 0:
                    return
        else:
            skey = ("d", key)
            v = val
        if self.seen[eng].get(skey, 0) >= v:
            return
        self.seen[eng][skey] = v
        self.ops[eng].append(("wait", skey, v))

    @staticmethod
    def _compact(toks):
        best = {}
        for t in toks:
            k = (t[0], t[1])
            if k not in best or best[k][2] < t[2]:
                best[k] = t
        return list(best.values())

    def _deps(self, eng, reads, writes):
        for b in reads:
            self._need(eng, b.last_w)
            if b.psum:
                for t in b.readers:
                    if t[0] == "e" and t[1] != eng:
                        self._need(eng, t)
        for b in writes:
            self._need(eng, b.last_w)
            for t in b.readers:
                self._need(eng, t)

    def _commit(self, tok, reads, writes):
        for b in reads:
            b.readers.append(tok)
            if len(b.readers) > 16:
                b.readers = self._compact(b.readers)
        for b in writes:
            b.last_w = tok
            b.readers = []
        self.n_ops += 1

    def op(self, eng, fn, reads=(), writes=()):
        self._deps(eng, reads, writes)
        self.cnt[eng] += 1
        tok = ("e", eng, self.cnt[eng])
        self.ops[eng].append(("op", fn, self.cnt[eng]))
        self._commit(tok, reads, writes)
        return tok

    def dma(self, eng, fn, reads=(), writes=(), final=False, bulk=False):
        self._deps(eng, reads, writes)
        pk = "bulk" if bulk else eng
        base_, n_ = SEM_POOLS[pk]
        s = base_ + self.dma_rr[pk]
        self.dma_rr[pk] = (self.dma_rr[pk] + 1) % n_
        if self.dma_cnt[s] > 0:
            self._need(eng, ("d", s, self.dma_cnt[s]))
        self.dma_cnt[s] += 16
        tok = ("d", s, self.dma_cnt[s])
        self.ops[eng].append(("dma", fn, s))
        self._commit(tok, reads, writes)
        if final:
            self.final_tokens.append(tok)
        return tok

    def fence(self):
        toks = [("e", e, self.cnt[e]) for e in ENGS if self.cnt[e] > 0]
        toks += [("d", s, self.dma_cnt[s]) for s in range(N_DMA_SEMS) if self.dma_cnt[s] > 0]
        for e in ENGS:
            for t in toks:
                if not (t[0] == "e" and t[1] == e):
                    self._need(e, t)

    def emit(self):
        nc = self.nc
        for t in self.final_tokens:
            self._need("sp", t)
        with contextlib.ExitStack() as es:
            esem = {}
            for e in ENGS:
                for g in range(max(1, (self.cnt[e] + GEN - 1) // GEN)):
                    esem[(e, g)] = es.enter_context(nc.semaphore(f"p_{e}{g}"))
            dsem = [es.enter_context(nc.semaphore(f"d{i}")) for i in range(N_DMA_SEMS)]
            block = es.enter_context(nc.Block())

            def run(eng_name, engine):
                for item in self.ops[eng_name]:
                    if item[0] == "wait":
                        skey, v = item[1], item[2]
                        if skey[0] == "e":
                            engine.wait_ge(esem[(skey[1], skey[2])], v)
                        else:
                            engine.wait_ge(dsem[skey[1]], v)
                    elif item[0] == "op":
                        ins = item[1](engine)
                        ins.then_inc(esem[(eng_name, (item[2] - 1) // GEN)], 1)
                    else:
                        ins = item[1](engine)
                        ins.then_inc(dsem[item[2]], 16)

            @block.tensor
            def _(eng):
                run("pe", eng)

            @block.vector
            def _(eng):
                run("dve", eng)

            @block.scalar
            def _(eng):
                run("act", eng)

            @block.gpsimd
            def _(eng):
                run("pool", eng)

            @block.sync
            def _(eng):
                run("sp", eng)


class EngP:
    def __init__(self, K, name):
        self.K = K
        self.name = name

    def __getattr__(self, meth):
        K, name = self.K, self.name

        def call(**kw):
            reads, writes = [], []
            for key, val in kw.items():
                if isinstance(val, V):
                    (writes if key in WRITE_KEYS else reads).append(val.b)

            def fn(e):
                args = {k: (v.ap if isinstance(v, V) else v) for k, v in kw.items()}
                return getattr(e, meth)(**args)

            return K.S.op(name, fn, reads, writes)

        return call


class Kern:
    ARENA_F32 = 53000

    def __init__(self):
        self.nc = bass.Bass("TRN2", target_bir_lowering=False)
        self.S = Sched(self.nc)
        self.es = contextlib.ExitStack()
        self.pe = EngP(self, "pe")
        self.dve = EngP(self, "dve")
        self.act = EngP(self, "act")
        self.pool = EngP(self, "pool")
        self.arena = self.es.enter_context(self.nc.sbuf_tensor("arena", [128, self.ARENA_F32], F32))
        self.top = 0
        self.banks = [V(self.es.enter_context(self.nc.psum_tensor(f"bank{i}", [128, 512], F32))[:, :],
                        Buf(f"bank{i}", psum=True)) for i in range(8)]
        self.ins = {}
        self.outs = {}

    def din(self, name, shape, dt=F32):
        ap = self.nc.dram_tensor(name, list(shape), dt, kind="ExternalInput").ap()
        self.ins[name] = (tuple(shape), dt)
        return ap

    def dout(self, name, shape, dt=F32):
        ap = self.nc.dram_tensor(name, list(shape), dt, kind="ExternalOutput").ap()
        self.outs[name] = (tuple(shape), dt)
        return ap

    def dscr(self, name, shape, dt):
        ap = self.nc.dram_tensor(name, list(shape), dt, kind="Internal").ap()
        return V(ap, Buf(name))

    def alloc(self, name, shape, dt=F32):
        n = int(np.prod(shape[1:]))
        words = n if dt in (F32, U32, I32) else (n + 1) // 2
        words = (words + 1) // 2 * 2
        off = self.top
        self.top += words
        assert self.top <= self.ARENA_F32, f"arena overflow at {name}: {self.top}"
        ap = self.arena[0:shape[0], off:off + words]
        if dt != F32:
            ap = ap.bitcast(dt)
        ap = ap[:, 0:n]
        if len(shape) == 3:
            ap = ap.rearrange("p (a b) -> p a b", a=shape[1])
        elif len(shape) == 4:
            ap = ap.rearrange("p (a b c) -> p a b c", a=shape[1], b=shape[2])
        return V(ap, Buf(name))

    def mark(self):
        return self.top

    def release(self, m):
        self.S.fence()
        self.top = m

    def bank(self, i, dt=F32, shape=None):
        v = self.banks[i]
        ap = v.ap
        if dt != F32:
            ap = ap.bitcast(dt)
        if shape is not None:
            n = int(np.prod(shape[1:]))
            ap = ap[0:shape[0], 0:n]
            if len(shape) == 3:
                ap = ap.rearrange("p (a b) -> p a b", a=shape[1])
        return V(ap, v.b)

    def dma(self, q, out, in_, final=False, bulk=False, **kw):
        reads = [in_.b] if isinstance(in_, V) else []
        writes = [out.b] if isinstance(out, V) else []
        o = out.ap if isinstance(out, V) else out
        i = in_.ap if isinstance(in_, V) else in_
        return self.S.dma(q, lambda e: e.dma_start(out=o, in_=i, **kw), reads, writes, final=final, bulk=bulk)

    def gather(self, out, table_ap, idx, extra_reads=()):
        o, ia = out.ap, idx.ap
        return self.S.dma(
            "pool",
            lambda e: e.indirect_dma_start(out=o, out_offset=None, in_=table_ap,
                                           in_offset=bass.IndirectOffsetOnAxis(ap=ia, axis=0)),
            reads=[idx.b] + list(extra_reads), writes=[out.b])


def build_consts(K):
    C = {}
    pool, dve = K.pool, K.dve

    def mask(name, steps, op, base, cm, shape=(128, 128)):
        t = K.alloc(name, list(shape), F32)
        pool.memset(ap=t, constant=1.0)
        pool.affine_select(out=t, in_=t, pattern=steps, compare_op=op, fill=0.0, base=base,
                           channel_multiplier=cm)
        return t

    C["ident"] = mask("ident", [[-1, 128]], ALU.is_equal, 0, 1)
    C["ones"] = K.alloc("ones", [128, 128], F32)
    pool.memset(ap=C["ones"], constant=1.0)
    C["identb"] = K.alloc("identb", [128, 128], BF16)
    dve.tensor_copy(out=C["identb"], in_=C["ident"])
    C["Lst_p"] = mask("Lst_p", [[-1, 128]], ALU.is_gt, 0, 1)
    C["Uin_p"] = mask("Uin_p", [[1, 128]], ALU.is_ge, 0, -1)
    C["LS_p"] = mask("LS_p", [[0, 128]], ALU.is_equal, -127, 1)
    blk = K.alloc("blk", [128, 128], F32)
    pool.memset(ap=blk, constant=1.0)
    pool.affine_select(out=blk, in_=blk, pattern=[[-8, 16], [0, 8]], compare_op=ALU.is_ge, fill=0.0,
                       base=0, channel_multiplier=1)
    pool.affine_select(out=blk, in_=blk, pattern=[[8, 16], [0, 8]], compare_op=ALU.is_ge, fill=0.0,
                       base=7, channel_multiplier=-1)
    C["blk"] = blk
    C["Lst_s"] = K.alloc("Lst_s", [128, 128], F32)
    dve.tensor_tensor(out=C["Lst_s"], in0=C["Lst_p"], in1=blk, op=ALU.mult)
    C["Uin_s"] = K.alloc("Uin_s", [128, 128], F32)
    dve.tensor_tensor(out=C["Uin_s"], in0=C["Uin_p"], in1=blk, op=ALU.mult)
    C["LS_s"] = mask("LS_s", [[-8, 16], [0, 8]], ALU.is_equal, -7, 1)
    rm = K.alloc("rowmask", [128, 16], F32)
    pool.memset(ap=rm, constant=1.0)
    pool.affine_select(out=rm, in_=rm, pattern=[[-8, 16]], compare_op=ALU.is_ge, fill=0.0, base=0,
                       channel_multiplier=1)
    pool.affine_select(out=rm, in_=rm, pattern=[[8, 16]], compare_op=ALU.is_ge, fill=0.0, base=7,
                       channel_multiplier=-1)
    C["rowmask"] = rm
    C["lastmask"] = mask("lastmask", [[-8, 16]], ALU.is_equal, -7, 1, shape=(128, 16))
    return C


def rmsnorm_tok(K, x, g, out, sq, ss, n=D):
    K.act.activation(out=sq, in_=x, func=AF.Square, accum_out=ss)
    K.act.activation(out=ss, in_=ss, func=AF.Sqrt, scale=1.0 / n, bias=EPS)
    K.dve.reciprocal(out=ss, in_=ss)
    K.dve.scalar_tensor_tensor(out=out, in0=x, scalar=ss, in1=g, op0=ALU.mult, op1=ALU.mult)


_flip = [0]


def evac(K, out, in_):
    _flip[0] ^= 1
    if _flip[0]:
        K.act.activation(out=out, in_=in_, func=AF.Copy)
    else:
        K.dve.tensor_copy(out=out, in_=in_)


def transpose_chunks(K, C, src, dst, nk, banks=(2, 3), npart=128):
    for g0 in range(0, nk, 4):
        n = min(4, nk - g0)
        pb = K.bank(banks[(g0 // 4) % 2], BF16, [128, 4, 128])
        for j in range(n):
            K.pe.transpose(out=pb[:, j, 0:npart], in_=src[0:npart, (g0 + j) * 128:(g0 + j + 1) * 128],
                           identity=C["identb"][0:npart, 0:npart])
        evac(K, dst[:, g0:g0 + n, 0:npart], pb[:, 0:n, 0:npart])


def stage_gdn(K, C, I, O, osel_scr, conv_chunks=None):
    dve, act, pe, pool = K.dve, K.act, K.pe, K.pool
    aT_scr = K.dscr("aT_scr", [128, 16, NT], BF16)
    w_in = I["gdn_w_in"]

    m0 = K.mark()
    gmix = K.alloc("gmix", [128, D])
    K.dma("sp", gmix, I["norm_mix"][0].partition_broadcast(128))
    xts = [K.alloc(f"xt{i}", [128, D]) for i in range(2)]
    xns = [K.alloc(f"xn{i}", [128, D], BF16) for i in range(2)]
    xTs = [K.alloc(f"xT{i}", [128, 16, 128], BF16) for i in range(2)]
    sq = K.alloc("sq", [128, D])
    sss = [K.alloc(f"ss{i}", [128, 1]) for i in range(2)]
    for i in range(NCH):
        xt, xn, xT, ss = xts[i % 2], xns[i % 2], xTs[i % 2], sss[i % 2]
        src = I["xp"][i * 128:(i + 1) * 128, :] if i < 16 else I["xs"][:, :]
        K.dma("sp", xt, src)
        rmsnorm_tok(K, xt, gmix, xn, sq, ss)
        transpose_chunks(K, C, xn, xT, 16)
        K.dma("sp", aT_scr[:, :, i * 128:(i + 1) * 128], xT)
    K.release(m0)
    STOP = os.environ.get("GDN_STOP", "")
    NHQ = int(os.environ.get("GDN_NHQ", "16"))
    if STOP == "g0":
        return

    P = {}
    for nm in ("negbeta", "gc", "kdec", "bge", "decS"):
        P[nm] = K.alloc(nm, [128, NCH, 32])
    P["decSs"] = K.alloc("decSs", [128, 32, 16])
    cw = K.alloc("cw", [128, 64, 4])
    for j in range(4):
        K.dma("sp", cw[:, :, j], I["gdn_conv_w"][j].rearrange("(c p) -> p c", p=128), allow_slow_non_contiguous=True)
    onorm = K.alloc("onorm", [128, 1])
    K.dma("sp", onorm, I["gdn_o_norm"].rearrange("o p -> p o"), allow_slow_non_contiguous=True)
    sel = K.alloc("sel", [128, 4])
    K.dma("sp", sel, I["sel"][:, :])

    m1 = K.mark()
    wbd = K.alloc("wbd", [128, 16, 64], BF16)
    K.dma("pool", wbd, w_in[:, 12288:12352].rearrange("(k p) n -> p k n", p=128))
    bd = K.alloc("bd", [128, NCH, 64])
    ab = K.alloc("aTbd", [128, 16, 1024], BF16)
    for c0 in range(0, NCH, 8):
        nchk = min(8, NCH - c0)
        K.dma("sp", ab[:, :, 0:nchk * 128], aT_scr[:, :, c0 * 128:(c0 + nchk) * 128])
        pb = K.bank(0, F32, [128, 8, 64])
        for j in range(nchk):
            for k in range(16):
                pe.matmul(out=pb[:, j, :], lhsT=ab[:, k, j * 128:(j + 1) * 128], rhs=wbd[:, k, :],
                          start=(k == 0), stop=(k == 15))
        evac(K, bd[:, c0:c0 + nchk, :], pb[:, 0:nchk, :])
    alog = K.alloc("alog", [128, 32])
    dtb = K.alloc("dtb", [128, 32])
    K.dma("sp", alog, I["gdn_a_log"][0].partition_broadcast(128))
    K.dma("sp", dtb, I["gdn_dt_bias"][0].partition_broadcast(128))
    act.activation(out=alog, in_=alog, func=AF.Exp)
    dve.tensor_scalar(out=alog, in0=alog, scalar1=-1.0, scalar2=None, op0=ALU.mult)
    beta = K.alloc("beta", [128, NCH, 32])
    act.activation(out=beta, in_=bd[:, :, 0:32], func=AF.Sigmoid)
    dve.tensor_scalar(out=P["negbeta"], in0=beta, scalar1=-1.0, scalar2=None, op0=ALU.mult)
    g = K.alloc("g", [128, NCH, 32])
    dve.tensor_tensor(out=g, in0=bd[:, :, 32:64], in1=dtb.un(1).bc([128, NCH, 32]), op=ALU.add)
    act.activation(out=g, in_=g, func=AF.Exp)
    act.activation(out=g, in_=g, func=AF.Ln, bias=1.0)
    dve.tensor_tensor(out=g, in0=g, in1=alog.un(1).bc([128, NCH, 32]), op=ALU.mult)
    pg = K.bank(1, F32, [128, 16, 32])
    pe.matmul(out=pg, lhsT=C["Uin_p"], rhs=g[:, 0:16, :], start=True, stop=True)
    evac(K, P["gc"][:, 0:16, :], pg)
    pg2 = K.bank(0, F32, [128, 1, 32])
    pe.matmul(out=pg2, lhsT=C["Uin_s"], rhs=g[:, 16:17, :], start=True, stop=True)
    evac(K, P["gc"][:, 16:17, :], pg2)
    gl = K.alloc("gl", [128, NCH, 32])
    pl = K.bank(1, F32, [128, 16, 32])
    pe.matmul(out=pl, lhsT=C["LS_p"], rhs=P["gc"][:, 0:16, :], start=True, stop=True)
    evac(K, gl[:, 0:16, :], pl)
    pl2 = K.bank(0, F32, [128, 1, 32])
    pe.matmul(out=pl2, lhsT=C["LS_s"], rhs=P["gc"][:, 16:17, :], start=True, stop=True)
    evac(K, gl[:, 16:17, :], pl2)
    act.activation(out=P["decS"], in_=gl, func=AF.Exp)
    dve.tensor_tensor(out=gl, in0=gl, in1=P["gc"], op=ALU.subtract)
    act.activation(out=P["kdec"], in_=gl, func=AF.Exp)
    eg = K.alloc("eg", [128, NCH, 32])
    act.activation(out=eg, in_=P["gc"], func=AF.Exp)
    dve.tensor_tensor(out=P["bge"], in0=eg, in1=beta, op=ALU.mult)
    lm = K.alloc("lm", [128, 32, 16])
    dve.tensor_tensor(out=lm, in0=P["gc"][:, 16, :].un(2).bc([128, 32, 16]),
                      in1=C["lastmask"].un(1).bc([128, 32, 16]), op=ALU.mult)
    pls = K.bank(1, F32, [128, 32, 16])
    pe.matmul(out=pls, lhsT=C["ones"], rhs=lm, start=True, stop=True)
    act.activation(out=P["decSs"], in_=pls, func=AF.Exp)
    K.release(m1)
    if STOP == "g1":
        return

    gbp_sb = K.alloc("gbp_sb", [128, 64, 3])
    aTb = [K.alloc(f"aTb{i}", [128, 16, 256], BF16) for i in range(2)]
    w6 = [K.alloc(f"w6_{i}", [128, 16, 128], BF16) for i in range(6)]
    raw = [K.alloc(f"raw{i}", [128, 2051 + 176], BF16) for i in range(4)]
    cv = K.alloc("cv", [128, NT])
    tmp1 = K.alloc("tmp1", [128, NT])
    rinv = K.alloc("rinv", [128, 512])
    qT = K.alloc("qT", [128, NT], BF16)
    kT = K.alloc("kT", [128, NT], BF16)
    zs = [K.alloc(f"zs{i}", [128, NT], BF16) for i in range(2)]
    vTb = tmp1.cast(BF16)[:, 0:NT]
    k_tok = K.alloc("k_tok", [128, NCH, 128], BF16)
    v_tok = [K.alloc(f"v_tok{i}", [128, NCH, 128], BF16) for i in range(2)]
    oTr = [K.alloc(f"oTr{i}", [128, NT], BF16) for i in range(2)]
    osel = K.alloc("osel", [128, NOWN], BF16)
    st48 = K.alloc("st48", [48, 4, 128])
    smp = K.alloc("smp", [128, 128])
    gb48 = K.alloc("gb48", [128, 128])
    dve.memset(ap=gb48, constant=0.0)
    gbs_sb = K.alloc("gbs_sb", [48, 128])
    Gs = K.alloc("Gs", [128, 4, 128])
    Atm = K.alloc("Atm", [128, 4, 128])
    dg = K.alloc("dg", [128, 4, 128])
    t1 = K.alloc("t1", [128, 4, 128])
    Dm = K.alloc("Dm", [128, 4, 128])
    Egm = K.alloc("Egm", [128, 4, 128])
    N0s = [K.alloc(f"N0_{i}", [128, 8, 128]) for i in range(2)]
    Pn = [K.alloc(f"Pn{i}", [128, 4, 128]) for i in range(2)]
    Qn = [K.alloc(f"Qn{i}", [128, 4, 128]) for i in range(2)]
    Xn = [K.alloc(f"Xn{i}", [128, 4, 128]) for i in range(2)]
    TmT = K.alloc("TmT", [128, 8, 128], BF16)
    vbs = [K.alloc(f"vb{i}", [128, 8, 128], BF16) for i in range(2)]
    kbgs = [K.alloc(f"kbg{i}", [128, 8, 128], BF16) for i in range(2)]
    CH = []
    for i in range(2):
        CH.append(dict(wT=K.alloc(f"wT{i}", [128, 8, 128], BF16), u=K.alloc(f"u{i}", [128, 8, 128], BF16),
                       qg=K.alloc(f"qg{i}", [128, 8, 128], BF16), A=K.alloc(f"A{i}", [128, 8, 128], BF16),
                       kd=K.alloc(f"kd{i}", [128, 8, 128], BF16)))
    Sf = [K.alloc(f"Sf{i}", [128, 128]) for i in range(2)]
    Sb = [K.alloc(f"Sb{i}", [128, 128], BF16) for i in range(2)]
    vnew = [K.alloc(f"vnew{i}", [128, 128], BF16) for i in range(2)]
    print("GDN arena top (f32 words):", K.top)

    def colsel(hq):
        return [hq * 128, 2048 + hq * 128, 4096 + (2 * hq) * 128, 4096 + (2 * hq + 1) * 128,
                8192 + (2 * hq) * 128, 8192 + (2 * hq + 1) * 128]

    blocks = [(i * 256, 256) for i in range(8)] + [(2048, 128)]
    xdbg = None

    def neumann(nchain, nsteps, N0, hook=None):
        pT = K.bank(3, F32, [128, 4, 128])
        pA = K.bank(4, F32, [128, 4, 128])
        pB = K.bank(5, F32, [128, 4, 128])
        pC = K.bank(6, F32, [128, 4, 128])
        for h0 in range(0, nchain, 4):
            n = min(4, nchain - h0)
            sl = slice(h0, h0 + n)
            for j in range(n):
                pe.transpose(out=pT[:, j, :], in_=N0[:, h0 + j, :], identity=C["ident"])
            act.activation(out=Qn[0][:, 0:n, :], in_=pT[:, 0:n, :], func=AF.Copy)
            dve.tensor_tensor(out=Xn[1][:, 0:n, :], in0=pT[:, 0:n, :],
                              in1=C["ident"].un(1).bc([128, n, 128]), op=ALU.add)
            for k in range(1, nsteps):
                a, b = (k - 1) % 2, k % 2
                last = (k == nsteps - 1)
                Pa = (lambda j: N0[:, h0 + j, :]) if k == 1 else (lambda j, a=a: Pn[a][:, j, :])
                for j in range(n):
                    pe.matmul(out=pA[:, j, :], lhsT=Qn[a][:, j, :], rhs=Pa(j), start=True, stop=True)
                if not last:
                    for j in range(n):
                        pe.matmul(out=pB[:, j, :], lhsT=Pa(j), rhs=Qn[a][:, j, :], start=True, stop=True)
                act.activation(out=Pn[b][:, 0:n, :], in_=pA[:, 0:n, :], func=AF.Copy)
                if not last:
                    dve.tensor_copy(out=Qn[b][:, 0:n, :], in_=pB[:, 0:n, :])
                for j in range(n):
                    pe.matmul(out=pC[:, j, :], lhsT=Pn[b][:, j, :], rhs=Xn[b][:, j, :], start=True, stop=True)
                if last:
                    dve.tensor_tensor(out=TmT[:, sl, :], in0=pC[:, 0:n, :], in1=Xn[b][:, 0:n, :], op=ALU.add)
                else:
                    dve.tensor_tensor(out=Xn[1 - b][:, 0:n, :], in0=pC[:, 0:n, :], in1=Xn[b][:, 0:n, :],
                                      op=ALU.add)
                if hook is not None:
                    hook()

    def elem(hq, chunks, ch, sample, N0, vb, kbg):
        nc_ = len(chunks)
        Lst = C["Lst_s"] if sample else C["Lst_p"]
        Uin = C["Uin_s"] if sample else C["Uin_p"]
        pG = K.bank(0, F32, [128, 4, 128])
        pAt = K.bank(1, F32, [128, 4, 128])
        for ci, c in enumerate(chunks):
            cs = slice(c * 128, (c + 1) * 128)
            pe.matmul(out=pG[:, ci, :], lhsT=kT[:, cs], rhs=kT[:, cs], start=True, stop=True)
            pe.matmul(out=pAt[:, ci, :], lhsT=kT[:, cs], rhs=qT[:, cs], start=True, stop=True)
        dve.tensor_tensor(out=Gs[:, 0:nc_, :], in0=pG[:, 0:nc_, :], in1=Lst.un(1).bc([128, nc_, 128]), op=ALU.mult)
        dve.tensor_tensor(out=Atm[:, 0:nc_, :], in0=pAt[:, 0:nc_, :], in1=Uin.un(1).bc([128, nc_, 128]), op=ALU.mult)
        yield
        pR = [K.bank(2, F32, [128, 4, 128]), K.bank(7, F32, [128, 4, 128])]
        for ci, c in enumerate(chunks):
            for e in range(2):
                x = ci * 2 + e
                hv = 2 * hq + e
                gcol = P["gc"][:, c, hv:hv + 1]
                dve.tensor_scalar(out=dg[:, x % 4, :], in0=C["ident"], scalar1=gcol, scalar2=None, op0=ALU.mult)
                pr = pR[x // 4][:, x % 4, :]
                pe.matmul(out=pr, lhsT=C["ones"], rhs=dg[:, x % 4, :], start=True, stop=True)
                dve.tensor_scalar(out=t1[:, x % 4, :], in0=pr, scalar1=gcol, scalar2=0.0, op0=ALU.subtract, op1=ALU.max)
                act.activation(out=Dm[:, x % 4, :], in_=t1[:, x % 4, :], func=AF.Exp, scale=-1.0)
                dve.scalar_tensor_tensor(out=N0[:, x, :], in0=Gs[:, ci, :], scalar=P["negbeta"][:, c, hv:hv + 1],
                                         in1=Dm[:, x % 4, :], op0=ALU.mult, op1=ALU.mult)
                dve.tensor_scalar(out=t1[:, x % 4, :], in0=pr, scalar1=gcol, scalar2=0.0, op0=ALU.subtract, op1=ALU.min)
                act.activation(out=Dm[:, x % 4, :], in_=t1[:, x % 4, :], func=AF.Exp)
                dve.tensor_tensor(out=ch["A"][:, x, :], in0=Atm[:, ci, :], in1=Dm[:, x % 4, :], op=ALU.mult)
                act.activation(out=Egm[:, x % 4, :], in_=pr, func=AF.Exp)
                dve.tensor_tensor(out=ch["qg"][:, x, :], in0=qT[:, c * 128:(c + 1) * 128], in1=Egm[:, x % 4, :], op=ALU.mult)
                dve.tensor_scalar(out=vb[:, x, :], in0=v_tok[e][:, c, :], scalar1=P["negbeta"][:, c, hv:hv + 1],
                                  scalar2=-1.0, op0=ALU.mult, op1=ALU.mult)
                pool.tensor_scalar(out=kbg[:, x, :], in0=k_tok[:, c, :], scalar1=P["bge"][:, c, hv:hv + 1],
                                   scalar2=1.0, op0=ALU.mult, op1=ALU.mult)
                pool.tensor_scalar(out=ch["kd"][:, x, :], in0=k_tok[:, c, :], scalar1=P["kdec"][:, c, hv:hv + 1],
                                   scalar2=1.0, op0=ALU.mult, op1=ALU.mult)
                yield

    def exhaust(g_):
        for _ in g_:
            pass

    def solve(nc_, ch, sample, N0, vb, kbg, hook=None):
        neumann(2 * nc_, 3 if sample else 7, N0, hook)
        pU = [K.bank(0, F32, [128, 4, 128]), K.bank(1, F32, [128, 4, 128])]
        pW = [K.bank(2, F32, [128, 4, 128]), K.bank(7, F32, [128, 4, 128])]
        for x in range(2 * nc_):
            if sample:
                pe.matmul(out=pU[x // 4][:, x % 4, :], lhsT=vb[:, x, :], rhs=TmT[:, x, :], start=True, stop=True)
            else:
                pe.matmul(out=pU[x // 4][:, x % 4, :], lhsT=TmT[:, x, :], rhs=vb[:, x, :], start=True, stop=True)
            pe.matmul(out=pW[x // 4][:, x % 4, :], lhsT=kbg[:, x, :], rhs=TmT[:, x, :], start=True, stop=True)
        for h0 in range(0, 2 * nc_, 4):
            n = min(4, 2 * nc_ - h0)
            act.activation(out=ch["u"][:, h0:h0 + n, :], in_=pU[h0 // 4][:, 0:n, :], func=AF.Copy)
            dve.tensor_copy(out=ch["wT"][:, h0:h0 + n, :], in_=pW[h0 // 4][:, 0:n, :])

    def load_w6(hq_):
        cols_ = colsel(hq_)
        for i in range(6):
            K.dma("pool", w6[i], w_in[:, cols_[i]:cols_[i] + 128].rearrange("(k p) n -> p k n", p=128))

    load_w6(0)
    for hq in range(NHQ):
        cols = colsel(hq)
        if STOP == "w6":
            continue
        for i in range(4):
            K.dma("sp", st48[:, i, :], I["sgc"].rearrange("s t c -> (s t) c")[:, cols[i]:cols[i] + 128])
        pst = K.bank(7, F32, [128, 4, 48])
        for i in range(4):
            pe.transpose(out=pst[:, i, :], in_=st48[:, i, :], identity=C["ident"][0:48, 0:48])
        for i in range(4):
            rs_ = raw[i][:, 2051:2051 + 176].re("p (s t) -> p s t", t=11)
            evac(K, rs_[:, :, 0:3], pst[:, i, :].re("p (s t) -> p s t", t=3))
            dve.memset(ap=raw[i][:, 0:3], constant=0.0)
        if STOP == "st":
            continue
        for bi, (c0, nb) in enumerate(blocks):
            if STOP == "blk0" and bi > 0:
                continue
            if (STOP == "blkp" or "nosamp" in os.environ.get("DBG", "")) and bi == 8:
                continue
            ab_ = aTb[bi % 2]
            for kh in range(2):
                K.dma("sp", ab_[:, 8 * kh:8 * kh + 8, 0:nb], aT_scr[:, 8 * kh:8 * kh + 8, c0:c0 + nb])
            DBG = os.environ.get("DBG", "")
            for i in range(6):
                pb = K.bank(i // 2, F32, [128, 2, 256])[:, i % 2, 0:nb]
                if "nomm" not in DBG:
                    for k in range(16):
                        pe.matmul(out=pb, lhsT=(C["identb"] if "idw" in DBG else w6[i][:, k, :]),
                                  rhs=(xdbg[:, 0:nb] if "xd" in DBG else ab_[:, k, 0:nb]), start=(k == 0), stop=(k == 15))
                if "noev" in DBG:
                    continue
                if i < 4:
                    if c0 < 2048:
                        evac(K, raw[i][:, 3 + c0:3 + c0 + nb], pb)
                        if c0 == 1792 and "nogbp" not in DBG:
                            dve.tensor_copy(out=gbp_sb[:, cols[i] // 128, :], in_=pb[:, 253:256])
                    else:
                        rs_ = raw[i][:, 2051:2051 + 176].re("p (s t) -> p s t", t=11)
                        act.activation(out=smp, in_=pb, func=AF.Copy)
                        dve.tensor_copy(out=rs_[:, :, 3:11], in_=smp.re("p (s t) -> p s t", t=8))
                        dve.tensor_copy(out=gb48[:, 0:48].re("p (s t) -> p s t", t=3),
                                        in_=smp.re("p (s t) -> p s t", t=8)[:, :, 5:8])
                        pgb = K.bank(3, F32, [128, 128])
                        pe.transpose(out=pgb, in_=gb48, identity=C["ident"])
                        act.activation(out=gbs_sb, in_=pgb[0:48, :], func=AF.Copy)
                        K.dma("sp", O["gbs"].rearrange("s t c -> (s t) c")[:, cols[i]:cols[i] + 128], gbs_sb, final=True)
                elif "nozs" not in DBG:
                    act.activation(out=zs[i - 4][:, c0:c0 + nb], in_=pb,
                                   func=(AF.Copy if os.environ.get("NOSILU") else AF.Silu))
        if STOP == "proj":
            continue
        for i in range(4):
            cc = cols[i] // 128
            rp = raw[i][:, 0:2051]
            rs_ = raw[i][:, 2051:2051 + 176].re("p (s t) -> p s t", t=11)
            cvp = cv[:, 0:2048]
            cvs = cv[:, 2048:NT].re("p (s t) -> p s t", t=8)
            dve.tensor_scalar(out=cvp, in0=rp[:, 3:2051], scalar1=cw[:, cc, 3:4], scalar2=None, op0=ALU.mult)
            dve.tensor_scalar(out=cvs, in0=rs_[:, :, 3:11], scalar1=cw[:, cc, 3:4], scalar2=None, op0=ALU.mult)
            for j in range(3):
                dve.scalar_tensor_tensor(out=cvp, in0=rp[:, j:j + 2048], scalar=cw[:, cc, j:j + 1], in1=cvp,
                                         op0=ALU.mult, op1=ALU.add)
                dve.scalar_tensor_tensor(out=cvs, in0=rs_[:, :, j:j + 8], scalar=cw[:, cc, j:j + 1], in1=cvs,
                                         op0=ALU.mult, op1=ALU.add)
            if i < 2:
                act.activation(out=cv, in_=cv, func=AF.Silu)
                act.activation(out=tmp1, in_=cv, func=AF.Square)
                dst = qT if i == 0 else kT
                for b0 in range(0, NT, 512):
                    nb = min(512, NT - b0)
                    pss = K.bank(3, F32, [128, 512])[:, 0:nb]
                    pe.matmul(out=pss, lhsT=C["ones"], rhs=tmp1[:, b0:b0 + nb], start=True, stop=True)
                    act.activation(out=rinv[:, 0:nb], in_=pss, func=AF.Sqrt, bias=EPS)
                    dve.reciprocal(out=rinv[:, 0:nb], in_=rinv[:, 0:nb])
                    dve.scalar_tensor_tensor(out=dst[:, b0:b0 + nb], in0=cv[:, b0:b0 + nb],
                                             scalar=(128.0 ** -0.5 if i == 0 else 1.0), in1=rinv[:, 0:nb],
                                             op0=ALU.mult, op1=ALU.mult)
                if i == 1:
                    transpose_chunks(K, C, kT, k_tok, NCH)
            else:
                act.activation(out=vTb, in_=cv, func=AF.Silu)
                transpose_chunks(K, C, vTb, v_tok[i - 2], NCH)
        if STOP == "conv":
            continue
        if conv_chunks:
            for _ in range(min(4, len(conv_chunks))):
                dst, src = conv_chunks.pop(0)
                K.dma("pool", dst, src, bulk=True)
        if hq + 1 < NHQ:
            load_w6(hq + 1)
        for e in range(2):
            dve.memset(ap=Sf[e], constant=0.0)
            dve.memset(ap=Sb[e], constant=0.0)
        exhaust(elem(hq, [0, 1, 2, 3], CH[0], False, N0s[0], vbs[0], kbgs[0]))
        for gi in range(4):
            chunks = list(range(gi * 4, gi * 4 + 4))
            ch = CH[gi % 2]
            p_ = gi % 2
            if gi + 1 < 4:
                nxt = elem(hq, list(range(gi * 4 + 4, gi * 4 + 8)), CH[1 - p_], False, N0s[1 - p_], vbs[1 - p_], kbgs[1 - p_])
            else:
                nxt = elem(hq, [16], CH[0], True, N0s[0], vbs[0], kbgs[0])
            solve(4, ch, False, N0s[p_], vbs[p_], kbgs[p_], hook=lambda g_=nxt: next(g_, None))
            exhaust(nxt)
            for ci, c in enumerate(chunks):
                for e in range(2):
                    x = ci * 2 + e
                    hv = 2 * hq + e
                    pv = K.bank(0 + e, F32, [128, 128])
                    pe.matmul(out=pv, lhsT=ch["wT"][:, x, :], rhs=Sb[e], start=True, stop=True)
                    dve.tensor_tensor(out=vnew[e], in0=ch["u"][:, x, :], in1=pv, op=ALU.subtract)
                    po = K.bank(2 + e, F32, [128, 128])
                    pe.matmul(out=po, lhsT=Sb[e], rhs=ch["qg"][:, x, :], start=True, stop=False)
                    pe.matmul(out=po, lhsT=vnew[e], rhs=ch["A"][:, x, :], start=False, stop=True)
                    pd = K.bank(4 + e, F32, [128, 128])
                    pe.matmul(out=pd, lhsT=ch["kd"][:, x, :], rhs=vnew[e], start=True, stop=True)
                    dve.scalar_tensor_tensor(out=Sf[e], in0=Sf[e], scalar=P["decS"][:, c, hv:hv + 1], in1=pd,
                                             op0=ALU.mult, op1=ALU.add)
                    act.activation(out=Sb[e], in_=Sf[e], func=AF.Copy)
                    act.activation(out=oTr[e][:, c * 128:(c + 1) * 128], in_=po, func=AF.Copy)
        for e in range(2):
            K.dma("sp", O["gSp"][2 * hq + e], Sf[e], final=True)
        ch = CH[0]
        solve(1, ch, True, N0s[0], vbs[0], kbgs[0])
        Sall = cv.re("p (s v) -> p s v", s=17)[:, 0:16, :]
        Snew = tmp1.re("p (s v) -> p s v", s=17)[:, 0:16, :]
        Sab = raw[0][:, 0:2048].re("p (s v) -> p s v", s=16)
        Vblk = raw[1][:, 0:2048].re("p (s v) -> p s v", s=16)
        cs = slice(2048, NT)
        for e in range(2):
            hv = 2 * hq + e
            for sh in range(2):
                K.dma("sp", Sall[:, 8 * sh:8 * sh + 8, :], I["sgr"][8 * sh:8 * sh + 8, hv].rearrange("s k v -> k s v"))
            act.activation(out=Sab, in_=Sall, func=AF.Copy)
            pws = K.bank(0, F32, [128, 128])
            pos = K.bank(1, F32, [128, 128])
            for s in range(16):
                pe.matmul(out=pws[:, 8 * s:8 * s + 8], lhsT=Sab[:, s, :], rhs=ch["wT"][:, e, 8 * s:8 * s + 8],
                          start=True, stop=True)
                pe.matmul(out=pos[:, 8 * s:8 * s + 8], lhsT=Sab[:, s, :], rhs=ch["qg"][:, e, 8 * s:8 * s + 8],
                          start=True, stop=True)
            vnT = Egm[:, 0, :]
            dve.tensor_tensor(out=vnT, in0=ch["u"][:, e, :], in1=pws, op=ALU.subtract)
            vnTb = TmT[:, 7, :]
            act.activation(out=vnTb, in_=vnT, func=AF.Copy)
            pvt = K.bank(2, BF16, [128, 128])
            pe.transpose(out=pvt, in_=vnTb, identity=C["identb"])
            act.activation(out=vnew[e], in_=pvt, func=AF.Copy)
            poa = K.bank(3, F32, [128, 128])
            pe.matmul(out=poa, lhsT=vnew[e], rhs=ch["A"][:, e, :], start=True, stop=True)
            osb = Egm[:, 1, :]
            act.activation(out=osb, in_=pos, func=AF.Copy)
            dve.tensor_tensor(out=oTr[e][:, cs], in0=osb, in1=poa, op=ALU.add)
            dve.tensor_tensor(out=Vblk, in0=vnew[e].un(1).bc([128, 16, 128]),
                              in1=C["rowmask"].un(2).bc([128, 16, 128]), op=ALU.mult)
            for q4 in range(4):
                psd = K.bank(4 + q4, F32, [128, 4, 128])
                pe.matmul(out=psd, lhsT=ch["kd"][:, e, :], rhs=Vblk[:, 4 * q4:4 * q4 + 4, :], start=True, stop=True)
                dve.tensor_tensor(out=Snew[:, 4 * q4:4 * q4 + 4, :], in0=Sall[:, 4 * q4:4 * q4 + 4, :],
                                  in1=P["decSs"][:, hv, 4 * q4:4 * q4 + 4].un(2).bc([128, 4, 128]), op=ALU.mult)
                dve.tensor_tensor(out=Snew[:, 4 * q4:4 * q4 + 4, :], in0=Snew[:, 4 * q4:4 * q4 + 4, :],
                                  in1=psd, op=ALU.add)
            K.dma("sp", O["gSs"][:, hv].rearrange("s k v -> k s v"), Snew, final=True)
        if STOP == "samp":
            continue
        for e in range(2):
            hv = 2 * hq + e
            og = cv
            act.activation(out=tmp1, in_=oTr[e], func=AF.Square)
            for b0 in range(0, NT, 512):
                nb = min(512, NT - b0)
                pss = K.bank(3, F32, [128, 512])[:, 0:nb]
                pe.matmul(out=pss, lhsT=C["ones"], rhs=tmp1[:, b0:b0 + nb], start=True, stop=True)
                act.activation(out=rinv[:, 0:nb], in_=pss, func=AF.Sqrt, scale=1.0 / 128, bias=EPS)
                dve.reciprocal(out=rinv[:, 0:nb], in_=rinv[:, 0:nb])
                dve.scalar_tensor_tensor(out=og[:, b0:b0 + nb], in0=oTr[e][:, b0:b0 + nb], scalar=onorm,
                                         in1=rinv[:, 0:nb], op0=ALU.mult, op1=ALU.mult)
            dve.tensor_tensor(out=og, in0=og, in1=zs[e], op=ALU.mult)
            dve.tensor_scalar(out=osel[:, 0:32], in0=og[:, 992:1024], scalar1=sel[:, 1:2], scalar2=None, op0=ALU.mult)
            dve.tensor_scalar(out=tmp1[:, 0:1024], in0=og[:, 0:1024], scalar1=sel[:, 0:1], scalar2=None, op0=ALU.mult)
            dve.scalar_tensor_tensor(out=osel[:, 32:1056], in0=og[:, 1024:2048], scalar=sel[:, 1:2],
                                     in1=tmp1[:, 0:1024], op0=ALU.mult, op1=ALU.add)
            act.activation(out=osel[:, 1056:NOWN], in_=og[:, 2048:NT], func=AF.Copy)
            K.dma("sp", osel_scr[hv], osel)
    for t in range(3):
        K.dma("sp", O["gbp"][t].rearrange("(c p) -> p c", p=128), gbp_sb[:, :, t], final=True,
              allow_slow_non_contiguous=True)


def stage_oproj(K, C, I, H, osel_scr):
    dve, act, pe = K.dve, K.act, K.pe
    w_out = I["gdn_w_out"]
    m = K.mark()
    ot = K.alloc("ot", [128, 32, NOWN], BF16)
    for hv in range(32):
        K.dma("sp", ot[:, hv, :], osel_scr[hv])
    dve.memset(ap=H[0], constant=0.0)
    K.dma("sp", H[0][0:32, :], I["xo"][0:32, :])
    for t in range(1, 9):
        K.dma("sp", H[t], I["xo"][32 + (t - 1) * 128:32 + t * 128, :])
    K.dma("sp", H[9], I["xs"][:, :])
    wb = [K.alloc(f"wob{i}", [128, 32, 256], BF16) for i in range(2)]
    n = 0
    for blk in range(8):
        w = wb[blk % 2]
        K.dma("pool", w, w_out[:, blk * 256:(blk + 1) * 256].rearrange("(h p) n -> p h n", p=128))
        for t in range(10):
            np_ = 32 if t == 0 else 128
            c0 = 0 if t == 0 else 32 + (t - 1) * 128
            ps = K.bank(n % 8, F32, [128, 2, 256])[0:np_, (n // 8) % 2, :]
            n += 1
            for hv in range(32):
                pe.matmul(out=ps, lhsT=ot[:, hv, c0:c0 + np_], rhs=w[:, hv, :], start=(hv == 0), stop=(hv == 31))
            hs = H[t][0:np_, blk * 256:(blk + 1) * 256]
            dve.tensor_tensor(out=hs, in0=hs, in1=ps, op=ALU.add)
    K.release(m)


def stage_peer(K, C, I, H, layer, tiles, TB):
    dve, act, pe, pool = K.dve, K.act, K.pe, K.pool
    NEG = -1.0e30
    m = K.mark()
    w_q = I["peer_w_q"][layer]
    uv_tab, uv_bufs = TB[0], TB[1][layer]
    gffn = K.alloc("gffn", [128, D])
    K.dma("sp", gffn, I["norm_ffn"][layer].partition_broadcast(128))
    kT = K.alloc("kTk", [128, 2, 128])
    ktmp = K.alloc("ktmp", [128, 2, 128])
    K.dma("sp", ktmp[:, 0, :], I["peer_keys1"][layer])
    K.dma("sp", ktmp[:, 1, :], I["peer_keys2"][layer])
    pk = K.bank(0, F32, [128, 2, 128])
    for hf in range(2):
        pe.transpose(out=pk[:, hf, :], in_=ktmp[:, hf, :], identity=C["ident"])
    dve.tensor_copy(out=kT, in_=pk)
    iota = K.alloc("iota", [128, 256])
    pool.iota(out=iota, pattern=[[1, 256]], base=0, channel_multiplier=0, allow_small_or_imprecise_dtypes=True)
    xnbs = [K.alloc(f"xnb{i}", [128, D], BF16) for i in range(2)]
    xnT = K.alloc("xnT", [128, 16, 128], BF16)
    ss = K.alloc("ssp", [128, 1])
    NW = 3
    wq = [K.alloc(f"wq{i}", [128, 16, 256], BF16) for i in range(NW)]
    tv = K.alloc("tv", [128, 16, 16])
    ti = K.alloc("ti", [128, 16, 16], U32)
    tif = K.alloc("tif", [128, 16, 16])
    cand = K.alloc("cand", [128, 8, 256])
    scr1 = K.alloc("scr1", [128, 256])
    cid = K.alloc("cid", [128, 8, 256])
    qT = cand.re("p h (a b) -> p (h a) b", a=2)
    sc0 = cid.re("p h (a b) -> p (h a) b", a=2)
    scv = K.alloc("scv", [128, 8, 16])
    pos = K.alloc("pos", [128, 8, 16], U32)
    posf = K.alloc("posf", [128, 8, 16])
    junk = K.alloc("junk", [128, 256])
    eidf = K.alloc("eidf", [128, 128])
    eids = [K.alloc(f"eid{i}", [128, 128], U32) for i in range(2)]
    negm = K.alloc("negm", [128, 8])
    zsum = K.alloc("zsum", [128, 8])
    gates = [K.alloc(f"gate{i}", [128, 8, 16]) for i in range(2)]
    NR = 8
    hvs = [K.alloc(f"hv{i}", [128, 1]) for i in range(NR)]
    acs = [K.alloc(f"ac{i}", [128, 1]) for i in range(NR)]
    NG = 6
    gb = [K.alloc(f"gb{i}", [128, 2 * D], BF16) for i in range(NG)]
    dgs = [K.alloc(f"dgd{i}", [128, 128], BF16) for i in range(3)]
    accb = [K.bank(b_, F32, [128, 512]) for b_ in (0, 1, 6, 7)]
    sq = xnT.re("p a b -> p (a b)")
    print("PEER arena top:", K.top)

    def load_wq(blk):
        K.dma("pool", wq[blk % NW], w_q[:, blk * 256:(blk + 1) * 256].rearrange("(k p) n -> p k n", p=128))

    def front(t, par):
        h = H[t]
        eid, gate, xnb = eids[par], gates[par], xnbs[par]
        load_wq(0)
        load_wq(1)
        rmsnorm_tok(K, h, gffn, xnb, sq, ss)
        yield
        transpose_chunks(K, C, xnb, xnT, 16)
        yield
        for blk in range(8):
            if blk + 2 < 8:
                load_wq(blk + 2)
            w = wq[blk % NW]
            pq = K.bank(4 + blk % 2, F32, [128, 2, 128])
            for j in range(2):
                for k in range(16):
                    pe.matmul(out=pq[:, j, :], lhsT=w[:, k, j * 128:(j + 1) * 128], rhs=xnT[:, k, :],
                              start=(k == 0), stop=(k == 15))
            evac(K, qT[:, blk * 2:(blk + 1) * 2, :], pq)
            yield
        for g4 in range(4):
            psc = K.bank(4 + g4 % 2, F32, [128, 4, 128])
            for j in range(4):
                hc = g4 * 4 + j
                pe.matmul(out=psc[:, j, :], lhsT=qT[:, hc, :], rhs=kT[:, hc % 2, :], start=True, stop=True)
            evac(K, sc0[:, g4 * 4:(g4 + 1) * 4, :], psc)
        yield
        for hc in range(16):
            dve.max(out=tv[:, hc, 0:8], in_=sc0[:, hc, :])
            dve.max_index(out=ti[:, hc, 0:8], in_max=tv[:, hc, 0:8], in_values=sc0[:, hc, :])
            dve.match_replace(out=scr1[:, 0:128], in_to_replace=tv[:, hc, 0:8], in_values=sc0[:, hc, :], imm_value=NEG)
            dve.max(out=tv[:, hc, 8:16], in_=scr1[:, 0:128])
            dve.max_index(out=ti[:, hc, 8:16], in_max=tv[:, hc, 8:16], in_values=scr1[:, 0:128])
            if hc % 4 == 3:
                yield
        dve.tensor_copy(out=tif, in_=ti)
        tv4 = tv.re("p (h f) k -> p h f k", f=2)
        ti4 = tif.re("p (h f) k -> p h f k", f=2)
        c4 = cand.re("p h (i j) -> p h i j", j=16)
        d4 = cid.re("p h (i j) -> p h i j", j=16)
        for hh in range(8):
            dve.tensor_tensor(out=c4[:, hh], in0=tv4[:, hh, 0, :].un(2).bc([128, 16, 16]),
                              in1=tv4[:, hh, 1, :].un(1).bc([128, 16, 16]), op=ALU.add)
            dve.scalar_tensor_tensor(out=d4[:, hh], in0=ti4[:, hh, 0, :].un(2).bc([128, 16, 16]), scalar=128.0,
                                     in1=ti4[:, hh, 1, :].un(1).bc([128, 16, 16]), op0=ALU.mult, op1=ALU.add)
        yield
        for hh in range(8):
            dve.max(out=scv[:, hh, 0:8], in_=cand[:, hh, :])
            dve.max_index(out=pos[:, hh, 0:8], in_max=scv[:, hh, 0:8], in_values=cand[:, hh, :])
            dve.match_replace(out=scr1, in_to_replace=scv[:, hh, 0:8], in_values=cand[:, hh, :], imm_value=NEG)
            dve.max(out=scv[:, hh, 8:16], in_=scr1)
            dve.max_index(out=pos[:, hh, 8:16], in_max=scv[:, hh, 8:16], in_values=scr1)
            if hh % 4 == 3:
                yield
        dve.tensor_copy(out=posf, in_=pos)
        for hh in range(8):
            for k in range(16):
                sl = hh * 16 + k
                dve.scalar_tensor_tensor(out=junk, in0=iota, scalar=posf[:, hh, k:k + 1], in1=cid[:, hh, :],
                                         op0=ALU.is_equal, op1=ALU.mult, accum_out=eidf[:, sl:sl + 1])
            yield
        if layer:
            dve.tensor_scalar(out=eidf, in0=eidf, scalar1=float(layer * 16384), scalar2=None, op0=ALU.add)
        dve.tensor_copy(out=eid, in_=eidf)
        dve.tensor_scalar(out=negm, in0=scv[:, :, 0], scalar1=-1.0, scalar2=None, op0=ALU.mult)
        for hh in range(8):
            act.activation(out=gate[:, hh, :], in_=scv[:, hh, :], func=AF.Exp, bias=negm[:, hh:hh + 1],
                           accum_out=zsum[:, hh:hh + 1])
        dve.reciprocal(out=zsum, in_=zsum)
        dve.tensor_tensor(out=gate, in0=gate, in1=zsum.un(2).bc([128, 8, 16]), op=ALU.mult)
        yield

    def exhaust(g):
        for _ in g:
            pass

    gi = 0
    exhaust(front(tiles[0], 0))
    for idx, t in enumerate(tiles):
        par = idx % 2
        h = H[t]
        eid, gate, xnb = eids[par], gates[par], xnbs[par]
        gflat = gate.re("p h k -> p (h k)")
        npr = 32 if (layer == 0 and t == 0) else 128
        nxt = front(tiles[idx + 1], 1 - par) if idx + 1 < len(tiles) else None
        for sl in range(128):
            g = gb[gi % NG]
            gi += 1
            K.gather(g[0:npr], uv_tab, eid[0:npr, sl:sl + 1], extra_reads=uv_bufs)
            hv, ac = hvs[sl % NR], acs[sl % NR]
            dve.scalar_tensor_tensor(out=g[0:npr, 0:D], in0=g[0:npr, 0:D], scalar=1.0, in1=xnb[0:npr], op0=ALU.mult,
                                     op1=ALU.mult, accum_out=hv[0:npr])
            act.activation(out=ac[0:npr], in_=hv[0:npr], func=AF.Gelu)
            act.activation(out=ac[0:npr], in_=ac[0:npr], func=AF.Copy, scale=gflat[0:npr, sl:sl + 1])
            dgt = dgs[sl % 3]
            act.activation(out=dgt[0:npr, 0:npr], in_=C["ident"][0:npr, 0:npr], func=AF.Copy, scale=ac[0:npr, 0:1])
            for q in range(4):
                pe.matmul(out=accb[q][0:npr], lhsT=dgt[0:npr, 0:npr], rhs=g[0:npr, D + q * 512:D + (q + 1) * 512],
                          start=(sl == 0), stop=(sl == 127))
            if nxt is not None and sl % 4 == 3:
                next(nxt, None)
        if nxt is not None:
            exhaust(nxt)
        for q in range(4):
            hq_ = h[0:npr, q * 512:(q + 1) * 512]
            dve.tensor_tensor(out=hq_, in0=hq_, in1=accb[q][0:npr], op=ALU.add)
    K.release(m)


def stage_ple(K, C, I, H, layer, tiles):
    dve, act, pe = K.dve, K.act, K.pe
    m = K.mark()
    gple = K.alloc("gple", [128, D])
    K.dma("sp", gple, I["norm_ple"][layer].partition_broadcast(128))
    bg = K.alloc("bg", [128, D])
    K.dma("sp", bg, I["ple_b_gate"][layer].partition_broadcast(128))
    wp = K.alloc("wp", [128, 2, D], BF16)
    K.dma("pool", wp, I["ple_w_proj"][layer].rearrange("(k p) n -> p k n", p=128))
    pnT = [K.alloc(f"pnT{t}", [128, 16, 128], BF16) for t in range(len(tiles))]
    pT = [K.alloc(f"pT{t}", [128, 2, 128], BF16) for t in range(len(tiles))]
    pn = K.alloc("pn", [128, D], BF16)
    pt = K.alloc("pt", [128, 256])
    ptb = K.alloc("ptb", [128, 256], BF16)
    sq = K.alloc("sqe", [128, D])
    ss = K.alloc("sse", [128, 1])
    gs = K.alloc("gs", [128, 256])
    wg = [K.alloc(f"wg{i}", [128, 16, 256], BF16) for i in range(2)]
    print("PLE arena top:", K.top)
    for ti_, t in enumerate(tiles):
        rmsnorm_tok(K, H[t], gple, pn, sq, ss)
        transpose_chunks(K, C, pn, pnT[ti_], 16)
        if t == 0:
            dve.memset(ap=pt, constant=0.0)
            K.dma("sp", pt[0:32, :], I["pp"][layer, 0:32, :])
        elif t == 9:
            K.dma("sp", pt, I["psm"][layer])
        else:
            K.dma("sp", pt, I["pp"][layer, 32 + (t - 1) * 128:32 + t * 128, :])
        act.activation(out=ptb, in_=pt, func=AF.Copy)
        transpose_chunks(K, C, ptb, pT[ti_], 2)
    n = 0
    for blk in range(8):
        w = wg[blk % 2]
        cs_ = slice(blk * 256, (blk + 1) * 256)
        K.dma("pool", w, I["ple_w_gate"][layer][:, cs_].rearrange("(k p) n -> p k n", p=128))
        for ti_, t in enumerate(tiles):
            pg = K.bank(n % 4, F32, [128, 256])
            pp_ = K.bank(4 + n % 4, F32, [128, 256])
            n += 1
            for k in range(16):
                pe.matmul(out=pg, lhsT=pnT[ti_][:, k, :], rhs=w[:, k, :], start=(k == 0), stop=(k == 15))
            for k in range(2):
                pe.matmul(out=pp_, lhsT=pT[ti_][:, k, :], rhs=wp[:, k, cs_], start=(k == 0), stop=(k == 1))
            dve.tensor_tensor(out=gs, in0=pg, in1=bg[:, cs_], op=ALU.add)
            act.activation(out=gs, in_=gs, func=AF.Sigmoid)
            dve.tensor_tensor(out=gs, in0=gs, in1=pp_, op=ALU.mult)
            hs = H[t][:, cs_]
            dve.tensor_tensor(out=hs, in0=hs, in1=gs, op=ALU.add)
    K.release(m)


NCT = 1184


def stage_conf(K, C, I, O, H):
    dve, act, pe, pool = K.dve, K.act, K.pe, K.pool
    w_in = I["conf_w_in"]
    mA = K.mark()
    cbf = K.alloc("cbf", [128, 16, 1152], BF16)
    mB = K.mark()
    def chan(name, src_row):
        t = K.alloc(name, [128, 16])
        K.dma("sp", t, src_row.rearrange("(c p) -> p c", p=128), allow_slow_non_contiguous=True)
        return t
    b_v = chan("b_v", I["conf_b_in"][0, 0:2048])
    b_g = chan("b_g", I["conf_b_in"][0, 2048:4096])
    dwb = chan("dwb", I["conf_dw_b"][0])
    dww = K.alloc("dww", [128, 16, 31])
    for j in range(31):
        K.dma("sp", dww[:, :, j], I["conf_dw_w"][j].rearrange("(c p) -> p c", p=128), allow_slow_non_contiguous=True)
    sel = K.alloc("selc", [128, 4])
    K.dma("sp", sel, I["sel"][:, :])
    aT = K.alloc("aTc", [128, 16, NCT], BF16)
    mT = K.mark()
    gmix = K.alloc("gmix1", [128, D])
    K.dma("sp", gmix, I["norm_mix"][1].partition_broadcast(128))
    xn = K.alloc("xnc", [128, D], BF16)
    xT = K.alloc("xTc", [128, 16, 128], BF16)
    sq = K.alloc("sqc", [128, D])
    ss = K.alloc("ssc", [128, 1])
    for t in range(10):
        rmsnorm_tok(K, H[t], gmix, xn, sq, ss)
        transpose_chunks(K, C, xn, xT, 16)
        if t == 0:
            dve.tensor_copy(out=aT[:, :, 0:32], in_=xT[:, :, 0:32])
        else:
            dve.tensor_copy(out=aT[:, :, 32 + (t - 1) * 128:32 + t * 128], in_=xT)
    K.release(mT)
    st = K.alloc("stc", [128, 4, 128])
    dve.memset(ap=st, constant=0.0)
    scc2 = I["scc"].rearrange("s t c -> (s t) c")
    K.dma("sp", O["cbs"][:, 0:22, :], I["scc"][:, 8:30, :], final=True)
    ext_p = K.alloc("ext_p", [128, 30 + 1024])
    ext_s = K.alloc("ext_s", [128, 16, 38])
    exs = K.alloc("exs", [128, 512])
    glu = K.alloc("glu", [128, NCT])
    sg = K.alloc("sg", [128, 512])
    cacc = K.alloc("cacc", [128, 1152])
    cb_sb = K.alloc("cb_sb", [128, 2, 128])
    wv = [K.alloc(f"wv{i}", [128, 16, 128], BF16) for i in range(1)]
    wgt = [K.alloc(f"wgt{i}", [128, 16, 128], BF16) for i in range(1)]
    print("CONF arena top:", K.top)
    blocks = [(0, 512), (512, 512), (1024, 160)]
    for c in range(16):
        wv_, wg_ = wv[0], wgt[0]
        for r in range(4):
            nr = 128 if r < 3 else 96
            K.dma("sp", st[0:nr, r, :], scc2[r * 128:r * 128 + nr, c * 128:(c + 1) * 128])
        K.dma("pool", wv_, w_in[:, c * 128:(c + 1) * 128].rearrange("(k p) n -> p k n", p=128))
        K.dma("pool", wg_, w_in[:, 2048 + c * 128:2048 + (c + 1) * 128].rearrange("(k p) n -> p k n", p=128))
        pst = K.bank(7, F32, [128, 512])
        for r in range(4):
            pe.transpose(out=pst[:, r * 128:(r + 1) * 128], in_=st[:, r, :], identity=C["ident"])
        act.activation(out=exs, in_=pst, func=AF.Copy)
        dve.tensor_copy(out=ext_s[:, :, 0:30], in_=exs[:, 0:480].re("p (s t) -> p s t", t=30))
        for bi, (c0, nb) in enumerate(blocks):
            pv = K.bank(2 * (bi % 2), F32, [128, 512])[:, 0:nb]
            pg = K.bank(2 * (bi % 2) + 1, F32, [128, 512])[:, 0:nb]
            for k in range(16):
                pe.matmul(out=pv, lhsT=wv_[:, k, :], rhs=aT[:, k, c0:c0 + nb], start=(k == 0), stop=(k == 15))
            for k in range(16):
                pe.matmul(out=pg, lhsT=wg_[:, k, :], rhs=aT[:, k, c0:c0 + nb], start=(k == 0), stop=(k == 15))
            act.activation(out=sg[:, 0:nb], in_=pg, func=AF.Sigmoid, bias=b_g[:, c:c + 1])
            dve.scalar_tensor_tensor(out=glu[:, c0:c0 + nb], in0=pv, scalar=b_v[:, c:c + 1], in1=sg[:, 0:nb],
                                     op0=ALU.add, op1=ALU.mult)
        dve.tensor_scalar(out=ext_p[:, 0:30], in0=glu[:, 2:32], scalar1=sel[:, 1:2], scalar2=None, op0=ALU.mult)
        act.activation(out=ext_p[:, 30:1054], in_=glu[:, 32:1056], func=AF.Copy)
        act.activation(out=ext_s[:, :, 30:38], in_=glu[:, 1056:1184].re("p (s t) -> p s t", t=8), func=AF.Copy)
        pcb = K.bank(6, F32, [128, 2, 128])
        pe.transpose(out=pcb[:, 0, :], in_=ext_p[:, 926:1054], identity=C["ident"])
        pe.transpose(out=pcb[:, 1, :], in_=glu[:, 1056:1184], identity=C["ident"])
        act.activation(out=cb_sb, in_=pcb, func=AF.Copy)
        K.dma("sp", O["cbp"][:, c * 128:(c + 1) * 128], cb_sb[98:128, 0, :], final=True)
        for s_ in range(16):
            K.dma("sp", O["cbs"][s_, 22:30, c * 128:(c + 1) * 128], cb_sb[8 * s_:8 * s_ + 8, 1, :], final=True)
        cp = cacc[:, 0:1024]
        cs = cacc[:, 1024:1152].re("p (s t) -> p s t", t=8)
        dve.tensor_scalar(out=cp, in0=ext_p[:, 0:1024], scalar1=dww[:, c, 0:1], scalar2=dwb[:, c:c + 1],
                          op0=ALU.mult, op1=ALU.add)
        dve.tensor_scalar(out=cs, in0=ext_s[:, :, 0:8], scalar1=dww[:, c, 0:1], scalar2=dwb[:, c:c + 1],
                          op0=ALU.mult, op1=ALU.add)
        for j in range(1, 31):
            dve.scalar_tensor_tensor(out=cp, in0=ext_p[:, j:j + 1024], scalar=dww[:, c, j:j + 1], in1=cp,
                                     op0=ALU.mult, op1=ALU.add)
            dve.scalar_tensor_tensor(out=cs, in0=ext_s[:, :, j:j + 8], scalar=dww[:, c, j:j + 1], in1=cs,
                                     op0=ALU.mult, op1=ALU.add)
        act.activation(out=cbf[:, c, :], in_=cacc, func=AF.Copy)
    K.release(mB)
    mC = K.mark()
    lng = K.alloc("lng", [128, 16])
    lnb = K.alloc("lnb", [128, 16])
    K.dma("sp", lng, I["conf_ln_g"][0].rearrange("(c p) -> p c", p=128), allow_slow_non_contiguous=True)
    K.dma("sp", lnb, I["conf_ln_b"][0].rearrange("(c p) -> p c", p=128), allow_slow_non_contiguous=True)
    cf = K.alloc("cf", [128, 512])
    c2 = K.alloc("c2", [128, 512])
    mu = K.alloc("mu", [128, 512])
    rstd = K.alloc("rstd", [128, 512])
    tmpn = K.alloc("tmpn", [128, 512])
    for (c0, nb) in [(0, 512), (512, 512), (1024, 128)]:
        psum_s = K.bank(0, F32, [128, 512])[:, 0:nb]
        psum_q = K.bank(1, F32, [128, 512])[:, 0:nb]
        for c in range(16):
            act.activation(out=cf[:, 0:nb], in_=cbf[:, c, c0:c0 + nb], func=AF.Copy)
            act.activation(out=c2[:, 0:nb], in_=cbf[:, c, c0:c0 + nb], func=AF.Square)
            pe.matmul(out=psum_s, lhsT=C["ones"], rhs=cf[:, 0:nb], start=(c == 0), stop=(c == 15))
            pe.matmul(out=psum_q, lhsT=C["ones"], rhs=c2[:, 0:nb], start=(c == 0), stop=(c == 15))
        act.activation(out=mu[:, 0:nb], in_=psum_s, func=AF.Copy, scale=1.0 / D)
        dve.tensor_tensor(out=tmpn[:, 0:nb], in0=mu[:, 0:nb], in1=mu[:, 0:nb], op=ALU.mult)
        dve.scalar_tensor_tensor(out=rstd[:, 0:nb], in0=psum_q, scalar=1.0 / D, in1=tmpn[:, 0:nb],
                                 op0=ALU.mult, op1=ALU.subtract)
        act.activation(out=rstd[:, 0:nb], in_=rstd[:, 0:nb], func=AF.Sqrt, bias=EPS)
        dve.reciprocal(out=rstd[:, 0:nb], in_=rstd[:, 0:nb])
        for c in range(16):
            dve.tensor_tensor(out=tmpn[:, 0:nb], in0=cbf[:, c, c0:c0 + nb], in1=mu[:, 0:nb], op=ALU.subtract)
            dve.tensor_tensor(out=tmpn[:, 0:nb], in0=tmpn[:, 0:nb], in1=rstd[:, 0:nb], op=ALU.mult)
            act.activation(out=cbf[:, c, c0:c0 + nb], in_=tmpn[:, 0:nb], func=AF.Silu,
                           scale=lng[:, c:c + 1], bias=lnb[:, c:c + 1])
    bo = K.alloc("bo", [128, D])
    K.dma("sp", bo, I["conf_b_out"][0].partition_broadcast(128))
    wo = [K.alloc(f"wo{i}", [128, 16, 512], BF16) for i in range(2)]
    ysb = K.alloc("ysb", [128, 512])
    n = 0
    for blk in range(4):
        w = wo[blk % 2]
        K.dma("pool", w, I["conf_w_out"][:, blk * 512:(blk + 1) * 512].rearrange("(k p) n -> p k n", p=128))
        for t in range(1, 10):
            ps = K.bank(2 + n % 6, F32, [128, 512])
            n += 1
            for c in range(16):
                pe.matmul(out=ps, lhsT=cbf[:, c, (t - 1) * 128:t * 128], rhs=w[:, c, :], start=(c == 0), stop=(c == 15))
            dve.tensor_tensor(out=ysb, in0=ps, in1=bo[:, blk * 512:(blk + 1) * 512], op=ALU.add)
            hs = H[t][:, blk * 512:(blk + 1) * 512]
            dve.tensor_tensor(out=hs, in0=hs, in1=ysb, op=ALU.add)
    K.release(mA)


def stage_final(K, C, I, O, H):
    m = K.mark()
    gf = K.alloc("gf", [128, D])
    K.dma("sp", gf, I["final_norm"][0].partition_broadcast(128))
    yo = [K.alloc(f"yo{i}", [128, D]) for i in range(2)]
    sq = K.alloc("sqf", [128, D])
    ss = K.alloc("ssf", [128, 1])
    for t in range(1, 10):
        y = yo[t % 2]
        rmsnorm_tok(K, H[t], gf, y, sq, ss)
        if t < 9:
            K.dma("sp", O["yp"][(t - 1) * 128:t * 128, :], y, final=True)
        else:
            K.dma("sp", O["ys"][:, :], y, final=True)
    K.release(m)

W_SHAPES = {
    "norm_mix": (2, D), "norm_ffn": (2, D), "norm_ple": (2, D), "final_norm": (1, D),
    "gdn_w_in": (D, 12352), "gdn_conv_w": (4, 8192), "gdn_a_log": (1, 32), "gdn_dt_bias": (1, 32),
    "gdn_o_norm": (1, 128), "gdn_w_out": (4096, D),
    "conf_w_in": (D, 4096), "conf_b_in": (1, 4096), "conf_dw_w": (31, D), "conf_dw_b": (1, D),
    "conf_ln_g": (1, D), "conf_ln_b": (1, D), "conf_w_out": (D, D), "conf_b_out": (1, D),
    "peer_w_q": (2, D, D), "peer_keys1": (2, 128, 128), "peer_keys2": (2, 128, 128),
    "peer_u": (2 * 16384, D), "peer_v": (2 * 16384, D),
    "ple_w_proj": (2, 256, D), "ple_w_gate": (2, D, D), "ple_b_gate": (2, D),
}
ACT_SHAPES = {
    "xp": (2048, D), "xo": (1056, D), "pp": (2, 1056, 256), "xs": (128, D), "psm": (2, 128, 256),
    "sel": (128, 4), "sgr": (16, 32, 128, 128), "sgc": (16, 3, 8192), "scc": (16, 30, D),
}
OUT_SHAPES = {
    "yp": (1024, D), "ys": (128, D), "gSp": (32, 128, 128), "gSs": (16, 32, 128, 128),
    "gbp": (3, 8192), "gbs": (16, 3, 8192), "cbp": (30, D), "cbs": (16, 30, D),
}
STAGE_INPUTS = {
    "gdn": ["xp", "xs", "sel", "sgr", "sgc", "norm_mix", "gdn_w_in", "gdn_conv_w", "gdn_a_log", "gdn_dt_bias",
            "gdn_o_norm"],
    "oproj": ["xo", "xs", "gdn_w_out"],
    "peer0": ["norm_ffn", "peer_w_q", "peer_keys1", "peer_keys2", "peer_u", "peer_v"],
    "ple0": ["norm_ple", "ple_w_proj", "ple_w_gate", "ple_b_gate", "pp", "psm"],
    "conf": ["norm_mix", "sel", "scc", "conf_w_in", "conf_b_in", "conf_dw_w", "conf_dw_b", "conf_ln_g", "conf_ln_b",
             "conf_w_out", "conf_b_out"],
    "peer1": ["norm_ffn", "peer_w_q", "peer_keys1", "peer_keys2", "peer_u", "peer_v"],
    "ple1": ["norm_ple", "ple_w_proj", "ple_w_gate", "ple_b_gate", "pp", "psm"],
    "final": ["final_norm"],
}
STAGE_OUTPUTS = {"gdn": ["gSp", "gSs", "gbp", "gbs"], "conf": ["cbp", "cbs"], "final": ["yp", "ys"]}
ALL_STAGES = ("gdn", "oproj", "peer0", "ple0", "conf", "peer1", "ple1", "final")


def build(stages=("gdn",)):
    K = Kern()
    need_in, need_out = [], []
    for s in stages:
        need_in += [n for n in STAGE_INPUTS[s] if n not in need_in]
        need_out += [n for n in STAGE_OUTPUTS.get(s, []) if n not in need_out]
    I = {n: K.din(n, ACT_SHAPES.get(n) or W_SHAPES[n]) for n in need_in}
    O = {n: K.dout(n, OUT_SHAPES[n]) for n in need_out}
    C = build_consts(K)
    osel_scr = K.dscr("osel_scr", [32, 128, NOWN], BF16)
    TB, conv_chunks = None, []
    if "peer0" in stages or "peer1" in stages:
        scr = K.nc.dram_tensor("peer_uvb", [2 * 16384, 2 * D], BF16, kind="Internal").ap()
        TB = (scr, {0: [], 1: []})
        for layer in range(2):
            for ci_, nm in enumerate(("u", "v")):
                for r0 in range(layer * 16384, (layer + 1) * 16384, 1024):
                    b_ = Buf(f"{nm}b{r0}")
                    TB[1][layer].append(b_)
                    conv_chunks.append((V(scr[r0:r0 + 1024, ci_ * D:(ci_ + 1) * D], b_),
                                        I[f"peer_{nm}"][r0:r0 + 1024, :]))
    base = K.mark()
    if "gdn" in stages:
        stage_gdn(K, C, I, O, osel_scr, conv_chunks)
        K.release(base)
    for dst, src in conv_chunks:
        K.dma("pool", dst, src, bulk=True)
    H = [K.alloc(f"H{t}", [128, D]) for t in range(10)]
    if "oproj" in stages:
        stage_oproj(K, C, I, H, osel_scr)
    if "peer0" in stages:
        stage_peer(K, C, I, H, 0, list(range(10)), TB)
    if "ple0" in stages:
        stage_ple(K, C, I, H, 0, list(range(10)))
    if "conf" in stages:
        stage_conf(K, C, I, O, H)
    if "peer1" in stages:
        stage_peer(K, C, I, H, 1, list(range(1, 10)), TB)
    if "ple1" in stages:
        stage_ple(K, C, I, H, 1, list(range(1, 10)))
    if "final" in stages:
        stage_final(K, C, I, O, H)
    K.S.emit()
    K.es.close()
    return K


def prep_inputs(inp, names):
    f = lambda a: np.ascontiguousarray(a, dtype=np.float32)
    shared = {}
    for n, shp in W_SHAPES.items():
        if n in names:
            shared[n] = f(np.asarray(inp[n]).reshape(shp))
    maps = []
    for c in range(8):
        b, half = c // 2, c % 2
        m = dict(shared)
        xp = np.asarray(inp["x_prompt"][b])
        if "xp" in names:
            m["xp"] = f(xp)
        lo = half * 1024 - 32
        if "xo" in names:
            xo = np.zeros((1056, D), np.float32)
            if lo >= 0:
                xo[:] = xp[lo:lo + 1056]
            else:
                xo[32:] = xp[0:1024]
            m["xo"] = xo
        if "pp" in names:
            pp = np.zeros((2, 1056, 256), np.float32)
            pr = np.asarray(inp["p_prompt"][:, b])
            if lo >= 0:
                pp[:] = pr[:, lo:lo + 1056]
            else:
                pp[:, 32:] = pr[:, 0:1024]
            m["pp"] = pp
        if "xs" in names:
            m["xs"] = f(np.asarray(inp["x_sample"][16 * c:16 * c + 16]).reshape(128, D))
        if "psm" in names:
            m["psm"] = f(np.asarray(inp["p_sample"][:, 16 * c:16 * c + 16]).reshape(2, 128, 256))
        if "sel" in names:
            s = np.zeros((128, 4), np.float32)
            s[:, 0] = 1.0 - half
            s[:, 1] = float(half)
            m["sel"] = s
        if "sgr" in names:
            m["sgr"] = f(inp["state_gdn_recurrent"][0, 16 * c:16 * c + 16])
        if "sgc" in names:
            m["sgc"] = f(inp["state_gdn_conv"][0, 16 * c:16 * c + 16])
        if "scc" in names:
            m["scc"] = f(inp["state_conf_conv"][0, 16 * c:16 * c + 16])
        maps.append(m)
    return maps


_PROG = {}


def _program():
    if "K" not in _PROG:
        _PROG["K"] = build(ALL_STAGES)
    return _PROG["K"]


def assemble(results, n_cores=8):
    nb = n_cores // 2
    y_p = np.zeros((4, 2048, D), np.float32)
    y_s = np.zeros((128, 8, D), np.float32)
    gS_p = np.zeros((1, 4, 32, 128, 128), np.float32)
    gS_s = np.zeros((1, 128, 32, 128, 128), np.float32)
    gb_p = np.zeros((1, 4, 3, 8192), np.float32)
    gb_s = np.zeros((1, 128, 3, 8192), np.float32)
    cb_p = np.zeros((1, 4, 30, D), np.float32)
    cb_s = np.zeros((1, 128, 30, D), np.float32)
    for c in range(n_cores):
        r = results[c]
        b, half = c // 2, c % 2
        y_p[b, half * 1024:(half + 1) * 1024] = r["yp"]
        y_s[16 * c:16 * c + 16] = r["ys"].reshape(16, 8, D)
        gS_s[0, 16 * c:16 * c + 16] = r["gSs"]
        gb_s[0, 16 * c:16 * c + 16] = r["gbs"]
        cb_s[0, 16 * c:16 * c + 16] = r["cbs"]
        if half == 0:
            gS_p[0, b] = r["gSp"]
            gb_p[0, b] = r["gbp"]
        else:
            cb_p[0, b] = r["cbp"]
    return (y_p, y_s, gS_p, gS_s, gb_p, gb_s, cb_p, cb_s)


def kernel(**inputs):
    K = _program()
    maps = prep_inputs(inputs, list(K.ins.keys()))
    res = run_bass_kernel_spmd(K.nc, maps, core_ids=list(range(8)))
    return assemble(res.results, 8)
```

```python
import contextlib
import os
import numpy as np
import concourse.bass as bass
import concourse.mybir as mybir
from concourse.bass_utils import run_bass_kernel_spmd

F32 = mybir.dt.float32
BF16 = mybir.dt.bfloat16
U32 = mybir.dt.uint32
I32 = mybir.dt.int32
ALU = mybir.AluOpType
AF = mybir.ActivationFunctionType
AX = mybir.AxisListType

ENGS = ("pe", "dve", "act", "pool", "sp")
GEN = 30000
N_DMA_SEMS = 24
SEM_POOLS = {"sp": (0, 12), "pool": (12, 8), "bulk": (20, 4)}
EPS = 1e-6

D = 2048
NTP = 2048
NT = 2176
NCH = 17
NOWN = 1184
BIG = 30000.0


class Buf:
    __slots__ = ("name", "last_w", "readers", "psum")

    def __init__(self, name, psum=False):
        self.name = name
        self.last_w = None
        self.readers = []
        self.psum = psum


class V:
    __slots__ = ("ap", "b")

    def __init__(self, ap, b):
        self.ap = ap
        self.b = b

    def __getitem__(self, k):
        return V(self.ap[k], self.b)

    def bc(self, shape):
        return V(self.ap.to_broadcast(list(shape)), self.b)

    def un(self, axis):
        return V(self.ap.unsqueeze(axis), self.b)

    def re(self, pattern, **kw):
        return V(self.ap.rearrange(pattern, **kw), self.b)

    def cast(self, dt):
        return V(self.ap.bitcast(dt), self.b)

    @property
    def shape(self):
        return self.ap.shape


WRITE_KEYS = ("out", "accum_out", "ap")


class Sched:
    def __init__(self, nc):
        self.nc = nc
        self.ops = {e: [] for e in ENGS}
        self.cnt = {e: 0 for e in ENGS}
        self.seen = {e: {} for e in ENGS}
        self.dma_cnt = [0] * N_DMA_SEMS
        self.dma_rr = {k: 0 for k in SEM_POOLS}
        self.final_tokens = []
        self.n_ops = 0

    def _need(self, eng, tok):
        if tok is None:
            return
        kind, key, val = tok
        if kind == "e":
            if key == eng and eng == "pe":
                return
            g = (val - 1) // GEN
            skey = ("e", key, g)
            v = val - g * GEN
            for gg in range(g + 1, g + 6):
                if self.seen[eng].get(("e", key, gg), 0) > 0:
                    return
        else:
            skey = ("d", key)
            v = val
        if self.seen[eng].get(skey, 0) >= v:
            return
        self.seen[eng][skey] = v
        self.ops[eng].append(("wait", skey, v))

    @staticmethod
    def _compact(toks):
        best = {}
        for t in toks:
            k = (t[0], t[1])
            if k not in best or best[k][2] < t[2]:
                best[k] = t
        return list(best.values())

    def _deps(self, eng, reads, writes):
        for b in reads:
            self._need(eng, b.last_w)
            if b.psum:
                for t in b.readers:
                    if t[0] == "e" and t[1] != eng:
                        self._need(eng, t)
        for b in writes:
            self._need(eng, b.last_w)
            for t in b.readers:
                self._need(eng, t)

    def _commit(self, tok, reads, writes):
        for b in reads:
            b.readers.append(tok)
            if len(b.readers) > 16:
                b.readers = self._compact(b.readers)
        for b in writes:
            b.last_w = tok
            b.readers = []
        self.n_ops += 1

    def op(self, eng, fn, reads=(), writes=()):
        self._deps(eng, reads, writes)
        self.cnt[eng] += 1
        tok = ("e", eng, self.cnt[eng])
        self.ops[eng].append(("op", fn, self.cnt[eng]))
        self._commit(tok, reads, writes)
        return tok

    def dma(self, eng, fn, reads=(), writes=(), final=False, bulk=False):
        self._deps(eng, reads, writes)
        pk = "bulk" if bulk else eng
        base_, n_ = SEM_POOLS[pk]
        s = base_ + self.dma_rr[pk]
        self.dma_rr[pk] = (self.dma_rr[pk] + 1) % n_
        if self.dma_cnt[s] > 0:
            self._need(eng, ("d", s, self.dma_cnt[s]))
        self.dma_cnt[s] += 16
        tok = ("d", s, self.dma_cnt[s])
        self.ops[eng].append(("dma", fn, s))
        self._commit(tok, reads, writes)
        if final:
            self.final_tokens.append(tok)
        return tok

    def fence(self):
        toks = [("e", e, self.cnt[e]) for e in ENGS if self.cnt[e] > 0]
        toks += [("d", s, self.dma_cnt[s]) for s in range(N_DMA_SEMS) if self.dma_cnt[s] > 0]
        for e in ENGS:
            for t in toks:
                if not (t[0] == "e" and t[1] == e):
                    self._need(e, t)

    def emit(self):
        nc = self.nc
        for t in self.final_tokens:
            self._need("sp", t)
        with contextlib.ExitStack() as es:
            esem = {}
            for e in ENGS:
                for g in range(max(1, (self.cnt[e] + GEN - 1) // GEN)):
                    esem[(e, g)] = es.enter_context(nc.semaphore(f"p_{e}{g}"))
            dsem = [es.enter_context(nc.semaphore(f"d{i}")) for i in range(N_DMA_SEMS)]
            block = es.enter_context(nc.Block())

            def run(eng_name, engine):
                for item in self.ops[eng_name]:
                    if item[0] == "wait":
                        skey, v = item[1], item[2]
                        if skey[0] == "e":
                            engine.wait_ge(esem[(skey[1], skey[2])], v)
                        else:
                            engine.wait_ge(dsem[skey[1]], v)
                    elif item[0] == "op":
                        ins = item[1](engine)
                        ins.then_inc(esem[(eng_name, (item[2] - 1) // GEN)], 1)
                    else:
                        ins = item[1](engine)
                        ins.then_inc(dsem[item[2]], 16)

            @block.tensor
            def _(eng):
                run("pe", eng)

            @block.vector
            def _(eng):
                run("dve", eng)

            @block.scalar
            def _(eng):
                run("act", eng)

            @block.gpsimd
            def _(eng):
                run("pool", eng)

            @block.sync
            def _(eng):
                run("sp", eng)


class EngP:
    def __init__(self, K, name):
        self.K = K
        self.name = name

    def __getattr__(self, meth):
        K, name = self.K, self.name

        def call(**kw):
            reads, writes = [], []
            for key, val in kw.items():
                if isinstance(val, V):
                    (writes if key in WRITE_KEYS else reads).append(val.b)

            def fn(e):
                args = {k: (v.ap if isinstance(v, V) else v) for k, v in kw.items()}
                return getattr(e, meth)(**args)

            return K.S.op(name, fn, reads, writes)

        return call


class Kern:
    ARENA_F32 = 53000

    def __init__(self):
        self.nc = bass.Bass("TRN2", target_bir_lowering=False)
        self.S = Sched(self.nc)
        self.es = contextlib.ExitStack()
        self.pe = EngP(self, "pe")
        self.dve = EngP(self, "dve")
        self.act = EngP(self, "act")
        self.pool = EngP(self, "pool")
        self.arena = self.es.enter_context(self.nc.sbuf_tensor("arena", [128, self.ARENA_F32], F32))
        self.top = 0
        self.banks = [V(self.es.enter_context(self.nc.psum_tensor(f"bank{i}", [128, 512], F32))[:, :],
                        Buf(f"bank{i}", psum=True)) for i in range(8)]
        self.ins = {}
        self.outs = {}

    def din(self, name, shape, dt=F32):
        ap = self.nc.dram_tensor(name, list(shape), dt, kind="ExternalInput").ap()
        self.ins[name] = (tuple(shape), dt)
        return ap

    def dout(self, name, shape, dt=F32):
        ap = self.nc.dram_tensor(name, list(shape), dt, kind="ExternalOutput").ap()
        self.outs[name] = (tuple(shape), dt)
        return ap

    def dscr(self, name, shape, dt):
        ap = self.nc.dram_tensor(name, list(shape), dt, kind="Internal").ap()
        return V(ap, Buf(name))

    def alloc(self, name, shape, dt=F32):
        n = int(np.prod(shape[1:]))
        words = n if dt in (F32, U32, I32) else (n + 1) // 2
        words = (words + 1) // 2 * 2
        off = self.top
        self.top += words
        assert self.top <= self.ARENA_F32, f"arena overflow at {name}: {self.top}"
        ap = self.arena[0:shape[0], off:off + words]
        if dt != F32:
            ap = ap.bitcast(dt)
        ap = ap[:, 0:n]
        if len(shape) == 3:
            ap = ap.rearrange("p (a b) -> p a b", a=shape[1])
        elif len(shape) == 4:
            ap = ap.rearrange("p (a b c) -> p a b c", a=shape[1], b=shape[2])
        return V(ap, Buf(name))

    def mark(self):
        return self.top

    def release(self, m):
        self.S.fence()
        self.top = m

    def bank(self, i, dt=F32, shape=None):
        v = self.banks[i]
        ap = v.ap
        if dt != F32:
            ap = ap.bitcast(dt)
        if shape is not None:
            n = int(np.prod(shape[1:]))
            ap = ap[0:shape[0], 0:n]
            if len(shape) == 3:
                ap = ap.rearrange("p (a b) -> p a b", a=shape[1])
        return V(ap, v.b)

    def dma(self, q, out, in_, final=False, bulk=False, **kw):
        reads = [in_.b] if isinstance(in_, V) else []
        writes = [out.b] if isinstance(out, V) else []
        o = out.ap if isinstance(out, V) else out
        i = in_.ap if isinstance(in_, V) else in_
        return self.S.dma(q, lambda e: e.dma_start(out=o, in_=i, **kw), reads, writes, final=final, bulk=bulk)

    def gather(self, out, table_ap, idx, extra_reads=()):
        o, ia = out.ap, idx.ap
        return self.S.dma(
            "pool",
            lambda e: e.indirect_dma_start(out=o, out_offset=None, in_=table_ap,
                                           in_offset=bass.IndirectOffsetOnAxis(ap=ia, axis=0)),
            reads=[idx.b] + list(extra_reads), writes=[out.b])


def build_consts(K):
    C = {}
    pool, dve = K.pool, K.dve

    def mask(name, steps, op, base, cm, shape=(128, 128)):
        t = K.alloc(name, list(shape), F32)
        pool.memset(ap=t, constant=1.0)
        pool.affine_select(out=t, in_=t, pattern=steps, compare_op=op, fill=0.0, base=base,
                           channel_multiplier=cm)
        return t

    C["ident"] = mask("ident", [[-1, 128]], ALU.is_equal, 0, 1)
    C["ones"] = K.alloc("ones", [128, 128], F32)
    pool.memset(ap=C["ones"], constant=1.0)
    C["identb"] = K.alloc("identb", [128, 128], BF16)
    dve.tensor_copy(out=C["identb"], in_=C["ident"])
    C["Lst_p"] = mask("Lst_p", [[-1, 128]], ALU.is_gt, 0, 1)
    C["Uin_p"] = mask("Uin_p", [[1, 128]], ALU.is_ge, 0, -1)
    C["LS_p"] = mask("LS_p", [[0, 128]], ALU.is_equal, -127, 1)
    blk = K.alloc("blk", [128, 128], F32)
    pool.memset(ap=blk, constant=1.0)
    pool.affine_select(out=blk, in_=blk, pattern=[[-8, 16], [0, 8]], compare_op=ALU.is_ge, fill=0.0,
                       base=0, channel_multiplier=1)
    pool.affine_select(out=blk, in_=blk, pattern=[[8, 16], [0, 8]], compare_op=ALU.is_ge, fill=0.0,
                       base=7, channel_multiplier=-1)
    C["blk"] = blk
    C["Lst_s"] = K.alloc("Lst_s", [128, 128], F32)
    dve.tensor_tensor(out=C["Lst_s"], in0=C["Lst_p"], in1=blk, op=ALU.mult)
    C["Uin_s"] = K.alloc("Uin_s", [128, 128], F32)
    dve.tensor_tensor(out=C["Uin_s"], in0=C["Uin_p"], in1=blk, op=ALU.mult)
    C["LS_s"] = mask("LS_s", [[-8, 16], [0, 8]], ALU.is_equal, -7, 1)
    rm = K.alloc("rowmask", [128, 16], F32)
    pool.memset(ap=rm, constant=1.0)
    pool.affine_select(out=rm, in_=rm, pattern=[[-8, 16]], compare_op=ALU.is_ge, fill=0.0, base=0,
                       channel_multiplier=1)
    pool.affine_select(out=rm, in_=rm, pattern=[[8, 16]], compare_op=ALU.is_ge, fill=0.0, base=7,
                       channel_multiplier=-1)
    C["rowmask"] = rm
    C["lastmask"] = mask("lastmask", [[-8, 16]], ALU.is_equal, -7, 1, shape=(128, 16))
    return C


def rmsnorm_tok(K, x, g, out, sq, ss, n=D):
    K.act.activation(out=sq, in_=x, func=AF.Square, accum_out=ss)
    K.act.activation(out=ss, in_=ss, func=AF.Sqrt, scale=1.0 / n, bias=EPS)
    K.dve.reciprocal(out=ss, in_=ss)
    K.dve.scalar_tensor_tensor(out=out, in0=x, scalar=ss, in1=g, op0=ALU.mult, op1=ALU.mult)


_flip = [0]


def evac(K, out, in_):
    _flip[0] ^= 1
    if _flip[0]:
        K.act.activation(out=out, in_=in_, func=AF.Copy)
    else:
        K.dve.tensor_copy(out=out, in_=in_)


def transpose_chunks(K, C, src, dst, nk, banks=(2, 3), npart=128):
    for g0 in range(0, nk, 4):
        n = min(4, nk - g0)
        pb = K.bank(banks[(g0 // 4) % 2], BF16, [128, 4, 128])
        for j in range(n):
            K.pe.transpose(out=pb[:, j, 0:npart], in_=src[0:npart, (g0 + j) * 128:(g0 + j + 1) * 128],
                           identity=C["identb"][0:npart, 0:npart])
        evac(K, dst[:, g0:g0 + n, 0:npart], pb[:, 0:n, 0:npart])


def stage_gdn(K, C, I, O, osel_scr, conv_chunks=None):
    dve, act, pe, pool = K.dve, K.act, K.pe, K.pool
    aT_scr = K.dscr("aT_scr", [128, 16, NT], BF16)
    w_in = I["gdn_w_in"]

    m0 = K.mark()
    gmix = K.alloc("gmix", [128, D])
    K.dma("sp", gmix, I["norm_mix"][0].partition_broadcast(128))
    xts = [K.alloc(f"xt{i}", [128, D]) for i in range(2)]
    xns = [K.alloc(f"xn{i}", [128, D], BF16) for i in range(2)]
    xTs = [K.alloc(f"xT{i}", [128, 16, 128], BF16) for i in range(2)]
    sq = K.alloc("sq", [128, D])
    sss = [K.alloc(f"ss{i}", [128, 1]) for i in range(2)]
    for i in range(NCH):
        xt, xn, xT, ss = xts[i % 2], xns[i % 2], xTs[i % 2], sss[i % 2]
        src = I["xp"][i * 128:(i + 1) * 128, :] if i < 16 else I["xs"][:, :]
        K.dma("sp", xt, src)
        rmsnorm_tok(K, xt, gmix, xn, sq, ss)
        transpose_chunks(K, C, xn, xT, 16)
        K.dma("sp", aT_scr[:, :, i * 128:(i + 1) * 128], xT)
    K.release(m0)
    STOP = os.environ.get("GDN_STOP", "")
    NHQ = int(os.environ.get("GDN_NHQ", "16"))
    if STOP == "g0":
        return

    P = {}
    for nm in ("negbeta", "gc", "kdec", "bge", "decS"):
        P[nm] = K.alloc(nm, [128, NCH, 32])
    P["decSs"] = K.alloc("decSs", [128, 32, 16])
    cw = K.alloc("cw", [128, 64, 4])
    for j in range(4):
        K.dma("sp", cw[:, :, j], I["gdn_conv_w"][j].rearrange("(c p) -> p c", p=128), allow_slow_non_contiguous=True)
    onorm = K.alloc("onorm", [128, 1])
    K.dma("sp", onorm, I["gdn_o_norm"].rearrange("o p -> p o"), allow_slow_non_contiguous=True)
    sel = K.alloc("sel", [128, 4])
    K.dma("sp", sel, I["sel"][:, :])

    m1 = K.mark()
    wbd = K.alloc("wbd", [128, 16, 64], BF16)
    K.dma("pool", wbd, w_in[:, 12288:12352].rearrange("(k p) n -> p k n", p=128))
    bd = K.alloc("bd", [128, NCH, 64])
    ab = K.alloc("aTbd", [128, 16, 1024], BF16)
    for c0 in range(0, NCH, 8):
        nchk = min(8, NCH - c0)
        K.dma("sp", ab[:, :, 0:nchk * 128], aT_scr[:, :, c0 * 128:(c0 + nchk) * 128])
        pb = K.bank(0, F32, [128, 8, 64])
        for j in range(nchk):
            for k in range(16):
                pe.matmul(out=pb[:, j, :], lhsT=ab[:, k, j * 128:(j + 1) * 128], rhs=wbd[:, k, :],
                          start=(k == 0), stop=(k == 15))
        evac(K, bd[:, c0:c0 + nchk, :], pb[:, 0:nchk, :])
    alog = K.alloc("alog", [128, 32])
    dtb = K.alloc("dtb", [128, 32])
    K.dma("sp", alog, I["gdn_a_log"][0].partition_broadcast(128))
    K.dma("sp", dtb, I["gdn_dt_bias"][0].partition_broadcast(128))
    act.activation(out=alog, in_=alog, func=AF.Exp)
    dve.tensor_scalar(out=alog, in0=alog, scalar1=-1.0, scalar2=None, op0=ALU.mult)
    beta = K.alloc("beta", [128, NCH, 32])
    act.activation(out=beta, in_=bd[:, :, 0:32], func=AF.Sigmoid)
    dve.tensor_scalar(out=P["negbeta"], in0=beta, scalar1=-1.0, scalar2=None, op0=ALU.mult)
    g = K.alloc("g", [128, NCH, 32])
    dve.tensor_tensor(out=g, in0=bd[:, :, 32:64], in1=dtb.un(1).bc([128, NCH, 32]), op=ALU.add)
    act.activation(out=g, in_=g, func=AF.Exp)
    act.activation(out=g, in_=g, func=AF.Ln, bias=1.0)
    dve.tensor_tensor(out=g, in0=g, in1=alog.un(1).bc([128, NCH, 32]), op=ALU.mult)
    pg = K.bank(1, F32, [128, 16, 32])
    pe.matmul(out=pg, lhsT=C["Uin_p"], rhs=g[:, 0:16, :], start=True, stop=True)
    evac(K, P["gc"][:, 0:16, :], pg)
    pg2 = K.bank(0, F32, [128, 1, 32])
    pe.matmul(out=pg2, lhsT=C["Uin_s"], rhs=g[:, 16:17, :], start=True, stop=True)
    evac(K, P["gc"][:, 16:17, :], pg2)
    gl = K.alloc("gl", [128, NCH, 32])
    pl = K.bank(1, F32, [128, 16, 32])
    pe.matmul(out=pl, lhsT=C["LS_p"], rhs=P["gc"][:, 0:16, :], start=True, stop=True)
    evac(K, gl[:, 0:16, :], pl)
    pl2 = K.bank(0, F32, [128, 1, 32])
    pe.matmul(out=pl2, lhsT=C["LS_s"], rhs=P["gc"][:, 16:17, :], start=True, stop=True)
    evac(K, gl[:, 16:17, :], pl2)
    act.activation(out=P["decS"], in_=gl, func=AF.Exp)
    dve.tensor_tensor(out=gl, in0=gl, in1=P["gc"], op=ALU.subtract)
    act.activation(out=P["kdec"], in_=gl, func=AF.Exp)
    eg = K.alloc("eg", [128, NCH, 32])
    act.activation(out=eg, in_=P["gc"], func=AF.Exp)
    dve.tensor_tensor(out=P["bge"], in0=eg, in1=beta, op=ALU.mult)
    lm = K.alloc("lm", [128, 32, 16])
    dve.tensor_tensor(out=lm, in0=P["gc"][:, 16, :].un(2).bc([128, 32, 16]),
                      in1=C["lastmask"].un(1).bc([128, 32, 16]), op=ALU.mult)
    pls = K.bank(1, F32, [128, 32, 16])
    pe.matmul(out=pls, lhsT=C["ones"], rhs=lm, start=True, stop=True)
    act.activation(out=P["decSs"], in_=pls, func=AF.Exp)
    K.release(m1)
    if STOP == "g1":
        return

    gbp_sb = K.alloc("gbp_sb", [128, 64, 3])
    aTb = [K.alloc(f"aTb{i}", [128, 16, 256], BF16) for i in range(2)]
    w6 = [K.alloc(f"w6_{i}", [128, 16, 128], BF16) for i in range(6)]
    raw = [K.alloc(f"raw{i}", [128, 2051 + 176], BF16) for i in range(4)]
    cv = K.alloc("cv", [128, NT])
    tmp1 = K.alloc("tmp1", [128, NT])
    rinv = K.alloc("rinv", [128, 512])
    qT = K.alloc("qT", [128, NT], BF16)
    kT = K.alloc("kT", [128, NT], BF16)
    zs = [K.alloc(f"zs{i}", [128, NT], BF16) for i in range(2)]
    vTb = tmp1.cast(BF16)[:, 0:NT]
    k_tok = K.alloc("k_tok", [128, NCH, 128], BF16)
    v_tok = [K.alloc(f"v_tok{i}", [128, NCH, 128], BF16) for i in range(2)]
    oTr = [K.alloc(f"oTr{i}", [128, NT], BF16) for i in range(2)]
    osel = K.alloc("osel", [128, NOWN], BF16)
    st48 = K.alloc("st48", [48, 4, 128])
    smp = K.alloc("smp", [128, 128])
    gb48 = K.alloc("gb48", [128, 128])
    dve.memset(ap=gb48, constant=0.0)
    gbs_sb = K.alloc("gbs_sb", [48, 128])
    Gs = K.alloc("Gs", [128, 4, 128])
    Atm = K.alloc("Atm", [128, 4, 128])
    dg = K.alloc("dg", [128, 4, 128])
    t1 = K.alloc("t1", [128, 4, 128])
    Dm = K.alloc("Dm", [128, 4, 128])
    Egm = K.alloc("Egm", [128, 4, 128])
    N0s = [K.alloc(f"N0_{i}", [128, 8, 128]) for i in range(2)]
    Pn = [K.alloc(f"Pn{i}", [128, 4, 128]) for i in range(2)]
    Qn = [K.alloc(f"Qn{i}", [128, 4, 128]) for i in range(2)]
    Xn = [K.alloc(f"Xn{i}", [128, 4, 128]) for i in range(2)]
    TmT = K.alloc("TmT", [128, 8, 128], BF16)
    vbs = [K.alloc(f"vb{i}", [128, 8, 128], BF16) for i in range(2)]
    kbgs = [K.alloc(f"kbg{i}", [128, 8, 128], BF16) for i in range(2)]
    CH = []
    for i in range(2):
        CH.append(dict(wT=K.alloc(f"wT{i}", [128, 8, 128], BF16), u=K.alloc(f"u{i}", [128, 8, 128], BF16),
                       qg=K.alloc(f"qg{i}", [128, 8, 128], BF16), A=K.alloc(f"A{i}", [128, 8, 128], BF16),
                       kd=K.alloc(f"kd{i}", [128, 8, 128], BF16)))
    Sf = [K.alloc(f"Sf{i}", [128, 128]) for i in range(2)]
    Sb = [K.alloc(f"Sb{i}", [128, 128], BF16) for i in range(2)]
    vnew = [K.alloc(f"vnew{i}", [128, 128], BF16) for i in range(2)]
    print("GDN arena top (f32 words):", K.top)

    def colsel(hq):
        return [hq * 128, 2048 + hq * 128, 4096 + (2 * hq) * 128, 4096 + (2 * hq + 1) * 128,
                8192 + (2 * hq) * 128, 8192 + (2 * hq + 1) * 128]

    blocks = [(i * 256, 256) for i in range(8)] + [(2048, 128)]
    xdbg = None

    def neumann(nchain, nsteps, N0, hook=None):
        pT = K.bank(3, F32, [128, 4, 128])
        pA = K.bank(4, F32, [128, 4, 128])
        pB = K.bank(5, F32, [128, 4, 128])
        pC = K.bank(6, F32, [128, 4, 128])
        for h0 in range(0, nchain, 4):
            n = min(4, nchain - h0)
            sl = slice(h0, h0 + n)
            for j in range(n):
                pe.transpose(out=pT[:, j, :], in_=N0[:, h0 + j, :], identity=C["ident"])
            act.activation(out=Qn[0][:, 0:n, :], in_=pT[:, 0:n, :], func=AF.Copy)
            dve.tensor_tensor(out=Xn[1][:, 0:n, :], in0=pT[:, 0:n, :],
                              in1=C["ident"].un(1).bc([128, n, 128]), op=ALU.add)
            for k in range(1, nsteps):
                a, b = (k - 1) % 2, k % 2
                last = (k == nsteps - 1)
                Pa = (lambda j: N0[:, h0 + j, :]) if k == 1 else (lambda j, a=a: Pn[a][:, j, :])
                for j in range(n):
                    pe.matmul(out=pA[:, j, :], lhsT=Qn[a][:, j, :], rhs=Pa(j), start=True, stop=True)
                if not last:
                    for j in range(n):
                        pe.matmul(out=pB[:, j, :], lhsT=Pa(j), rhs=Qn[a][:, j, :], start=True, stop=True)
                act.activation(out=Pn[b][:, 0:n, :], in_=pA[:, 0:n, :], func=AF.Copy)
                if not last:
                    dve.tensor_copy(out=Qn[b][:, 0:n, :], in_=pB[:, 0:n, :])
                for j in range(n):
                    pe.matmul(out=pC[:, j, :], lhsT=Pn[b][:, j, :], rhs=Xn[b][:, j, :], start=True, stop=True)
                if last:
                    dve.tensor_tensor(out=TmT[:, sl, :], in0=pC[:, 0:n, :], in1=Xn[b][:, 0:n, :], op=ALU.add)
                else:
                    dve.tensor_tensor(out=Xn[1 - b][:, 0:n, :], in0=pC[:, 0:n, :], in1=Xn[b][:, 0:n, :],
                                      op=ALU.add)
                if hook is not None:
                    hook()

    def elem(hq, chunks, ch, sample, N0, vb, kbg):
        nc_ = len(chunks)
        Lst = C["Lst_s"] if sample else C["Lst_p"]
        Uin = C["Uin_s"] if sample else C["Uin_p"]
        pG = K.bank(0, F32, [128, 4, 128])
        pAt = K.bank(1, F32, [128, 4, 128])
        for ci, c in enumerate(chunks):
            cs = slice(c * 128, (c + 1) * 128)
            pe.matmul(out=pG[:, ci, :], lhsT=kT[:, cs], rhs=kT[:, cs], start=True, stop=True)
            pe.matmul(out=pAt[:, ci, :], lhsT=kT[:, cs], rhs=qT[:, cs], start=True, stop=True)
        dve.tensor_tensor(out=Gs[:, 0:nc_, :], in0=pG[:, 0:nc_, :], in1=Lst.un(1).bc([128, nc_, 128]), op=ALU.mult)
        dve.tensor_tensor(out=Atm[:, 0:nc_, :], in0=pAt[:, 0:nc_, :], in1=Uin.un(1).bc([128, nc_, 128]), op=ALU.mult)
        yield
        pR = [K.bank(2, F32, [128, 4, 128]), K.bank(7, F32, [128, 4, 128])]
        for ci, c in enumerate(chunks):
            for e in range(2):
                x = ci * 2 + e
                hv = 2 * hq + e
                gcol = P["gc"][:, c, hv:hv + 1]
                dve.tensor_scalar(out=dg[:, x % 4, :], in0=C["ident"], scalar1=gcol, scalar2=None, op0=ALU.mult)
                pr = pR[x // 4][:, x % 4, :]
                pe.matmul(out=pr, lhsT=C["ones"], rhs=dg[:, x % 4, :], start=True, stop=True)
                dve.tensor_scalar(out=t1[:, x % 4, :], in0=pr, scalar1=gcol, scalar2=0.0, op0=ALU.subtract, op1=ALU.max)
                act.activation(out=Dm[:, x % 4, :], in_=t1[:, x % 4, :], func=AF.Exp, scale=-1.0)
                dve.scalar_tensor_tensor(out=N0[:, x, :], in0=Gs[:, ci, :], scalar=P["negbeta"][:, c, hv:hv + 1],
                                         in1=Dm[:, x % 4, :], op0=ALU.mult, op1=ALU.mult)
                dve.tensor_scalar(out=t1[:, x % 4, :], in0=pr, scalar1=gcol, scalar2=0.0, op0=ALU.subtract, op1=ALU.min)
                act.activation(out=Dm[:, x % 4, :], in_=t1[:, x % 4, :], func=AF.Exp)
                dve.tensor_tensor(out=ch["A"][:, x, :], in0=Atm[:, ci, :], in1=Dm[:, x % 4, :], op=ALU.mult)
                act.activation(out=Egm[:, x % 4, :], in_=pr, func=AF.Exp)
                dve.tensor_tensor(out=ch["qg"][:, x, :], in0=qT[:, c * 128:(c + 1) * 128], in1=Egm[:, x % 4, :], op=ALU.mult)
                dve.tensor_scalar(out=vb[:, x, :], in0=v_tok[e][:, c, :], scalar1=P["negbeta"][:, c, hv:hv + 1],
                                  scalar2=-1.0, op0=ALU.mult, op1=ALU.mult)
                pool.tensor_scalar(out=kbg[:, x, :], in0=k_tok[:, c, :], scalar1=P["bge"][:, c, hv:hv + 1],
                                   scalar2=1.0, op0=ALU.mult, op1=ALU.mult)
                pool.tensor_scalar(out=ch["kd"][:, x, :], in0=k_tok[:, c, :], scalar1=P["kdec"][:, c, hv:hv + 1],
                                   scalar2=1.0, op0=ALU.mult, op1=ALU.mult)
                yield

    def exhaust(g_):
        for _ in g_:
            pass

    def solve(nc_, ch, sample, N0, vb, kbg, hook=None):
        neumann(2 * nc_, 3 if sample else 7, N0, hook)
        pU = [K.bank(0, F32, [128, 4, 128]), K.bank(1, F32, [128, 4, 128])]
        pW = [K.bank(2, F32, [128, 4, 128]), K.bank(7, F32, [128, 4, 128])]
        for x in range(2 * nc_):
            if sample:
                pe.matmul(out=pU[x // 4][:, x % 4, :], lhsT=vb[:, x, :], rhs=TmT[:, x, :], start=True, stop=True)
            else:
                pe.matmul(out=pU[x // 4][:, x % 4, :], lhsT=TmT[:, x, :], rhs=vb[:, x, :], start=True, stop=True)
            pe.matmul(out=pW[x // 4][:, x % 4, :], lhsT=kbg[:, x, :], rhs=TmT[:, x, :], start=True, stop=True)
        for h0 in range(0, 2 * nc_, 4):
            n = min(4, 2 * nc_ - h0)
            act.activation(out=ch["u"][:, h0:h0 + n, :], in_=pU[h0 // 4][:, 0:n, :], func=AF.Copy)
            dve.tensor_copy(out=ch["wT"][:, h0:h0 + n, :], in_=pW[h0 // 4][:, 0:n, :])

    def load_w6(hq_):
        cols_ = colsel(hq_)
        for i in range(6):
            K.dma("pool", w6[i], w_in[:, cols_[i]:cols_[i] + 128].rearrange("(k p) n -> p k n", p=128))

    load_w6(0)
    for hq in range(NHQ):
        cols = colsel(hq)
        if STOP == "w6":
            continue
        for i in range(4):
            K.dma("sp", st48[:, i, :], I["sgc"].rearrange("s t c -> (s t) c")[:, cols[i]:cols[i] + 128])
        pst = K.bank(7, F32, [128, 4, 48])
        for i in range(4):
            pe.transpose(out=pst[:, i, :], in_=st48[:, i, :], identity=C["ident"][0:48, 0:48])
        for i in range(4):
            rs_ = raw[i][:, 2051:2051 + 176].re("p (s t) -> p s t", t=11)
            evac(K, rs_[:, :, 0:3], pst[:, i, :].re("p (s t) -> p s t", t=3))
            dve.memset(ap=raw[i][:, 0:3], constant=0.0)
        if STOP == "st":
            continue
        for bi, (c0, nb) in enumerate(blocks):
            if STOP == "blk0" and bi > 0:
                continue
            if (STOP == "blkp" or "nosamp" in os.environ.get("DBG", "")) and bi == 8:
                continue
            ab_ = aTb[bi % 2]
            for kh in range(2):
                K.dma("sp", ab_[:, 8 * kh:8 * kh + 8, 0:nb], aT_scr[:, 8 * kh:8 * kh + 8, c0:c0 + nb])
            DBG = os.environ.get("DBG", "")
            for i in range(6):
                pb = K.bank(i // 2, F32, [128, 2, 256])[:, i % 2, 0:nb]
                if "nomm" not in DBG:
                    for k in range(16):
                        pe.matmul(out=pb, lhsT=(C["identb"] if "idw" in DBG else w6[i][:, k, :]),
                                  rhs=(xdbg[:, 0:nb] if "xd" in DBG else ab_[:, k, 0:nb]), start=(k == 0), stop=(k == 15))
                if "noev" in DBG:
                    continue
                if i < 4:
                    if c0 < 2048:
                        evac(K, raw[i][:, 3 + c0:3 + c0 + nb], pb)
                        if c0 == 1792 and "nogbp" not in DBG:
                            dve.tensor_copy(out=gbp_sb[:, cols[i] // 128, :], in_=pb[:, 253:256])
                    else:
                        rs_ = raw[i][:, 2051:2051 + 176].re("p (s t) -> p s t", t=11)
                        act.activation(out=smp, in_=pb, func=AF.Copy)
                        dve.tensor_copy(out=rs_[:, :, 3:11], in_=smp.re("p (s t) -> p s t", t=8))
                        dve.tensor_copy(out=gb48[:, 0:48].re("p (s t) -> p s t", t=3),
                                        in_=smp.re("p (s t) -> p s t", t=8)[:, :, 5:8])
                        pgb = K.bank(3, F32, [128, 128])
                        pe.transpose(out=pgb, in_=gb48, identity=C["ident"])
                        act.activation(out=gbs_sb, in_=pgb[0:48, :], func=AF.Copy)
                        K.dma("sp", O["gbs"].rearrange("s t c -> (s t) c")[:, cols[i]:cols[i] + 128], gbs_sb, final=True)
                elif "nozs" not in DBG:
                    act.activation(out=zs[i - 4][:, c0:c0 + nb], in_=pb,
                                   func=(AF.Copy if os.environ.get("NOSILU") else AF.Silu))
        if STOP == "proj":
            continue
        for i in range(4):
            cc = cols[i] // 128
            rp = raw[i][:, 0:2051]
            rs_ = raw[i][:, 2051:2051 + 176].re("p (s t) -> p s t", t=11)
            cvp = cv[:, 0:2048]
            cvs = cv[:, 2048:NT].re("p (s t) -> p s t", t=8)
            dve.tensor_scalar(out=cvp, in0=rp[:, 3:2051], scalar1=cw[:, cc, 3:4], scalar2=None, op0=ALU.mult)
            dve.tensor_scalar(out=cvs, in0=rs_[:, :, 3:11], scalar1=cw[:, cc, 3:4], scalar2=None, op0=ALU.mult)
            for j in range(3):
                dve.scalar_tensor_tensor(out=cvp, in0=rp[:, j:j + 2048], scalar=cw[:, cc, j:j + 1], in1=cvp,
                                         op0=ALU.mult, op1=ALU.add)
                dve.scalar_tensor_tensor(out=cvs, in0=rs_[:, :, j:j + 8], scalar=cw[:, cc, j:j + 1], in1=cvs,
                                         op0=ALU.mult, op1=ALU.add)
            if i < 2:
                act.activation(out=cv, in_=cv, func=AF.Silu)
                act.activation(out=tmp1, in_=cv, func=AF.Square)
                dst = qT if i == 0 else kT
                for b0 in range(0, NT, 512):
                    nb = min(512, NT - b0)
                    pss = K.bank(3, F32, [128, 512])[:, 0:nb]
                    pe.matmul(out=pss, lhsT=C["ones"], rhs=tmp1[:, b0:b0 + nb], start=True, stop=True)
                    act.activation(out=rinv[:, 0:nb], in_=pss, func=AF.Sqrt, bias=EPS)
                    dve.reciprocal(out=rinv[:, 0:nb], in_=rinv[:, 0:nb])
                    dve.scalar_tensor_tensor(out=dst[:, b0:b0 + nb], in0=cv[:, b0:b0 + nb],
                                             scalar=(128.0 ** -0.5 if i == 0 else 1.0), in1=rinv[:, 0:nb],
                                             op0=ALU.mult, op1=ALU.mult)
                if i == 1:
                    transpose_chunks(K, C, kT, k_tok, NCH)
            else:
                act.activation(out=vTb, in_=cv, func=AF.Silu)
                transpose_chunks(K, C, vTb, v_tok[i - 2], NCH)
        if STOP == "conv":
            continue
        if conv_chunks:
            for _ in range(min(4, len(conv_chunks))):
                dst, src = conv_chunks.pop(0)
                K.dma("pool", dst, src, bulk=True)
        if hq + 1 < NHQ:
            load_w6(hq + 1)
        for e in range(2):
            dve.memset(ap=Sf[e], constant=0.0)
            dve.memset(ap=Sb[e], constant=0.0)
        exhaust(elem(hq, [0, 1, 2, 3], CH[0], False, N0s[0], vbs[0], kbgs[0]))
        for gi in range(4):
            chunks = list(range(gi * 4, gi * 4 + 4))
            ch = CH[gi % 2]
            p_ = gi % 2
            if gi + 1 < 4:
                nxt = elem(hq, list(range(gi * 4 + 4, gi * 4 + 8)), CH[1 - p_], False, N0s[1 - p_], vbs[1 - p_], kbgs[1 - p_])
            else:
                nxt = elem(hq, [16], CH[0], True, N0s[0], vbs[0], kbgs[0])
            solve(4, ch, False, N0s[p_], vbs[p_], kbgs[p_], hook=lambda g_=nxt: next(g_, None))
            exhaust(nxt)
            for ci, c in enumerate(chunks):
                for e in range(2):
                    x = ci * 2 + e
                    hv = 2 * hq + e
                    pv = K.bank(0 + e, F32, [128, 128])
                    pe.matmul(out=pv, lhsT=ch["wT"][:, x, :], rhs=Sb[e], start=True, stop=True)
                    dve.tensor_tensor(out=vnew[e], in0=ch["u"][:, x, :], in1=pv, op=ALU.subtract)
                    po = K.bank(2 + e, F32, [128, 128])
                    pe.matmul(out=po, lhsT=Sb[e], rhs=ch["qg"][:, x, :], start=True, stop=False)
                    pe.matmul(out=po, lhsT=vnew[e], rhs=ch["A"][:, x, :], start=False, stop=True)
                    pd = K.bank(4 + e, F32, [128, 128])
                    pe.matmul(out=pd, lhsT=ch["kd"][:, x, :], rhs=vnew[e], start=True, stop=True)
                    dve.scalar_tensor_tensor(out=Sf[e], in0=Sf[e], scalar=P["decS"][:, c, hv:hv + 1], in1=pd,
                                             op0=ALU.mult, op1=ALU.add)
                    act.activation(out=Sb[e], in_=Sf[e], func=AF.Copy)
                    act.activation(out=oTr[e][:, c * 128:(c + 1) * 128], in_=po, func=AF.Copy)
        for e in range(2):
            K.dma("sp", O["gSp"][2 * hq + e], Sf[e], final=True)
        ch = CH[0]
        solve(1, ch, True, N0s[0], vbs[0], kbgs[0])
        Sall = cv.re("p (s v) -> p s v", s=17)[:, 0:16, :]
        Snew = tmp1.re("p (s v) -> p s v", s=17)[:, 0:16, :]
        Sab = raw[0][:, 0:2048].re("p (s v) -> p s v", s=16)
        Vblk = raw[1][:, 0:2048].re("p (s v) -> p s v", s=16)
        cs = slice(2048, NT)
        for e in range(2):
            hv = 2 * hq + e
            for sh in range(2):
                K.dma("sp", Sall[:, 8 * sh:8 * sh + 8, :], I["sgr"][8 * sh:8 * sh + 8, hv].rearrange("s k v -> k s v"))
            act.activation(out=Sab, in_=Sall, func=AF.Copy)
            pws = K.bank(0, F32, [128, 128])
            pos = K.bank(1, F32, [128, 128])
            for s in range(16):
                pe.matmul(out=pws[:, 8 * s:8 * s + 8], lhsT=Sab[:, s, :], rhs=ch["wT"][:, e, 8 * s:8 * s + 8],
                          start=True, stop=True)
                pe.matmul(out=pos[:, 8 * s:8 * s + 8], lhsT=Sab[:, s, :], rhs=ch["qg"][:, e, 8 * s:8 * s + 8],
                          start=True, stop=True)
            vnT = Egm[:, 0, :]
            dve.tensor_tensor(out=vnT, in0=ch["u"][:, e, :], in1=pws, op=ALU.subtract)
            vnTb = TmT[:, 7, :]
            act.activation(out=vnTb, in_=vnT, func=AF.Copy)
            pvt = K.bank(2, BF16, [128, 128])
            pe.transpose(out=pvt, in_=vnTb, identity=C["identb"])
            act.activation(out=vnew[e], in_=pvt, func=AF.Copy)
            poa = K.bank(3, F32, [128, 128])
            pe.matmul(out=poa, lhsT=vnew[e], rhs=ch["A"][:, e, :], start=True, stop=True)
            osb = Egm[:, 1, :]
            act.activation(out=osb, in_=pos, func=AF.Copy)
            dve.tensor_tensor(out=oTr[e][:, cs], in0=osb, in1=poa, op=ALU.add)
            dve.tensor_tensor(out=Vblk, in0=vnew[e].un(1).bc([128, 16, 128]),
                              in1=C["rowmask"].un(2).bc([128, 16, 128]), op=ALU.mult)
            for q4 in range(4):
                psd = K.bank(4 + q4, F32, [128, 4, 128])
                pe.matmul(out=psd, lhsT=ch["kd"][:, e, :], rhs=Vblk[:, 4 * q4:4 * q4 + 4, :], start=True, stop=True)
                dve.tensor_tensor(out=Snew[:, 4 * q4:4 * q4 + 4, :], in0=Sall[:, 4 * q4:4 * q4 + 4, :],
                                  in1=P["decSs"][:, hv, 4 * q4:4 * q4 + 4].un(2).bc([128, 4, 128]), op=ALU.mult)
                dve.tensor_tensor(out=Snew[:, 4 * q4:4 * q4 + 4, :], in0=Snew[:, 4 * q4:4 * q4 + 4, :],
                                  in1=psd, op=ALU.add)
            K.dma("sp", O["gSs"][:, hv].rearrange("s k v -> k s v"), Snew, final=True)
        if STOP == "samp":
            continue
        for e in range(2):
            hv = 2 * hq + e
            og = cv
            act.activation(out=tmp1, in_=oTr[e], func=AF.Square)
            for b0 in range(0, NT, 512):
                nb = min(512, NT - b0)
                pss = K.bank(3, F32, [128, 512])[:, 0:nb]
                pe.matmul(out=pss, lhsT=C["ones"], rhs=tmp1[:, b0:b0 + nb], start=True, stop=True)
                act.activation(out=rinv[:, 0:nb], in_=pss, func=AF.Sqrt, scale=1.0 / 128, bias=EPS)
                dve.reciprocal(out=rinv[:, 0:nb], in_=rinv[:, 0:nb])
                dve.scalar_tensor_tensor(out=og[:, b0:b0 + nb], in0=oTr[e][:, b0:b0 + nb], scalar=onorm,
                                         in1=rinv[:, 0:nb], op0=ALU.mult, op1=ALU.mult)
            dve.tensor_tensor(out=og, in0=og, in1=zs[e], op=ALU.mult)
            dve.tensor_scalar(out=osel[:, 0:32], in0=og[:, 992:1024], scalar1=sel[:, 1:2], scalar2=None, op0=ALU.mult)
            dve.tensor_scalar(out=tmp1[:, 0:1024], in0=og[:, 0:1024], scalar1=sel[:, 0:1], scalar2=None, op0=ALU.mult)
            dve.scalar_tensor_tensor(out=osel[:, 32:1056], in0=og[:, 1024:2048], scalar=sel[:, 1:2],
                                     in1=tmp1[:, 0:1024], op0=ALU.mult, op1=ALU.add)
            act.activation(out=osel[:, 1056:NOWN], in_=og[:, 2048:NT], func=AF.Copy)
            K.dma("sp", osel_scr[hv], osel)
    for t in range(3):
        K.dma("sp", O["gbp"][t].rearrange("(c p) -> p c", p=128), gbp_sb[:, :, t], final=True,
              allow_slow_non_contiguous=True)


def stage_oproj(K, C, I, H, osel_scr):
    dve, act, pe = K.dve, K.act, K.pe
    w_out = I["gdn_w_out"]
    m = K.mark()
    ot = K.alloc("ot", [128, 32, NOWN], BF16)
    for hv in range(32):
        K.dma("sp", ot[:, hv, :], osel_scr[hv])
    dve.memset(ap=H[0], constant=0.0)
    K.dma("sp", H[0][0:32, :], I["xo"][0:32, :])
    for t in range(1, 9):
        K.dma("sp", H[t], I["xo"][32 + (t - 1) * 128:32 + t * 128, :])
    K.dma("sp", H[9], I["xs"][:, :])
    wb = [K.alloc(f"wob{i}", [128, 32, 256], BF16) for i in range(2)]
    n = 0
    for blk in range(8):
        w = wb[blk % 2]
        K.dma("pool", w, w_out[:, blk * 256:(blk + 1) * 256].rearrange("(h p) n -> p h n", p=128))
        for t in range(10):
            np_ = 32 if t == 0 else 128
            c0 = 0 if t == 0 else 32 + (t - 1) * 128
            ps = K.bank(n % 8, F32, [128, 2, 256])[0:np_, (n // 8) % 2, :]
            n += 1
            for hv in range(32):
                pe.matmul(out=ps, lhsT=ot[:, hv, c0:c0 + np_], rhs=w[:, hv, :], start=(hv == 0), stop=(hv == 31))
            hs = H[t][0:np_, blk * 256:(blk + 1) * 256]
            dve.tensor_tensor(out=hs, in0=hs, in1=ps, op=ALU.add)
    K.release(m)


def stage_peer(K, C, I, H, layer, tiles, TB):
    dve, act, pe, pool = K.dve, K.act, K.pe, K.pool
    NEG = -1.0e30
    m = K.mark()
    w_q = I["peer_w_q"][layer]
    uv_tab, uv_bufs = TB[0], TB[1][layer]
    gffn = K.alloc("gffn", [128, D])
    K.dma("sp", gffn, I["norm_ffn"][layer].partition_broadcast(128))
    kT = K.alloc("kTk", [128, 2, 128])
    ktmp = K.alloc("ktmp", [128, 2, 128])
    K.dma("sp", ktmp[:, 0, :], I["peer_keys1"][layer])
    K.dma("sp", ktmp[:, 1, :], I["peer_keys2"][layer])
    pk = K.bank(0, F32, [128, 2, 128])
    for hf in range(2):
        pe.transpose(out=pk[:, hf, :], in_=ktmp[:, hf, :], identity=C["ident"])
    dve.tensor_copy(out=kT, in_=pk)
    iota = K.alloc("iota", [128, 256])
    pool.iota(out=iota, pattern=[[1, 256]], base=0, channel_multiplier=0, allow_small_or_imprecise_dtypes=True)
    xnbs = [K.alloc(f"xnb{i}", [128, D], BF16) for i in range(2)]
    xnT = K.alloc("xnT", [128, 16, 128], BF16)
    ss = K.alloc("ssp", [128, 1])
    NW = 2
    wq = [K.alloc(f"wq{i}", [128, 16, 256], BF16) for i in range(NW)]
    tv = K.alloc("tv", [128, 16, 16])
    ti = K.alloc("ti", [128, 16, 16], U32)
    tif = K.alloc("tif", [128, 16, 16])
    cand = K.alloc("cand", [128, 8, 256])
    scr1 = K.alloc("scr1", [128, 256])
    cid = K.alloc("cid", [128, 8, 256])
    qT = cand.re("p h (a b) -> p (h a) b", a=2)
    sc0 = cid.re("p h (a b) -> p (h a) b", a=2)
    scv = K.alloc("scv", [128, 8, 16])
    pos = K.alloc("pos", [128, 8, 16], U32)
    posf = K.alloc("posf", [128, 8, 16])
    junk = K.alloc("junk", [128, 256])
    eidf = K.alloc("eidf", [128, 128])
    eids = [K.alloc(f"eid{i}", [128, 128], U32) for i in range(2)]
    negm = K.alloc("negm", [128, 8])
    zsum = K.alloc("zsum", [128, 8])
    gates = [K.alloc(f"gate{i}", [128, 8, 16]) for i in range(2)]
    NR = 8
    hvs = [K.alloc(f"hv{i}", [128, 1]) for i in range(NR)]
    acs = [K.alloc(f"ac{i}", [128, 1]) for i in range(NR)]
    NG = 7
    gb = [K.alloc(f"gb{i}", [128, 2 * D], BF16) for i in range(NG)]
    dgs = [K.alloc(f"dgd{i}", [128, 128], BF16) for i in range(3)]
    accb = [K.bank(b_, F32, [128, 512]) for b_ in (0, 1, 6, 7)]
    sq = xnT.re("p a b -> p (a b)")
    print("PEER arena top:", K.top)

    def load_wq(blk):
        K.dma("pool", wq[blk % NW], w_q[:, blk * 256:(blk + 1) * 256].rearrange("(k p) n -> p k n", p=128))

    def front(t, par):
        h = H[t]
        eid, gate, xnb = eids[par], gates[par], xnbs[par]
        load_wq(0)
        rmsnorm_tok(K, h, gffn, xnb, sq, ss)
        yield
        transpose_chunks(K, C, xnb, xnT, 16)
        yield
        for blk in range(8):
            if blk + 1 < 8:
                load_wq(blk + 1)
            w = wq[blk % NW]
            pq = K.bank(4 + blk % 2, F32, [128, 2, 128])
            for j in range(2):
                for k in range(16):
                    pe.matmul(out=pq[:, j, :], lhsT=w[:, k, j * 128:(j + 1) * 128], rhs=xnT[:, k, :],
                              start=(k == 0), stop=(k == 15))
            evac(K, qT[:, blk * 2:(blk + 1) * 2, :], pq)
            yield
        for g4 in range(4):
            psc = K.bank(4 + g4 % 2, F32, [128, 4, 128])
            for j in range(4):
                hc = g4 * 4 + j
                pe.matmul(out=psc[:, j, :], lhsT=qT[:, hc, :], rhs=kT[:, hc % 2, :], start=True, stop=True)
            evac(K, sc0[:, g4 * 4:(g4 + 1) * 4, :], psc)
        yield
        for hc in range(16):
            dve.max(out=tv[:, hc, 0:8], in_=sc0[:, hc, :])
            dve.max_index(out=ti[:, hc, 0:8], in_max=tv[:, hc, 0:8], in_values=sc0[:, hc, :])
            dve.match_replace(out=scr1[:, 0:128], in_to_replace=tv[:, hc, 0:8], in_values=sc0[:, hc, :], imm_value=NEG)
            dve.max(out=tv[:, hc, 8:16], in_=scr1[:, 0:128])
            dve.max_index(out=ti[:, hc, 8:16], in_max=tv[:, hc, 8:16], in_values=scr1[:, 0:128])
            if hc % 4 == 3:
                yield
        dve.tensor_copy(out=tif, in_=ti)
        tv4 = tv.re("p (h f) k -> p h f k", f=2)
        ti4 = tif.re("p (h f) k -> p h f k", f=2)
        c4 = cand.re("p h (i j) -> p h i j", j=16)
        d4 = cid.re("p h (i j) -> p h i j", j=16)
        for hh in range(8):
            dve.tensor_tensor(out=c4[:, hh], in0=tv4[:, hh, 0, :].un(2).bc([128, 16, 16]),
                              in1=tv4[:, hh, 1, :].un(1).bc([128, 16, 16]), op=ALU.add)
            dve.scalar_tensor_tensor(out=d4[:, hh], in0=ti4[:, hh, 0, :].un(2).bc([128, 16, 16]), scalar=128.0,
                                     in1=ti4[:, hh, 1, :].un(1).bc([128, 16, 16]), op0=ALU.mult, op1=ALU.add)
        yield
        for hh in range(8):
            dve.max(out=scv[:, hh, 0:8], in_=cand[:, hh, :])
            dve.max_index(out=pos[:, hh, 0:8], in_max=scv[:, hh, 0:8], in_values=cand[:, hh, :])
            dve.match_replace(out=scr1, in_to_replace=scv[:, hh, 0:8], in_values=cand[:, hh, :], imm_value=NEG)
            dve.max(out=scv[:, hh, 8:16], in_=scr1)
            dve.max_index(out=pos[:, hh, 8:16], in_max=scv[:, hh, 8:16], in_values=scr1)
            if hh % 4 == 3:
                yield
        dve.tensor_copy(out=posf, in_=pos)
        for hh in range(8):
            for k in range(16):
                sl = hh * 16 + k
                dve.scalar_tensor_tensor(out=junk, in0=iota, scalar=posf[:, hh, k:k + 1], in1=cid[:, hh, :],
                                         op0=ALU.is_equal, op1=ALU.mult, accum_out=eidf[:, sl:sl + 1])
            yield
        if layer:
            dve.tensor_scalar(out=eidf, in0=eidf, scalar1=float(layer * 16384), scalar2=None, op0=ALU.add)
        dve.tensor_copy(out=eid, in_=eidf)
        dve.tensor_scalar(out=negm, in0=scv[:, :, 0], scalar1=-1.0, scalar2=None, op0=ALU.mult)
        for hh in range(8):
            act.activation(out=gate[:, hh, :], in_=scv[:, hh, :], func=AF.Exp, bias=negm[:, hh:hh + 1],
                           accum_out=zsum[:, hh:hh + 1])
        dve.reciprocal(out=zsum, in_=zsum)
        dve.tensor_tensor(out=gate, in0=gate, in1=zsum.un(2).bc([128, 8, 16]), op=ALU.mult)
        yield

    def exhaust(g):
        for _ in g:
            pass

    gi = 0
    exhaust(front(tiles[0], 0))
    for idx, t in enumerate(tiles):
        par = idx % 2
        h = H[t]
        eid, gate, xnb = eids[par], gates[par], xnbs[par]
        gflat = gate.re("p h k -> p (h k)")
        npr = 32 if (layer == 0 and t == 0) else 128
        nxt = front(tiles[idx + 1], 1 - par) if idx + 1 < len(tiles) else None
        for sl in range(128):
            g = gb[gi % NG]
            gi += 1
            K.gather(g[0:npr], uv_tab, eid[0:npr, sl:sl + 1], extra_reads=uv_bufs)
            hv, ac = hvs[sl % NR], acs[sl % NR]
            dve.scalar_tensor_tensor(out=g[0:npr, 0:D], in0=g[0:npr, 0:D], scalar=1.0, in1=xnb[0:npr], op0=ALU.mult,
                                     op1=ALU.mult, accum_out=hv[0:npr])
            act.activation(out=ac[0:npr], in_=hv[0:npr], func=AF.Gelu)
            act.activation(out=ac[0:npr], in_=ac[0:npr], func=AF.Copy, scale=gflat[0:npr, sl:sl + 1])
            dgt = dgs[sl % 3]
            act.activation(out=dgt[0:npr, 0:npr], in_=C["ident"][0:npr, 0:npr], func=AF.Copy, scale=ac[0:npr, 0:1])
            for q in range(4):
                pe.matmul(out=accb[q][0:npr], lhsT=dgt[0:npr, 0:npr], rhs=g[0:npr, D + q * 512:D + (q + 1) * 512],
                          start=(sl == 0), stop=(sl == 127))
            if nxt is not None and sl % 4 == 3:
                next(nxt, None)
        if nxt is not None:
            exhaust(nxt)
        for q in range(4):
            hq_ = h[0:npr, q * 512:(q + 1) * 512]
            dve.tensor_tensor(out=hq_, in0=hq_, in1=accb[q][0:npr], op=ALU.add)
    K.release(m)


def stage_ple(K, C, I, H, layer, tiles):
    dve, act, pe = K.dve, K.act, K.pe
    m = K.mark()
    gple = K.alloc("gple", [128, D])
    K.dma("sp", gple, I["norm_ple"][layer].partition_broadcast(128))
    bg = K.alloc("bg", [128, D])
    K.dma("sp", bg, I["ple_b_gate"][layer].partition_broadcast(128))
    wp = K.alloc("wp", [128, 2, D], BF16)
    K.dma("pool", wp, I["ple_w_proj"][layer].rearrange("(k p) n -> p k n", p=128))
    pnT = [K.alloc(f"pnT{t}", [128, 16, 128], BF16) for t in range(len(tiles))]
    pT = [K.alloc(f"pT{t}", [128, 2, 128], BF16) for t in range(len(tiles))]
    pn = K.alloc("pn", [128, D], BF16)
    pt = K.alloc("pt", [128, 256])
    ptb = K.alloc("ptb", [128, 256], BF16)
    sq = K.alloc("sqe", [128, D])
    ss = K.alloc("sse", [128, 1])
    gs = K.alloc("gs", [128, 256])
    wg = [K.alloc(f"wg{i}", [128, 16, 256], BF16) for i in range(2)]
    print("PLE arena top:", K.top)
    for ti_, t in enumerate(tiles):
        rmsnorm_tok(K, H[t], gple, pn, sq, ss)
        transpose_chunks(K, C, pn, pnT[ti_], 16)
        if t == 0:
            dve.memset(ap=pt, constant=0.0)
            K.dma("sp", pt[0:32, :], I["pp"][layer, 0:32, :])
        elif t == 9:
            K.dma("sp", pt, I["psm"][layer])
        else:
            K.dma("sp", pt, I["pp"][layer, 32 + (t - 1) * 128:32 + t * 128, :])
        act.activation(out=ptb, in_=pt, func=AF.Copy)
        transpose_chunks(K, C, ptb, pT[ti_], 2)
    n = 0
    for blk in range(8):
        w = wg[blk % 2]
        cs_ = slice(blk * 256, (blk + 1) * 256)
        K.dma("pool", w, I["ple_w_gate"][layer][:, cs_].rearrange("(k p) n -> p k n", p=128))
        for ti_, t in enumerate(tiles):
            pg = K.bank(n % 4, F32, [128, 256])
            pp_ = K.bank(4 + n % 4, F32, [128, 256])
            n += 1
            for k in range(16):
                pe.matmul(out=pg, lhsT=pnT[ti_][:, k, :], rhs=w[:, k, :], start=(k == 0), stop=(k == 15))
            for k in range(2):
                pe.matmul(out=pp_, lhsT=pT[ti_][:, k, :], rhs=wp[:, k, cs_], start=(k == 0), stop=(k == 1))
            dve.tensor_tensor(out=gs, in0=pg, in1=bg[:, cs_], op=ALU.add)
            act.activation(out=gs, in_=gs, func=AF.Sigmoid)
            dve.tensor_tensor(out=gs, in0=gs, in1=pp_, op=ALU.mult)
            hs = H[t][:, cs_]
            dve.tensor_tensor(out=hs, in0=hs, in1=gs, op=ALU.add)
    K.release(m)


NCT = 1184


def stage_conf(K, C, I, O, H):
    dve, act, pe, pool = K.dve, K.act, K.pe, K.pool
    w_in = I["conf_w_in"]
    mA = K.mark()
    cbf = K.alloc("cbf", [128, 16, 1152], BF16)
    mB = K.mark()
    def chan(name, src_row):
        t = K.alloc(name, [128, 16])
        K.dma("sp", t, src_row.rearrange("(c p) -> p c", p=128), allow_slow_non_contiguous=True)
        return t
    b_v = chan("b_v", I["conf_b_in"][0, 0:2048])
    b_g = chan("b_g", I["conf_b_in"][0, 2048:4096])
    dwb = chan("dwb", I["conf_dw_b"][0])
    dww = K.alloc("dww", [128, 16, 31])
    for j in range(31):
        K.dma("sp", dww[:, :, j], I["conf_dw_w"][j].rearrange("(c p) -> p c", p=128), allow_slow_non_contiguous=True)
    sel = K.alloc("selc", [128, 4])
    K.dma("sp", sel, I["sel"][:, :])
    aT = K.alloc("aTc", [128, 16, NCT], BF16)
    mT = K.mark()
    gmix = K.alloc("gmix1", [128, D])
    K.dma("sp", gmix, I["norm_mix"][1].partition_broadcast(128))
    xn = K.alloc("xnc", [128, D], BF16)
    xT = K.alloc("xTc", [128, 16, 128], BF16)
    sq = K.alloc("sqc", [128, D])
    ss = K.alloc("ssc", [128, 1])
    for t in range(10):
        rmsnorm_tok(K, H[t], gmix, xn, sq, ss)
        transpose_chunks(K, C, xn, xT, 16)
        if t == 0:
            dve.tensor_copy(out=aT[:, :, 0:32], in_=xT[:, :, 0:32])
        else:
            dve.tensor_copy(out=aT[:, :, 32 + (t - 1) * 128:32 + t * 128], in_=xT)
    K.release(mT)
    st = K.alloc("stc", [128, 4, 128])
    dve.memset(ap=st, constant=0.0)
    scc2 = I["scc"].rearrange("s t c -> (s t) c")
    K.dma("sp", O["cbs"][:, 0:22, :], I["scc"][:, 8:30, :], final=True)
    ext_p = K.alloc("ext_p", [128, 30 + 1024])
    ext_s = K.alloc("ext_s", [128, 16, 38])
    exs = K.alloc("exs", [128, 512])
    glu = K.alloc("glu", [128, NCT])
    sg = K.alloc("sg", [128, 512])
    cacc = K.alloc("cacc", [128, 1152])
    cb_sb = K.alloc("cb_sb", [128, 2, 128])
    wv = [K.alloc(f"wv{i}", [128, 16, 128], BF16) for i in range(1)]
    wgt = [K.alloc(f"wgt{i}", [128, 16, 128], BF16) for i in range(1)]
    print("CONF arena top:", K.top)
    blocks = [(0, 512), (512, 512), (1024, 160)]
    for c in range(16):
        wv_, wg_ = wv[0], wgt[0]
        for r in range(4):
            nr = 128 if r < 3 else 96
            K.dma("sp", st[0:nr, r, :], scc2[r * 128:r * 128 + nr, c * 128:(c + 1) * 128])
        K.dma("pool", wv_, w_in[:, c * 128:(c + 1) * 128].rearrange("(k p) n -> p k n", p=128))
        K.dma("pool", wg_, w_in[:, 2048 + c * 128:2048 + (c + 1) * 128].rearrange("(k p) n -> p k n", p=128))
        pst = K.bank(7, F32, [128, 512])
        for r in range(4):
            pe.transpose(out=pst[:, r * 128:(r + 1) * 128], in_=st[:, r, :], identity=C["ident"])
        act.activation(out=exs, in_=pst, func=AF.Copy)
        dve.tensor_copy(out=ext_s[:, :, 0:30], in_=exs[:, 0:480].re("p (s t) -> p s t", t=30))
        for bi, (c0, nb) in enumerate(blocks):
            pv = K.bank(2 * (bi % 2), F32, [128, 512])[:, 0:nb]
            pg = K.bank(2 * (bi % 2) + 1, F32, [128, 512])[:, 0:nb]
            for k in range(16):
                pe.matmul(out=pv, lhsT=wv_[:, k, :], rhs=aT[:, k, c0:c0 + nb], start=(k == 0), stop=(k == 15))
            for k in range(16):
                pe.matmul(out=pg, lhsT=wg_[:, k, :], rhs=aT[:, k, c0:c0 + nb], start=(k == 0), stop=(k == 15))
            act.activation(out=sg[:, 0:nb], in_=pg, func=AF.Sigmoid, bias=b_g[:, c:c + 1])
            dve.scalar_tensor_tensor(out=glu[:, c0:c0 + nb], in0=pv, scalar=b_v[:, c:c + 1], in1=sg[:, 0:nb],
                                     op0=ALU.add, op1=ALU.mult)
        dve.tensor_scalar(out=ext_p[:, 0:30], in0=glu[:, 2:32], scalar1=sel[:, 1:2], scalar2=None, op0=ALU.mult)
        act.activation(out=ext_p[:, 30:1054], in_=glu[:, 32:1056], func=AF.Copy)
        act.activation(out=ext_s[:, :, 30:38], in_=glu[:, 1056:1184].re("p (s t) -> p s t", t=8), func=AF.Copy)
        pcb = K.bank(6, F32, [128, 2, 128])
        pe.transpose(out=pcb[:, 0, :], in_=ext_p[:, 926:1054], identity=C["ident"])
        pe.transpose(out=pcb[:, 1, :], in_=glu[:, 1056:1184], identity=C["ident"])
        act.activation(out=cb_sb, in_=pcb, func=AF.Copy)
        K.dma("sp", O["cbp"][:, c * 128:(c + 1) * 128], cb_sb[98:128, 0, :], final=True)
        for s_ in range(16):
            K.dma("sp", O["cbs"][s_, 22:30, c * 128:(c + 1) * 128], cb_sb[8 * s_:8 * s_ + 8, 1, :], final=True)
        cp = cacc[:, 0:1024]
        cs = cacc[:, 1024:1152].re("p (s t) -> p s t", t=8)
        dve.tensor_scalar(out=cp, in0=ext_p[:, 0:1024], scalar1=dww[:, c, 0:1], scalar2=dwb[:, c:c + 1],
                          op0=ALU.mult, op1=ALU.add)
        dve.tensor_scalar(out=cs, in0=ext_s[:, :, 0:8], scalar1=dww[:, c, 0:1], scalar2=dwb[:, c:c + 1],
                          op0=ALU.mult, op1=ALU.add)
        for j in range(1, 31):
            dve.scalar_tensor_tensor(out=cp, in0=ext_p[:, j:j + 1024], scalar=dww[:, c, j:j + 1], in1=cp,
                                     op0=ALU.mult, op1=ALU.add)
            dve.scalar_tensor_tensor(out=cs, in0=ext_s[:, :, j:j + 8], scalar=dww[:, c, j:j + 1], in1=cs,
                                     op0=ALU.mult, op1=ALU.add)
        act.activation(out=cbf[:, c, :], in_=cacc, func=AF.Copy)
    K.release(mB)
    mC = K.mark()
    lng = K.alloc("lng", [128, 16])
    lnb = K.alloc("lnb", [128, 16])
    K.dma("sp", lng, I["conf_ln_g"][0].rearrange("(c p) -> p c", p=128), allow_slow_non_contiguous=True)
    K.dma("sp", lnb, I["conf_ln_b"][0].rearrange("(c p) -> p c", p=128), allow_slow_non_contiguous=True)
    cf = K.alloc("cf", [128, 512])
    c2 = K.alloc("c2", [128, 512])
    mu = K.alloc("mu", [128, 512])
    rstd = K.alloc("rstd", [128, 512])
    tmpn = K.alloc("tmpn", [128, 512])
    for (c0, nb) in [(0, 512), (512, 512), (1024, 128)]:
        psum_s = K.bank(0, F32, [128, 512])[:, 0:nb]
        psum_q = K.bank(1, F32, [128, 512])[:, 0:nb]
        for c in range(16):
            act.activation(out=cf[:, 0:nb], in_=cbf[:, c, c0:c0 + nb], func=AF.Copy)
            act.activation(out=c2[:, 0:nb], in_=cbf[:, c, c0:c0 + nb], func=AF.Square)
            pe.matmul(out=psum_s, lhsT=C["ones"], rhs=cf[:, 0:nb], start=(c == 0), stop=(c == 15))
            pe.matmul(out=psum_q, lhsT=C["ones"], rhs=c2[:, 0:nb], start=(c == 0), stop=(c == 15))
        act.activation(out=mu[:, 0:nb], in_=psum_s, func=AF.Copy, scale=1.0 / D)
        dve.tensor_tensor(out=tmpn[:, 0:nb], in0=mu[:, 0:nb], in1=mu[:, 0:nb], op=ALU.mult)
        dve.scalar_tensor_tensor(out=rstd[:, 0:nb], in0=psum_q, scalar=1.0 / D, in1=tmpn[:, 0:nb],
                                 op0=ALU.mult, op1=ALU.subtract)
        act.activation(out=rstd[:, 0:nb], in_=rstd[:, 0:nb], func=AF.Sqrt, bias=EPS)
        dve.reciprocal(out=rstd[:, 0:nb], in_=rstd[:, 0:nb])
        for c in range(16):
            dve.tensor_tensor(out=tmpn[:, 0:nb], in0=cbf[:, c, c0:c0 + nb], in1=mu[:, 0:nb], op=ALU.subtract)
            dve.tensor_tensor(out=tmpn[:, 0:nb], in0=tmpn[:, 0:nb], in1=rstd[:, 0:nb], op=ALU.mult)
            act.activation(out=cbf[:, c, c0:c0 + nb], in_=tmpn[:, 0:nb], func=AF.Silu,
                           scale=lng[:, c:c + 1], bias=lnb[:, c:c + 1])
    bo = K.alloc("bo", [128, D])
    K.dma("sp", bo, I["conf_b_out"][0].partition_broadcast(128))
    wo = [K.alloc(f"wo{i}", [128, 16, 512], BF16) for i in range(2)]
    ysb = K.alloc("ysb", [128, 512])
    n = 0
    for blk in range(4):
        w = wo[blk % 2]
        K.dma("pool", w, I["conf_w_out"][:, blk * 512:(blk + 1) * 512].rearrange("(k p) n -> p k n", p=128))
        for t in range(1, 10):
            ps = K.bank(2 + n % 6, F32, [128, 512])
            n += 1
            for c in range(16):
                pe.matmul(out=ps, lhsT=cbf[:, c, (t - 1) * 128:t * 128], rhs=w[:, c, :], start=(c == 0), stop=(c == 15))
            dve.tensor_tensor(out=ysb, in0=ps, in1=bo[:, blk * 512:(blk + 1) * 512], op=ALU.add)
            hs = H[t][:, blk * 512:(blk + 1) * 512]
            dve.tensor_tensor(out=hs, in0=hs, in1=ysb, op=ALU.add)
    K.release(mA)


def stage_final(K, C, I, O, H):
    m = K.mark()
    gf = K.alloc("gf", [128, D])
    K.dma("sp", gf, I["final_norm"][0].partition_broadcast(128))
    yo = [K.alloc(f"yo{i}", [128, D]) for i in range(2)]
    sq = K.alloc("sqf", [128, D])
    ss = K.alloc("ssf", [128, 1])
    for t in range(1, 10):
        y = yo[t % 2]
        rmsnorm_tok(K, H[t], gf, y, sq, ss)
        if t < 9:
            K.dma("sp", O["yp"][(t - 1) * 128:t * 128, :], y, final=True)
        else:
            K.dma("sp", O["ys"][:, :], y, final=True)
    K.release(m)

W_SHAPES = {
    "norm_mix": (2, D), "norm_ffn": (2, D), "norm_ple": (2, D), "final_norm": (1, D),
    "gdn_w_in": (D, 12352), "gdn_conv_w": (4, 8192), "gdn_a_log": (1, 32), "gdn_dt_bias": (1, 32),
    "gdn_o_norm": (1, 128), "gdn_w_out": (4096, D),
    "conf_w_in": (D, 4096), "conf_b_in": (1, 4096), "conf_dw_w": (31, D), "conf_dw_b": (1, D),
    "conf_ln_g": (1, D), "conf_ln_b": (1, D), "conf_w_out": (D, D), "conf_b_out": (1, D),
    "peer_w_q": (2, D, D), "peer_keys1": (2, 128, 128), "peer_keys2": (2, 128, 128),
    "peer_u": (2 * 16384, D), "peer_v": (2 * 16384, D),
    "ple_w_proj": (2, 256, D), "ple_w_gate": (2, D, D), "ple_b_gate": (2, D),
}
ACT_SHAPES = {
    "xp": (2048, D), "xo": (1056, D), "pp": (2, 1056, 256), "xs": (128, D), "psm": (2, 128, 256),
    "sel": (128, 4), "sgr": (16, 32, 128, 128), "sgc": (16, 3, 8192), "scc": (16, 30, D),
}
OUT_SHAPES = {
    "yp": (1024, D), "ys": (128, D), "gSp": (32, 128, 128), "gSs": (16, 32, 128, 128),
    "gbp": (3, 8192), "gbs": (16, 3, 8192), "cbp": (30, D), "cbs": (16, 30, D),
}
STAGE_INPUTS = {
    "gdn": ["xp", "xs", "sel", "sgr", "sgc", "norm_mix", "gdn_w_in", "gdn_conv_w", "gdn_a_log", "gdn_dt_bias",
            "gdn_o_norm"],
    "oproj": ["xo", "xs", "gdn_w_out"],
    "peer0": ["norm_ffn", "peer_w_q", "peer_keys1", "peer_keys2", "peer_u", "peer_v"],
    "ple0": ["norm_ple", "ple_w_proj", "ple_w_gate", "ple_b_gate", "pp", "psm"],
    "conf": ["norm_mix", "sel", "scc", "conf_w_in", "conf_b_in", "conf_dw_w", "conf_dw_b", "conf_ln_g", "conf_ln_b",
             "conf_w_out", "conf_b_out"],
    "peer1": ["norm_ffn", "peer_w_q", "peer_keys1", "peer_keys2", "peer_u", "peer_v"],
    "ple1": ["norm_ple", "ple_w_proj", "ple_w_gate", "ple_b_gate", "pp", "psm"],
    "final": ["final_norm"],
}
STAGE_OUTPUTS = {"gdn": ["gSp", "gSs", "gbp", "gbs"], "conf": ["cbp", "cbs"], "final": ["yp", "ys"]}
ALL_STAGES = ("gdn", "oproj", "peer0", "ple0", "conf", "peer1", "ple1", "final")


def build(stages=("gdn",)):
    K = Kern()
    need_in, need_out = [], []
    for s in stages:
        need_in += [n for n in STAGE_INPUTS[s] if n not in need_in]
        need_out += [n for n in STAGE_OUTPUTS.get(s, []) if n not in need_out]
    I = {n: K.din(n, ACT_SHAPES.get(n) or W_SHAPES[n]) for n in need_in}
    O = {n: K.dout(n, OUT_SHAPES[n]) for n in need_out}
    C = build_consts(K)
    osel_scr = K.dscr("osel_scr", [32, 128, NOWN], BF16)
    TB, conv_chunks = None, []
    if "peer0" in stages or "peer1" in stages:
        scr = K.nc.dram_tensor("peer_uvb", [2 * 16384, 2 * D], BF16, kind="Internal").ap()
        TB = (scr, {0: [], 1: []})
        for layer in range(2):
            for ci_, nm in enumerate(("u", "v")):
                for r0 in range(layer * 16384, (layer + 1) * 16384, 1024):
                    b_ = Buf(f"{nm}b{r0}")
                    TB[1][layer].append(b_)
                    conv_chunks.append((V(scr[r0:r0 + 1024, ci_ * D:(ci_ + 1) * D], b_),
                                        I[f"peer_{nm}"][r0:r0 + 1024, :]))
    base = K.mark()
    if "gdn" in stages:
        stage_gdn(K, C, I, O, osel_scr, conv_chunks)
        K.release(base)
    for dst, src in conv_chunks:
        K.dma("pool", dst, src, bulk=True)
    H = [K.alloc(f"H{t}", [128, D]) for t in range(10)]
    if "oproj" in stages:
        stage_oproj(K, C, I, H, osel_scr)
    if "peer0" in stages:
        stage_peer(K, C, I, H, 0, list(range(10)), TB)
    if "ple0" in stages:
        stage_ple(K, C, I, H, 0, list(range(10)))
    if "conf" in stages:
        stage_conf(K, C, I, O, H)
    if "peer1" in stages:
        stage_peer(K, C, I, H, 1, list(range(1, 10)), TB)
    if "ple1" in stages:
        stage_ple(K, C, I, H, 1, list(range(1, 10)))
    if "final" in stages:
        stage_final(K, C, I, O, H)
    K.S.emit()
    K.es.close()
    return K


def prep_inputs(inp, names):
    f = lambda a: np.ascontiguousarray(a, dtype=np.float32)
    shared = {}
    for n, shp in W_SHAPES.items():
        if n in names:
            shared[n] = f(np.asarray(inp[n]).reshape(shp))
    maps = []
    for c in range(8):
        b, half = c // 2, c % 2
        m = dict(shared)
        xp = np.asarray(inp["x_prompt"][b])
        if "xp" in names:
            m["xp"] = f(xp)
        lo = half * 1024 - 32
        if "xo" in names:
            xo = np.zeros((1056, D), np.float32)
            if lo >= 0:
                xo[:] = xp[lo:lo + 1056]
            else:
                xo[32:] = xp[0:1024]
            m["xo"] = xo
        if "pp" in names:
            pp = np.zeros((2, 1056, 256), np.float32)
            pr = np.asarray(inp["p_prompt"][:, b])
            if lo >= 0:
                pp[:] = pr[:, lo:lo + 1056]
            else:
                pp[:, 32:] = pr[:, 0:1024]
            m["pp"] = pp
        if "xs" in names:
            m["xs"] = f(np.asarray(inp["x_sample"][16 * c:16 * c + 16]).reshape(128, D))
        if "psm" in names:
            m["psm"] = f(np.asarray(inp["p_sample"][:, 16 * c:16 * c + 16]).reshape(2, 128, 256))
        if "sel" in names:
            s = np.zeros((128, 4), np.float32)
            s[:, 0] = 1.0 - half
            s[:, 1] = float(half)
            m["sel"] = s
        if "sgr" in names:
            m["sgr"] = f(inp["state_gdn_recurrent"][0, 16 * c:16 * c + 16])
        if "sgc" in names:
            m["sgc"] = f(inp["state_gdn_conv"][0, 16 * c:16 * c + 16])
        if "scc" in names:
            m["scc"] = f(inp["state_conf_conv"][0, 16 * c:16 * c + 16])
        maps.append(m)
    return maps


_PROG = {}


def _program():
    if "K" not in _PROG:
        _PROG["K"] = build(ALL_STAGES)
    return _PROG["K"]


def assemble(results, n_cores=8):
    nb = n_cores // 2
    y_p = np.zeros((4, 2048, D), np.float32)
    y_s = np.zeros((128, 8, D), np.float32)
    gS_p = np.zeros((1, 4, 32, 128, 128), np.float32)
    gS_s = np.zeros((1, 128, 32, 128, 128), np.float32)
    gb_p = np.zeros((1, 4, 3, 8192), np.float32)
    gb_s = np.zeros((1, 128, 3, 8192), np.float32)
    cb_p = np.zeros((1, 4, 30, D), np.float32)
    cb_s = np.zeros((1, 128, 30, D), np.float32)
    for c in range(n_cores):
        r = results[c]
        b, half = c // 2, c % 2
        y_p[b, half * 1024:(half + 1) * 1024] = r["yp"]
        y_s[16 * c:16 * c + 16] = r["ys"].reshape(16, 8, D)
        gS_s[0, 16 * c:16 * c + 16] = r["gSs"]
        gb_s[0, 16 * c:16 * c + 16] = r["gbs"]
        cb_s[0, 16 * c:16 * c + 16] = r["cbs"]
        if half == 0:
            gS_p[0, b] = r["gSp"]
            gb_p[0, b] = r["gbp"]
        else:
            cb_p[0, b] = r["cbp"]
    return (y_p, y_s, gS_p, gS_s, gb_p, gb_s, cb_p, cb_s)


def kernel(**inputs):
    K = _program()
    maps = prep_inputs(inputs, list(K.ins.keys()))
    res = run_bass_kernel_spmd(K.nc, maps, core_ids=list(range(8)))
    return assemble(res.results, 8)
```
